# Optimizing a Trainium2 kernel written in Bass

```python
import math
import jax
import jax.numpy as jnp
from jax import lax
import numpy as np

D_MODEL = 1024
BATCH = 4
SEQ = 4096
DEPTH = 2
DEC_BATCH = 128
DEC_SEQ = 8
PAST_LEN = 2048
PAGE_SIZE = 128

N_EVEN = (DEPTH + 1) // 2
N_ODD = DEPTH // 2
HEAD_DIM = 64
A_HEADS = 8
A_GROUPS = ((128, 1), (512, 4), (2048, 16))
A_MAX_WINDOW = 2048
A_QBLOCK = 128
A_WIDTH = A_HEADS * HEAD_DIM
B_HEADS = 8
B_DK = 64
B_DV = 64
B_CHUNK = 64
B_WIDTH = B_HEADS * B_DV
C_HEADS = 4
C_HD = 64
C_QBLOCK = 128
C_WIDTH = C_HEADS * 2 * C_HD
S5_GROUPS = 32
S5_GROUP_CH = 16
S5_STATE = 64
S5_WIDTH = S5_GROUPS * S5_GROUP_CH
EVEN_IN = 3 * A_WIDTH + 2 * B_HEADS * B_DK + 2 * B_WIDTH
EVEN_MIX = A_WIDTH + B_WIDTH
ODD_IN = 3 * C_WIDTH + S5_WIDTH
ODD_MIX = C_WIDTH + S5_WIDTH
D_FF = 2816
N_EXPERTS = 8
TOP_K = 2
EXPERT_FF = 2816
RMS_EPS = 1e-6

kernel_name = 'hybrid_dilated_hgrn2_diffattn_s5_step'


def rmsnorm(x, g):
    xf = x.astype(jnp.float32)
    y = xf * lax.rsqrt(jnp.mean(xf * xf, axis=-1, keepdims=True) + RMS_EPS)
    return (y * g.astype(jnp.float32)).astype(x.dtype)


def alibi_slopes(n):
    return jnp.asarray(2.0 ** (-8.0 * np.arange(1, n + 1) / n), dtype=jnp.float32)


def dilated_attention(q, k_ext, v_ext, q_start, n_pad):
    b, tq, h, hd = q.shape
    f32 = jnp.float32
    blk = A_QBLOCK if tq % A_QBLOCK == 0 else tq
    span = A_MAX_WINDOW + blk
    slopes = alibi_slopes(h)
    ti = jnp.arange(blk)

    def one_block(bi):
        t0 = bi * blk
        qb = lax.dynamic_slice_in_dim(q, t0, blk, axis=1).astype(f32) * hd ** -0.5
        start = q_start - A_MAX_WINDOW + t0
        kb = lax.dynamic_slice_in_dim(k_ext, start, span, axis=1)
        vb = lax.dynamic_slice_in_dim(v_ext, start, span, axis=1)
        outs, lses = [], []
        for window, dil in A_GROUPS:
            j = jnp.arange(window // dil + 1)
            loc = A_MAX_WINDOW + ti[:, None] - dil * j[None, :]
            valid = (start + loc) >= n_pad
            kg = jnp.take(kb, loc, axis=1).astype(f32)
            vg = jnp.take(vb, loc, axis=1).astype(f32)
            s = jnp.einsum('bthd,btjhd->bhtj', qb, kg) - slopes[:, None, None] * (dil * j).astype(f32)
            s = jnp.where(valid, s, -jnp.inf)
            m = jnp.max(s, axis=-1, keepdims=True)
            pr = jnp.exp(s - m)
            den = jnp.sum(pr, axis=-1)
            o = jnp.einsum('bhtj,btjhd->bthd', pr, vg) / jnp.swapaxes(den, 1, 2)[..., None]
            outs.append(o)
            lses.append(m[..., 0] + jnp.log(den))
        wts = jnp.swapaxes(jax.nn.softmax(jnp.stack(lses), axis=0), 2, 3)[..., None]
        return jnp.sum(wts * jnp.stack(outs), axis=0)

    ob = lax.map(one_block, jnp.arange(tq // blk))
    return jnp.moveaxis(ob, 0, 1).reshape(b, tq, h, hd)


def hgrn2_recurrence(q, k, v, log_f, s0):
    b, t, h, dk = q.shape
    dv = v.shape[-1]
    c = B_CHUNK if t % B_CHUNK == 0 else t
    n = t // c

    def chunks(a):
        return jnp.moveaxis(a.reshape(b, n, c, *a.shape[2:]), 1, 0)

    causal = jnp.tril(jnp.ones((c, c), dtype=bool))[None, :, :, None, None]

    def step(s, inp):
        qc, kc, vc, gc = inp
        cum = jnp.cumsum(gc, axis=1)
        o_inter = jnp.einsum('bthk,bhkv->bthv', qc * jnp.exp(cum), s)
        decay = jnp.exp(jnp.where(causal, cum[:, :, None] - cum[:, None, :], -jnp.inf))
        att = jnp.einsum('bthk,bshk,btshk->bhts', qc, kc, decay)
        o_intra = jnp.einsum('bhts,bshv->bthv', att, vc)
        last = cum[:, -1]
        s_new = jnp.exp(last)[..., None] * s + jnp.einsum('bshk,bshv->bhkv', kc * jnp.exp(last[:, None] - cum), vc)
        return s_new, o_inter + o_intra

    s_fin, o = lax.scan(step, s0, (chunks(q), chunks(k), chunks(v), chunks(log_f)))
    return jnp.moveaxis(o, 0, 1).reshape(b, t, h, dv), s_fin


def diff_attention(q, k_segs, v_segs, kpos_segs, q_pos, lam, slopes):
    f32 = jnp.float32
    qf = q.astype(f32) * C_HD ** -0.5
    q1, q2 = qf[..., :C_HD], qf[..., C_HD:]
    s1, s2 = [], []
    for k, kp in zip(k_segs, kpos_segs):
        kf = k.astype(f32)
        dist = (q_pos[:, None] - kp[None, :]).astype(f32)
        bias = jnp.where(dist >= 0, -slopes[:, None, None] * dist, -jnp.inf)
        s1.append(jnp.einsum('bqhd,bkhd->bhqk', q1, kf[..., :C_HD]) + bias)
        s2.append(jnp.einsum('bqhd,bkhd->bhqk', q2, kf[..., C_HD:]) + bias)
    a = jax.nn.softmax(jnp.concatenate(s1, -1), axis=-1) - lam * jax.nn.softmax(jnp.concatenate(s2, -1), axis=-1)
    out = 0.0
    off = 0
    for v in v_segs:
        nk = v.shape[1]
        out = out + jnp.einsum('bhqk,bkhd->bqhd', a[..., off:off + nk], v.astype(f32))
        off += nk
    return out


def diff_attention_blocked(q, k_segs, v_segs, kpos_segs, q_pos0, lam, slopes):
    b, t, h, dq = q.shape
    blk = C_QBLOCK if t % C_QBLOCK == 0 else t

    def one_block(bi):
        t0 = bi * blk
        qb = lax.dynamic_slice_in_dim(q, t0, blk, axis=1)
        return diff_attention(qb, k_segs, v_segs, kpos_segs, q_pos0 + t0 + jnp.arange(blk), lam, slopes)

    ob = lax.map(one_block, jnp.arange(t // blk))
    return jnp.moveaxis(ob, 0, 1).reshape(b, t, h, 2 * C_HD)


def complex_affine_combine(e1, e2):
    a1r, a1i, b1r, b1i = e1
    a2r, a2i, b2r, b2i = e2
    return (a2r * a1r - a2i * a1i, a2r * a1i + a2i * a1r,
            a2r * b1r - a2i * b1i + b2r, a2r * b1i + a2i * b1r + b2i)


def s5_scan(u, h0_re, h0_im, a_re, a_im, log_dt, b_re, b_im, c_re, c_im, d_skip):
    dt = jnp.exp(log_dt)[:, None]
    mag = jnp.exp(a_re * dt)
    ab_re, ab_im = mag * jnp.cos(a_im * dt), mag * jnp.sin(a_im * dt)
    den = a_re * a_re + a_im * a_im
    xr, xi = ab_re - 1.0, ab_im
    z_re = (xr * a_re + xi * a_im) / den
    z_im = (xi * a_re - xr * a_im) / den
    bb_re = z_re[..., None] * b_re - z_im[..., None] * b_im
    bb_im = z_re[..., None] * b_im + z_im[..., None] * b_re
    bu_re = jnp.einsum('btgc,gpc->btgp', u, bb_re)
    bu_im = jnp.einsum('btgc,gpc->btgp', u, bb_im)
    bu_re = bu_re.at[:, 0].add(ab_re * h0_re - ab_im * h0_im)
    bu_im = bu_im.at[:, 0].add(ab_re * h0_im + ab_im * h0_re)
    elems = (jnp.broadcast_to(ab_re, bu_re.shape), jnp.broadcast_to(ab_im, bu_re.shape), bu_re, bu_im)
    _, _, h_re, h_im = lax.associative_scan(complex_affine_combine, elems, axis=1)
    y = jnp.einsum('btgp,gcp->btgc', h_re, c_re) - jnp.einsum('btgp,gcp->btgc', h_im, c_im) + d_skip * u
    return y, h_re[:, -1], h_im[:, -1]


def even_mixer(h, l, p, past):
    e = l // 2
    f32 = jnp.float32
    b, t, _ = h.shape
    proj = h @ p['w_in_even'][e]
    cuts = list(np.cumsum([A_WIDTH, A_WIDTH, A_WIDTH, B_HEADS * B_DK, B_HEADS * B_DK, B_WIDTH]))
    qa, ka, va, qb, fb, ib, gb = jnp.split(proj, cuts, axis=-1)
    qa = qa.reshape(b, t, A_HEADS, HEAD_DIM)
    ka = ka.reshape(b, t, A_HEADS, HEAD_DIM)
    va = va.reshape(b, t, A_HEADS, HEAD_DIM)
    if past is None:
        pad = A_MAX_WINDOW
        k_ext = jnp.concatenate([jnp.zeros((b, pad, A_HEADS, HEAD_DIM), ka.dtype), ka], axis=1)
        v_ext = jnp.concatenate([jnp.zeros((b, pad, A_HEADS, HEAD_DIM), va.dtype), va], axis=1)
        q_start = pad
        keep = min(A_MAX_WINDOW, t)
        new_k, new_v = ka[:, t - keep:], va[:, t - keep:]
    else:
        kbuf, vbuf = p_get(past, 'a_k')[e], p_get(past, 'a_v')[e]
        a_buf = kbuf.shape[1]
        pad = A_MAX_WINDOW - a_buf
        k_ext = jnp.concatenate([jnp.zeros((b, pad, A_HEADS, HEAD_DIM), ka.dtype), kbuf.astype(ka.dtype), ka], axis=1)
        v_ext = jnp.concatenate([jnp.zeros((b, pad, A_HEADS, HEAD_DIM), va.dtype), vbuf.astype(va.dtype), va], axis=1)
        q_start = pad + a_buf
        new_k, new_v = ka, va
    o_a = dilated_attention(qa, k_ext, v_ext, q_start, pad)
    lb = jnp.cumsum(jax.nn.softmax(p['hgrn_lb'].astype(f32), axis=0), axis=0)[l].reshape(B_HEADS, B_DK)
    f = lb + (1.0 - lb) * jax.nn.sigmoid(fb.reshape(b, t, B_HEADS, B_DK).astype(f32))
    qh = jax.nn.silu(qb.reshape(b, t, B_HEADS, B_DK).astype(f32)) * B_DK ** -0.5
    vh = ib.reshape(b, t, B_HEADS, B_DV).astype(f32)
    if past is None:
        s0 = jnp.zeros((b, B_HEADS, B_DK, B_DV), f32)
    else:
        s0 = p_get(past, 'hgrn')[e].astype(f32)
    o_b, s_fin = hgrn2_recurrence(qh, 1.0 - f, vh, jnp.log(f), s0)
    o_b = rmsnorm(o_b, p['hgrn_gnorm'][e]) * jax.nn.silu(gb.reshape(b, t, B_HEADS, B_DV).astype(f32))
    mixed = jnp.concatenate([o_a.reshape(b, t, A_WIDTH), o_b.reshape(b, t, B_WIDTH)], axis=-1).astype(h.dtype)
    return mixed @ p['w_out_even'][e], {'a_k': new_k, 'a_v': new_v, 'hgrn': s_fin}


def p_get(d, name):
    return d[name]


def odd_mixer(h, l, p, past):
    o = l // 2
    f32 = jnp.float32
    b, t, _ = h.shape
    proj = h @ p['w_in_odd'][o]
    qc, kc, vc, u = jnp.split(proj, [C_WIDTH, 2 * C_WIDTH, 3 * C_WIDTH], axis=-1)
    qc = qc.reshape(b, t, C_HEADS, 2 * C_HD)
    kc = kc.reshape(b, t, C_HEADS, 2 * C_HD)
    vc = vc.reshape(b, t, C_HEADS, 2 * C_HD)
    lam_init = 0.8 - 0.6 * math.exp(-0.3 * l)
    lam = (jnp.exp(jnp.sum(p['diff_lq1'][o].astype(f32) * p['diff_lk1'][o].astype(f32)))
           - jnp.exp(jnp.sum(p['diff_lq2'][o].astype(f32) * p['diff_lk2'][o].astype(f32))) + lam_init)
    slopes = alibi_slopes(C_HEADS)
    if past is None:
        k_segs, v_segs, kpos = (kc,), (vc,), (jnp.arange(t),)
        q_pos0 = 0
    else:
        table = p_get(past, 'page_table')
        past_len = table.shape[1] * PAGE_SIZE
        k_past = p_get(past, 'c_k')[o][table].reshape(b, past_len, C_HEADS, 2 * C_HD)
        v_past = p_get(past, 'c_v')[o][table].reshape(b, past_len, C_HEADS, 2 * C_HD)
        k_segs, v_segs = (k_past, kc), (v_past, vc)
        kpos = (jnp.arange(past_len), past_len + jnp.arange(t))
        q_pos0 = past_len
    o_c = diff_attention_blocked(qc, k_segs, v_segs, kpos, q_pos0, lam, slopes)
    o_c = rmsnorm(o_c, p['diff_subln'][o]) * (1.0 - lam_init)
    uu = u.reshape(b, t, S5_GROUPS, S5_GROUP_CH).astype(f32)
    if past is None:
        h0_re = jnp.zeros((b, S5_GROUPS, S5_STATE), f32)
        h0_im = jnp.zeros((b, S5_GROUPS, S5_STATE), f32)
    else:
        h0_re = p_get(past, 's5_re')[o].astype(f32)
        h0_im = p_get(past, 's5_im')[o].astype(f32)
    y, h_re, h_im = s5_scan(uu, h0_re, h0_im,
                            p['s5_a_re'][o].astype(f32), p['s5_a_im'][o].astype(f32), p['s5_log_dt'][o].astype(f32),
                            p['s5_b_re'][o].astype(f32), p['s5_b_im'][o].astype(f32),
                            p['s5_c_re'][o].astype(f32), p['s5_c_im'][o].astype(f32), p['s5_d'][o].astype(f32))
    z = jax.nn.gelu(y.reshape(b, t, S5_WIDTH))
    o_d = z * jax.nn.sigmoid(z @ p['s5_w_glu'][o].astype(f32) + p['s5_b_glu'][o].astype(f32))
    mixed = jnp.concatenate([o_c.reshape(b, t, C_WIDTH), o_d], axis=-1).astype(h.dtype)
    return mixed @ p['w_out_odd'][o], {'c_k': kc, 'c_v': vc, 's5_re': h_re, 's5_im': h_im}


def swiglu(h, wg, wu, wd):
    return (jax.nn.silu(h @ wg) * (h @ wu)) @ wd


def moe_swiglu(h, router_w, router_b, wg, wu, wd):
    b, t, d = h.shape
    xf = h.reshape(b * t, d)
    logits = (xf @ router_w + router_b).astype(jnp.float32)
    top_v, top_i = lax.top_k(logits, TOP_K)
    gates = jax.nn.softmax(top_v, axis=-1)
    combine = jnp.sum(jax.nn.one_hot(top_i, N_EXPERTS, dtype=jnp.float32) * gates[..., None], axis=1)
    y = jnp.zeros_like(xf)
    for e in range(N_EXPERTS):
        y = y + combine[:, e:e + 1].astype(h.dtype) * swiglu(xf, wg[e], wu[e], wd[e])
    return y.reshape(b, t, d)


def trunk(x, p, past):
    new = {'a_k': [], 'a_v': [], 'hgrn': [], 'c_k': [], 'c_v': [], 's5_re': [], 's5_im': []}
    for l in range(DEPTH):
        hn = rmsnorm(x, p['norm_mix'][l])
        if l % 2 == 0:
            out, st = even_mixer(hn, l, p, past)
        else:
            out, st = odd_mixer(hn, l, p, past)
        for name in st:
            new[name].append(st[name])
        x = x + out
        hn = rmsnorm(x, p['norm_ffn'][l])
        if l % 2 == 0:
            e = l // 2
            x = x + swiglu(hn, p['ffn_w_gate'][e], p['ffn_w_up'][e], p['ffn_w_down'][e])
        else:
            o = l // 2
            x = x + moe_swiglu(hn, p['moe_router_w'][o], p['moe_router_b'][o],
                               p['moe_w_gate'][o], p['moe_w_up'][o], p['moe_w_down'][o])
    y = rmsnorm(x, p['norm_final'])
    return y, {name: jnp.stack(v) for name, v in new.items()}


def setup_inputs(seed: int = 0) -> dict:
    key = jax.random.key(seed)
    keys = iter(jax.random.split(key, 64))
    f32 = jnp.float32

    def nrm(shape, scale=1.0):
        return scale * jax.random.normal(next(keys), shape, f32)

    def gain(shape):
        return 1.0 + 0.01 * jax.random.normal(next(keys), shape, f32)

    n_pages = PAST_LEN // PAGE_SIZE
    n_used = DEC_BATCH * n_pages
    n_phys = n_used + max(1, n_used // 4)
    a_buf = min(A_MAX_WINDOW, PAST_LEN)
    page_table = jax.random.permutation(next(keys), n_phys)[:n_used].reshape(DEC_BATCH, n_pages).astype(jnp.int32)
    s5_im_init = jnp.pi * jnp.arange(S5_STATE, dtype=f32)[None, None, :]
    return {
        'x_prompt': nrm((BATCH, SEQ, D_MODEL)),
        'x_sample': nrm((DEC_BATCH, DEC_SEQ, D_MODEL)),
        'cache_a_k': nrm((N_EVEN, DEC_BATCH, a_buf, A_HEADS, HEAD_DIM)),
        'cache_a_v': nrm((N_EVEN, DEC_BATCH, a_buf, A_HEADS, HEAD_DIM)),
        'state_hgrn': nrm((N_EVEN, DEC_BATCH, B_HEADS, B_DK, B_DV), 0.5),
        'cache_c_k': nrm((N_ODD, n_phys, PAGE_SIZE, C_HEADS, 2 * C_HD)),
        'cache_c_v': nrm((N_ODD, n_phys, PAGE_SIZE, C_HEADS, 2 * C_HD)),
        'state_s5_re': nrm((N_ODD, DEC_BATCH, S5_GROUPS, S5_STATE), 0.5),
        'state_s5_im': nrm((N_ODD, DEC_BATCH, S5_GROUPS, S5_STATE), 0.5),
        'page_table': page_table,
        'norm_mix': gain((DEPTH, D_MODEL)),
        'norm_ffn': gain((DEPTH, D_MODEL)),
        'norm_final': gain((D_MODEL,)),
        'w_in_even': nrm((N_EVEN, D_MODEL, EVEN_IN), D_MODEL ** -0.5),
        'w_out_even': nrm((N_EVEN, EVEN_MIX, D_MODEL), EVEN_MIX ** -0.5),
        'hgrn_lb': nrm((DEPTH + 1, B_HEADS * B_DK), 0.1),
        'hgrn_gnorm': gain((N_EVEN, B_DV)),
        'ffn_w_gate': nrm((N_EVEN, D_MODEL, D_FF), D_MODEL ** -0.5),
        'ffn_w_up': nrm((N_EVEN, D_MODEL, D_FF), D_MODEL ** -0.5),
        'ffn_w_down': nrm((N_EVEN, D_FF, D_MODEL), D_FF ** -0.5),
        'w_in_odd': nrm((N_ODD, D_MODEL, ODD_IN), D_MODEL ** -0.5),
        'w_out_odd': nrm((N_ODD, ODD_MIX, D_MODEL), ODD_MIX ** -0.5),
        'diff_lq1': nrm((N_ODD, C_HD), 0.1),
        'diff_lk1': nrm((N_ODD, C_HD), 0.1),
        'diff_lq2': nrm((N_ODD, C_HD), 0.1),
        'diff_lk2': nrm((N_ODD, C_HD), 0.1),
        'diff_subln': gain((N_ODD, 2 * C_HD)),
        's5_a_re': -0.5 + nrm((N_ODD, S5_GROUPS, S5_STATE), 0.01),
        's5_a_im': s5_im_init + nrm((N_ODD, S5_GROUPS, S5_STATE), 0.01),
        's5_log_dt': jax.random.uniform(next(keys), (N_ODD, S5_GROUPS), f32, math.log(1e-3), math.log(1e-1)),
        's5_b_re': nrm((N_ODD, S5_GROUPS, S5_STATE, S5_GROUP_CH), (2 * S5_GROUP_CH) ** -0.5),
        's5_b_im': nrm((N_ODD, S5_GROUPS, S5_STATE, S5_GROUP_CH), (2 * S5_GROUP_CH) ** -0.5),
        's5_c_re': nrm((N_ODD, S5_GROUPS, S5_GROUP_CH, S5_STATE), (2 * S5_STATE) ** -0.5),
        's5_c_im': nrm((N_ODD, S5_GROUPS, S5_GROUP_CH, S5_STATE), (2 * S5_STATE) ** -0.5),
        's5_d': nrm((N_ODD, S5_GROUPS, S5_GROUP_CH)),
        's5_w_glu': nrm((N_ODD, S5_WIDTH, S5_WIDTH), S5_WIDTH ** -0.5),
        's5_b_glu': nrm((N_ODD, S5_WIDTH), 0.01),
        'moe_router_w': nrm((N_ODD, D_MODEL, N_EXPERTS), D_MODEL ** -0.5),
        'moe_router_b': nrm((N_ODD, N_EXPERTS), 0.01),
        'moe_w_gate': nrm((N_ODD, N_EXPERTS, D_MODEL, EXPERT_FF), D_MODEL ** -0.5),
        'moe_w_up': nrm((N_ODD, N_EXPERTS, D_MODEL, EXPERT_FF), D_MODEL ** -0.5),
        'moe_w_down': nrm((N_ODD, N_EXPERTS, EXPERT_FF, D_MODEL), EXPERT_FF ** -0.5),
    }


def reference(x_prompt, x_sample, cache_a_k, cache_a_v, state_hgrn, cache_c_k, cache_c_v, state_s5_re, state_s5_im,
              page_table, norm_mix, norm_ffn, norm_final, w_in_even, w_out_even, hgrn_lb, hgrn_gnorm,
              ffn_w_gate, ffn_w_up, ffn_w_down, w_in_odd, w_out_odd, diff_lq1, diff_lk1, diff_lq2, diff_lk2,
              diff_subln, s5_a_re, s5_a_im, s5_log_dt, s5_b_re, s5_b_im, s5_c_re, s5_c_im, s5_d, s5_w_glu,
              s5_b_glu, moe_router_w, moe_router_b, moe_w_gate, moe_w_up, moe_w_down):
    p = {
        'norm_mix': norm_mix, 'norm_ffn': norm_ffn, 'norm_final': norm_final,
        'w_in_even': w_in_even, 'w_out_even': w_out_even, 'hgrn_lb': hgrn_lb, 'hgrn_gnorm': hgrn_gnorm,
        'ffn_w_gate': ffn_w_gate, 'ffn_w_up': ffn_w_up, 'ffn_w_down': ffn_w_down,
        'w_in_odd': w_in_odd, 'w_out_odd': w_out_odd, 'diff_lq1': diff_lq1, 'diff_lk1': diff_lk1,
        'diff_lq2': diff_lq2, 'diff_lk2': diff_lk2, 'diff_subln': diff_subln,
        's5_a_re': s5_a_re, 's5_a_im': s5_a_im, 's5_log_dt': s5_log_dt, 's5_b_re': s5_b_re, 's5_b_im': s5_b_im,
        's5_c_re': s5_c_re, 's5_c_im': s5_c_im, 's5_d': s5_d, 's5_w_glu': s5_w_glu, 's5_b_glu': s5_b_glu,
        'moe_router_w': moe_router_w, 'moe_router_b': moe_router_b,
        'moe_w_gate': moe_w_gate, 'moe_w_up': moe_w_up, 'moe_w_down': moe_w_down,
    }
    past = {'a_k': cache_a_k, 'a_v': cache_a_v, 'hgrn': state_hgrn, 'c_k': cache_c_k, 'c_v': cache_c_v,
            's5_re': state_s5_re, 's5_im': state_s5_im, 'page_table': page_table}
    y_prompt, sp = trunk(x_prompt, p, None)
    y_sample, ss = trunk(x_sample, p, past)
    return (y_prompt, y_sample,
            sp['a_k'], sp['a_v'], sp['hgrn'], sp['c_k'], sp['c_v'], sp['s5_re'], sp['s5_im'],
            ss['a_k'], ss['a_v'], ss['hgrn'], ss['c_k'], ss['c_v'], ss['s5_re'], ss['s5_im'])
```

```python
import contextlib
import os
import numpy as np
import ml_dtypes
import concourse.bass as bass
import concourse.mybir as mybir
from concourse.bass_utils import run_bass_kernel_spmd

F32 = mybir.dt.float32
BF16 = mybir.dt.bfloat16
I32 = mybir.dt.int32
ALU = mybir.AluOpType
AF = mybir.ActivationFunctionType
AX = mybir.AxisListType

NCORES = 8
D = 1024
SEQ = int(os.environ.get("KSEQ", "4096"))
NKEEP = min(2048, SEQ) // 128
NT = SEQ // 128
EVEN_IN = 3584
DFF = 2816
NFF = DFF // 128
EPOCH = 12000
NDSEM = 32
STRIP = 2944
A_SLOPES = [2.0 ** (-(h + 1)) for h in range(8)]


MAXPH = int(os.environ.get("KMAXPH", "99"))


class _Stop(Exception):
    pass


class Res:
    __slots__ = ("name", "w", "r", "excl")

    def __init__(self, name, excl=False):
        self.name = name
        self.w = None
        self.r = {}
        self.excl = excl


def _res(x):
    return x.res if hasattr(x, "res") else x


class Prog:
    def __init__(self, nc, es):
        self.nc = nc
        self.es = es
        self.q = {e: [] for e in ("pe", "act", "dve", "pool", "sp")}
        self.cnt = {e: 0 for e in ("pe", "act", "dve", "pool")}
        self.esems = {e: [] for e in self.cnt}
        self.known = {e: {} for e in self.q}
        self.dsems = [es.enter_context(nc.semaphore(f"dsem{i}")) for i in range(NDSEM)]
        self.dval = [0] * NDSEM
        self.dnext = 0
        self.dnext_sw = NDSEM - 8

    def _esem(self, E, idx):
        ep = idx // EPOCH
        while len(self.esems[E]) <= ep:
            self.esems[E].append(self.es.enter_context(self.nc.semaphore(f"es_{E}{len(self.esems[E])}")))
        return self.esems[E][ep], idx % EPOCH + 1

    def _wait(self, E, tok):
        if tok is None:
            return
        if tok[0] == "eng":
            _, F, idx = tok
            if F == E and E == "pe":
                return
            if self.known[E].get(F, -1) >= idx:
                return
            self.known[E][F] = idx
            sem, val = self._esem(F, idx)
        else:
            _, si, val = tok
            key = ("d", si)
            if self.known[E].get(key, 0) >= val:
                return
            self.known[E][key] = val
            sem = self.dsems[si]
        self.q[E].append(lambda e, sem=sem, val=val: e.wait_ge(sem, val))

    def _deps(self, E, reads, writes):
        for r in reads:
            self._wait(E, r.w)
            if r.excl:
                for t in list(r.r.values()):
                    if not (t[0] == "eng" and t[1] == E):
                        self._wait(E, t)
        for w in writes:
            self._wait(E, w.w)
            for t in list(w.r.values()):
                self._wait(E, t)

    def _mark(self, tok, reads, writes):
        key = tok[1] if tok[0] == "eng" else ("d", tok[1])
        for r in reads:
            r.r[key] = tok
        for w in writes:
            w.w = tok
            w.r = {}

    def op(self, E, fn, reads=(), writes=()):
        reads = [_res(r) for r in reads]
        writes = [_res(r) for r in writes]
        self._deps(E, reads, writes)
        idx = self.cnt[E]
        self.cnt[E] += 1
        sem, _ = self._esem(E, idx)
        self.q[E].append(lambda e, fn=fn, sem=sem: fn(e).then_inc(sem, 1))
        self._mark(("eng", E, idx), reads, writes)

    def dma(self, Q, out, in_, reads=(), writes=(), slow=False):
        reads = [_res(r) for r in reads]
        writes = [_res(r) for r in writes]
        self._deps(Q, reads, writes)
        if Q == "pool":
            si = self.dnext_sw
            self.dnext_sw = NDSEM - 8 + (self.dnext_sw - (NDSEM - 8) + 1) % 8
        else:
            si = self.dnext
            self.dnext = (self.dnext + 1) % (NDSEM - 8)
        if self.dval[si] > 0:
            self._wait(Q, ("dma", si, self.dval[si]))
        self.dval[si] += 16
        sem = self.dsems[si]
        kw = {'allow_slow_non_contiguous': True} if slow else {}
        self.q[Q].append(lambda e, sem=sem, out=out, in_=in_, kw=kw: e.dma_start(out=out, in_=in_, **kw).then_inc(sem, 16))
        self._mark(("dma", si, self.dval[si]), reads, writes)

    def idma(self, out, in_, idx_ap, nrows, reads=(), writes=()):
        Q = "pool"
        reads = [_res(r) for r in reads]
        writes = [_res(r) for r in writes]
        self._deps(Q, reads, writes)
        si = self.dnext_sw
        self.dnext_sw = NDSEM - 8 + (self.dnext_sw - (NDSEM - 8) + 1) % 8
        if self.dval[si] > 0:
            self._wait(Q, ("dma", si, self.dval[si]))
        self.dval[si] += 16
        sem = self.dsems[si]
        self.q[Q].append(lambda e, sem=sem: e.indirect_dma_start(
            out=out, out_offset=None, in_=in_, in_offset=bass.IndirectOffsetOnAxis(ap=idx_ap, axis=0),
            ).then_inc(sem, 16))
        self._mark(("dma", si, self.dval[si]), reads, writes)

    def barrier(self):
        for E in self.q:
            for si in range(NDSEM):
                if self.dval[si] > 0:
                    self._wait(E, ("dma", si, self.dval[si]))
            for F in self.cnt:
                if self.cnt[F] > 0 and F != E:
                    self._wait(E, ("eng", F, self.cnt[F] - 1))

    def emit(self):
        nc = self.nc
        q = self.q
        with nc.Block() as block:
            @block.sync
            def _(e):
                for f in q["sp"]:
                    f(e)

            @block.tensor
            def _(e):
                for f in q["pe"]:
                    f(e)

            @block.scalar
            def _(e):
                for f in q["act"]:
                    f(e)

            @block.vector
            def _(e):
                for f in q["dve"]:
                    f(e)

            @block.gpsimd
            def _(e):
                for f in q["pool"]:
                    f(e)
        self.q = {e: [] for e in q}


class T:
    def __init__(self, P, es, name, shape, dtype, psum=False):
        ctx = P.nc.psum_tensor(name, shape, dtype) if psum else P.nc.sbuf_tensor(name, shape, dtype)
        self.t = es.enter_context(ctx)
        self.res = Res(name, excl=psum)
        self.shape = shape

    def __getitem__(self, k):
        return self.t[k]


class DR:
    def __init__(self, ap, name):
        self.ap = ap
        self.res = Res(name)

    def __getitem__(self, k):
        return self.ap[k]


def host_consts():
    c = {}
    c["ident"] = np.eye(128, dtype=np.float32).astype(ml_dtypes.bfloat16)
    s = np.arange(128)[:, None]
    t = np.arange(128)[None, :]
    lt = ((s // 64 == t // 64) & (s <= t)).astype(np.float32)
    c["lt64"] = lt
    c["mask64"] = np.tile(lt, (1, 8)).astype(ml_dtypes.bfloat16)
    ki = np.arange(128)[:, None]
    cc = np.arange(STRIP)[None, :]
    delta = cc - ki - 384
    mult = ((delta >= 0) & (delta <= 128)).astype(np.float32) \
        + ((delta >= 0) & (delta <= 512) & (delta % 4 == 0)) \
        + ((delta >= 0) & (delta <= 2048) & (delta % 16 == 0))
    c["dstrip"] = np.where(mult > 0, delta, 1.0e6).astype(np.float32)
    c["mstrip"] = mult.astype(ml_dtypes.bfloat16)
    c["ones_f"] = np.ones((128, 1), np.float32)
    ncol = (SEQ // 128 + 3) * 128
    cc2 = np.arange(ncol)[None, :]
    d2 = cc2 - ki - 384
    c["dstrip2"] = np.where(d2 >= 0, d2, 1.0e6).astype(np.float32)
    c["idx1"] = (np.arange(128, dtype=np.float32) + 1.0)[:, None]
    c["nidx1"] = -c["idx1"]
    c["lt128"] = (s <= t).astype(np.float32)
    sel = np.zeros((128, 128), np.float32)
    sel[127, :] = 1.0
    c["sel127"] = sel
    gm = np.zeros((128, 8), np.float32)
    gm[np.arange(128), np.arange(128) // 16] = 1.0
    c["gmask"] = gm
    c["ident_f"] = np.eye(128, dtype=np.float32)
    lt8 = ((s // 8 == t // 8) & (s <= t)).astype(np.float32)
    c["lt8"] = lt8
    c["mask8"] = np.tile(lt8, (1, 8)).astype(ml_dtypes.bfloat16)
    sm = np.zeros((128, 16), np.float32)
    sm[np.arange(128), np.arange(128) // 8] = 1.0
    c["seqmask"] = sm
    c["seqmask_b"] = sm.astype(ml_dtypes.bfloat16)
    c["seqselT"] = np.ascontiguousarray(sm.T)
    c["idx8"] = ((np.arange(128) % 8).astype(np.float32) + 1.0)[:, None]
    c["nidx8"] = -c["idx8"]
    s8 = np.zeros((128, 16), np.float32)
    s8[np.arange(16) * 8 + 7, np.arange(16)] = 1.0
    c["sel8"] = s8

    def mult_of(d):
        return ((d >= 0) & (d <= 128)).astype(np.float64) + ((d >= 0) & (d <= 512) & (d % 4 == 0)) \
            + ((d >= 0) & (d <= 2048) & (d % 16 == 0))
    ba = np.full((128, 33, 8, 8), -30000.0, np.float64)
    kk_ = np.arange(128)[:, None, None]
    qi = np.arange(8)[None, None, :]
    sl = np.array(A_SLOPES)[None, :, None]
    for kt in range(16):
        d = 2048 + qi - (128 * kt + kk_)
        m = mult_of(d)
        val = -sl * d + np.log(np.maximum(m, 1e-30))
        ba[:, kt] = np.where(m > 0, val, -30000.0)
    for sq_ in range(16):
        d = qi - (kk_ % 8)
        m = mult_of(d) * ((kk_ // 8) == sq_)
        val = -sl * d + np.log(np.maximum(m, 1e-30))
        ba[:, 16 + sq_] = np.where(m > 0, val, -30000.0)
    c["bias_a"] = ba.reshape(128, 33 * 64).astype(np.float32)
    bc = np.full((128, 33, 4, 2, 8), -30000.0, np.float64)
    slc = np.array([2.0 ** (-2.0 * (h + 1)) for h in range(4)])[None, :, None, None]
    kq = np.arange(128)[:, None, None, None]
    qq = np.arange(8)[None, None, None, :]
    for j in range(16):
        bc[:, j] = -slc * (2048 + qq - (128 * j + kq)) + np.zeros((1, 1, 2, 1))
    for sq_ in range(16):
        d = qq - (kq % 8)
        ok = (d >= 0) & ((kq // 8) == sq_)
        bc[:, 16 + sq_] = np.where(ok, -slc * d, -30000.0) + np.zeros((1, 1, 2, 1))
    c["bias_c"] = bc.reshape(128, 33 * 64).astype(np.float32)
    c["kidx"] = np.arange(128, dtype=np.float32)[:, None]
    return c


def build_program():
    nc = bass.Bass("TRN2", target_bir_lowering=False)
    es = contextlib.ExitStack()
    hc = host_consts()

    def din(name, shape, dt=F32):
        return nc.dram_tensor(name, list(shape), dt, kind="ExternalInput").ap()

    def dout(name, shape, dt=F32):
        return DR(nc.dram_tensor(name, list(shape), dt, kind="ExternalOutput").ap(), name)

    def dscr(name, shape, dt=F32):
        return DR(nc.dram_tensor(name, list(shape), dt).ap(), name)

    x_seq = din("x_seq", [SEQ, D])
    x_smp = din("x_smp", [128, D])
    norm_mix = din("norm_mix", [2, D])
    norm_ffn = din("norm_ffn", [2, D])
    w_in_even = din("w_in_even", [D, EVEN_IN])
    w_out_even = din("w_out_even", [D, D])
    hgrn_lb = din("hgrn_lb", [3, 512])
    hgrn_gnorm = din("hgrn_gnorm", [1, 64])
    ffn_wg = din("ffn_w_gate", [D, DFF])
    ffn_wu = din("ffn_w_up", [D, DFF])
    ffn_wd = din("ffn_w_down", [DFF, D])
    w_in_odd = din("w_in_odd", [D, 2048])
    w_out_odd = din("w_out_odd", [D, D])
    norm_final = din("norm_final", [1, D])
    dlq1 = din("diff_lq1", [1, 64]); dlk1 = din("diff_lk1", [1, 64])
    dlq2 = din("diff_lq2", [1, 64]); dlk2 = din("diff_lk2", [1, 64])
    dsubln = din("diff_subln", [1, 128])
    s5_a_re = din("s5_a_re", [32, 64]); s5_a_im = din("s5_a_im", [32, 64])
    s5_log_dt = din("s5_log_dt", [1, 32])
    s5_b_re = din("s5_b_re", [32, 64, 16]); s5_b_im = din("s5_b_im", [32, 64, 16])
    s5_c_re = din("s5_c_re", [512, 64]); s5_c_im = din("s5_c_im", [512, 64])
    s5_d = din("s5_d", [1, 512])
    s5_w_glu = din("s5_w_glu", [512, 512]); s5_b_glu = din("s5_b_glu", [1, 512])
    moe_rw = din("moe_router_w", [D, 8]); moe_rb = din("moe_router_b", [1, 8])
    moe_wg = din("moe_w_gate", [8, D, DFF]); moe_wu = din("moe_w_up", [8, D, DFF])
    moe_wd = din("moe_w_down", [8, DFF, D])
    cache_a_k = din("cache_a_k", [16, 2048, 512]); cache_a_v = din("cache_a_v", [16, 2048, 512])
    state_hg = din("state_hgrn", [16, 8, 64, 64])
    pool_k = din("pool_k", [2560 * 128, 512]); pool_v = din("pool_v", [2560 * 128, 512])
    ptab = din("page_table", [1, 256], I32)
    own_rows = din("own_rows", [128, NT // 2], I32)
    st_s5r = din("state_s5_re", [16, 32, 64]); st_s5i = din("state_s5_im", [16, 32, 64])
    cst = {k: din("c_" + k, v.shape, BF16 if v.dtype == ml_dtypes.bfloat16 else F32) for k, v in hc.items()}

    o_ak = dout("o_ak", [NKEEP * 128, 512])
    o_av = dout("o_av", [NKEEP * 128, 512])
    o_hg = dout("o_hg", [8, 64, 64])
    o_ck = dout("o_ck", [SEQ, 512])
    o_cv = dout("o_cv", [SEQ, 512])
    o_s5r = dout("o_s5r", [32, 64])
    o_s5i = dout("o_s5i", [32, 64])
    o_hg_s = dout("o_hg_s", [16, 8, 64, 64])
    o_s5r_s = dout("o_s5r_s", [16, 32, 64])
    o_s5i_s = dout("o_s5i_s", [16, 32, 64])
    o_y_s = dout("o_y_s", [128, D])
    o_yo = dout("o_yo", [SEQ // 2, D])
    o_ck_s = dout("o_ck_s", [128, 512])
    o_cv_s = dout("o_cv_s", [128, 512])
    o_ak_s = dout("o_ak_s", [128, 512])
    o_av_s = dout("o_av_s", [128, 512])

    qT_d = dscr("qT_d", [4, 128, SEQ + 128], BF16)
    kT_d = dscr("kT_d", [4, 128, SEQ + 128], BF16)
    Vp_d = dscr("Vp_d", [NT + 1, 128, 528], BF16)
    hgpre_d = dscr("hgpre_d", [128, 2048], F32)
    mixed_d = dscr("mixed_d", [SEQ + 128, D], BF16)
    x1_d = dscr("x1_d", [SEQ + 128, D], F32)
    x2_d = dscr("x2_d", [SEQ + 128, D], F32)
    x3_d = dscr("x3_d", [SEQ + 128, D], F32)
    ya_d = dscr("ya_d", [SEQ + 128, D], F32)
    yb_d = dscr("yb_d", [SEQ + 128, D], F32)
    cw_d = dscr("cw_d", [SEQ + 128, 8], F32)
    x3o_d = dscr("x3o_d", [SEQ // 2 + 128, D], F32)
    cwo_d = dscr("cwo_d", [SEQ // 2 + 128, 8], F32)
    q2T_d = dscr("q2T_d", [4, 128, SEQ + 128], BF16)
    k2T_d = dscr("k2T_d", [4, 128, SEQ + 128], BF16)
    uT_d = dscr("uT_d", [4, 128, SEQ + 128], BF16)
    Vc_d = dscr("Vc_d", [NT + 1, 128, 520], BF16)
    ud_d = dscr("ud_d", [SEQ + 128, 512], F32)
    mixed2_d = dscr("mixed2_d", [SEQ + 128, D], BF16)

    try:
        _build_body(nc, es, hc, locals())
    except _Stop:
        pass
    return nc


def _build_body(nc, es, hc, L):
    globals().update({k: v for k, v in L.items() if k not in ("nc", "es", "hc")})
    with es:
        P = Prog(nc, es)

        def op(E, fn, r=(), w=()):
            P.op(E, fn, r, w)

        ident = T(P, es, "ident", [128, 128], BF16)
        P.dma("sp", ident[:], cst["ident"], writes=[ident])

        def dbg_idma(tag):
            if os.environ.get("KDBG", "") != tag:
                return
            dd = contextlib.ExitStack()
            with dd:
                t_ = T(P, dd, "dbg_t" + tag, [128, 512], F32)
                i_ = T(P, dd, "dbg_i" + tag, [128, 4], I32)
                op("dve", lambda e: e.memset(i_[:], 0), [], [i_])
                for rep in range(int(os.environ.get("KDBGN", "1"))):
                    P.idma(t_[:], pool_k[:, :], i_[:, 0:1], 2560 * 128, reads=[i_], writes=[t_])
                    if rep % 30 == 29:
                        P.emit()
                P.barrier()
                P.emit()
            print("DBG idma ok at", tag)
            raise _Stop()

        wstg = [T(P, es, f"wstg{i}", [128, 512], F32) for i in range(3)]
        wctr = [0]

        def load_w(dst, src_view, nk):
            ncols = dst.shape[2]
            for kc in range(nk):
                for c0 in range(0, ncols, 512):
                    w = min(512, ncols - c0)
                    i = wctr[0] % 3
                    wctr[0] += 1
                    st = wstg[i]
                    P.dma("sp", st[:, 0:w], src_view[:, kc, c0:c0 + w], writes=[st])
                    eng = ("pool", "act", "dve")[i]
                    if eng == "act":
                        op(eng, lambda e, st=st, kc=kc, c0=c0, w=w: e.copy(out=dst[:, kc, c0:c0 + w], in_=st[:, 0:w]),
                           [st], [dst])
                    else:
                        op(eng, lambda e, st=st, kc=kc, c0=c0, w=w: e.tensor_copy(out=dst[:, kc, c0:c0 + w],
                                                                                 in_=st[:, 0:w]), [st], [dst])

        def rmsnorm_T(x_ap, x_res, g_t, hn_, sq_, ss_, rstd_, pt_, hnT_ap, hnT_res):
            op("act", lambda e: e.activation(out=sq_[:], in_=x_ap, func=AF.Square, accum_out=ss_[:]),
               [x_res], [sq_, ss_])
            op("dve", lambda e: e.tensor_scalar(out=rstd_[:], in0=ss_[:], scalar1=1.0 / D, scalar2=1e-6,
                                                op0=ALU.mult, op1=ALU.add), [ss_], [rstd_])
            op("act", lambda e: e.sqrt(out=rstd_[:], in_=rstd_[:]), [rstd_], [rstd_])
            op("dve", lambda e: e.reciprocal(out=rstd_[:], in_=rstd_[:]), [rstd_], [rstd_])
            op("dve", lambda e: e.scalar_tensor_tensor(out=hn_[:], in0=x_ap, scalar=rstd_[:, 0:1], in1=g_t[:],
                                                       op0=ALU.mult, op1=ALU.mult), [x_res, rstd_, g_t], [hn_])
            for c in range(8):
                op("pe", lambda e, c=c: e.transpose(out=pt_[:, c * 128:(c + 1) * 128],
                                                    in_=hn_[:, c * 128:(c + 1) * 128], identity=ident[:]),
                   [hn_, ident], [pt_])
            op("act", lambda e: e.copy(out=hnT_ap, in_=pt_[:].rearrange("p (a b) -> p a b", a=8)),
               [pt_], [hnT_res])

        dbg_idma("start")
        pes = contextlib.ExitStack()
        with pes:

            a_es = contextlib.ExitStack()
            with a_es:
                def TA(name, shape, dt=F32, psum=False):
                    return T(P, a_es, name, shape, dt, psum)
                w_in = TA("w_in", [128, 8, EVEN_IN], BF16)
                load_w(w_in, w_in_even.rearrange("(kc kp) n -> kp kc n", kp=128), 8)
                gmix0 = TA("gmix0", [128, D])
                P.dma("sp", gmix0[:], norm_mix[0:1, :].partition_broadcast(128), writes=[gmix0])
                lt64 = TA("lt64", [128, 128])
                P.dma("sp", lt64[:], cst["lt64"], writes=[lt64])
                mask64 = TA("mask64", [128, 1024], BF16)
                P.dma("sp", mask64[:], cst["mask64"], writes=[mask64])
                ones_f = TA("ones_f", [128, 1])
                P.dma("sp", ones_f[:], cst["ones_f"], writes=[ones_f])
                gn_t = TA("gn_t", [128, 8, 64])
                for h in range(8):
                    P.dma("sp", gn_t[:, h, :], hgrn_gnorm[0:1, :].partition_broadcast(128), writes=[gn_t])
                lb3 = TA("lb3", [128, 3, 512])
                for r in range(3):
                    P.dma("sp", lb3[:, r, :], hgrn_lb[r:r + 1, :].partition_broadcast(128), writes=[lb3])
                lb_t = TA("lb_t", [128, 512])
                oml_t = TA("oml_t", [128, 512])
                lbs = TA("lbs", [128, 512])
                op("act", lambda e: e.activation(out=lb3[:], in_=lb3[:], func=AF.Exp), [lb3], [lb3])
                op("dve", lambda e: e.tensor_tensor(out=lbs[:], in0=lb3[:, 0, :], in1=lb3[:, 1, :], op=ALU.add),
                   [lb3], [lbs])
                op("dve", lambda e: e.tensor_tensor(out=lbs[:], in0=lbs[:], in1=lb3[:, 2, :], op=ALU.add),
                   [lb3, lbs], [lbs])
                op("dve", lambda e: e.reciprocal(out=lbs[:], in_=lbs[:]), [lbs], [lbs])
                op("dve", lambda e: e.tensor_tensor(out=lb_t[:], in0=lb3[:, 0, :], in1=lbs[:], op=ALU.mult),
                   [lb3, lbs], [lb_t])
                op("dve", lambda e: e.tensor_scalar(out=oml_t[:], in0=lb_t[:], scalar1=-1.0, scalar2=1.0,
                                                    op0=ALU.mult, op1=ALU.add), [lb_t], [oml_t])

                xt = [TA(f"xt{i}", [128, D]) for i in range(2)]
                hn = [TA(f"hn{i}", [128, D], BF16) for i in range(2)]
                hnT = [TA(f"hnT{i}", [128, 8, 128], BF16) for i in range(2)]
                sq = TA("sq", [128, D], BF16)
                ss = [TA(f"ss{i}", [128, 1]) for i in range(2)]
                rstd = [TA(f"rstd{i}", [128, 1]) for i in range(2)]
                kv_sb = [TA(f"kv_sb{i}", [128, 1024]) for i in range(2)]
                qT_sb = [TA(f"qT_sb{i}", [128, 4, 128], BF16) for i in range(2)]
                kT_sb = [TA(f"kT_sb{i}", [128, 4, 128], BF16) for i in range(2)]
                vp_sb = [TA(f"vp_sb{i}", [128, 8, 66], BF16) for i in range(2)]
                for i in range(2):
                    op("dve", lambda e, i=i: e.memset(vp_sb[i][:], 1.0), [], [vp_sb[i]])
                ps_tr = [TA(f"ps_tr{i}", [128, 1024], BF16, psum=True) for i in range(2)]
                ps_mm = [TA(f"ps_mm{i}", [128, 512], F32, psum=True) for i in range(4)]
                ps_w = [TA(f"ps_w{i}", [128, 1024], F32, psum=True) for i in range(1)]
                sg = TA("sg", [128, 512])
                fgate = TA("fgate", [128, 512])
                glog = TA("glog", [128, 512])
                ecum = TA("ecum", [128, 512])
                encum = TA("encum", [128, 512])
                kk = TA("kk", [128, 512])
                ke = TA("ke", [128, 512], BF16)
                qh = TA("qh", [128, 512])
                qe = TA("qe", [128, 512], BF16)
                v_bf = TA("v_bf", [128, 512], BF16)
                gsil = TA("gsil", [128, 512])
                qeT = TA("qeT", [64, 8, 128], BF16)
                qeT0 = TA("qeT0", [64, 8, 128], BF16)
                qeT1 = TA("qeT1", [64, 8, 128], BF16)
                keT = TA("keT", [64, 8, 128], BF16)
                attT = TA("attT", [128, 1024], BF16)
                elast = TA("elast", [64, 16])
                S_f = TA("S_f", [64, 8, 64])
                S_tmp = TA("S_tmp", [64, 8, 64])
                S_bf = [TA(f"S_bf{i}", [64, 8, 64], BF16) for i in range(2)]
                o_sb = TA("o_sb", [128, 512])
                o_sq = TA("o_sq", [128, 512])
                ssq = TA("ssq", [128, 8])
                ob_bf = TA("ob_bf", [128, 512], BF16)

                op("dve", lambda e: e.memset(S_f[:], 0.0), [], [S_f])
                op("dve", lambda e: e.memset(S_bf[0][:], 0.0), [], [S_bf[0]])
                op("dve", lambda e: e.memset(qeT0[:], 0.0), [], [qeT0])
                op("dve", lambda e: e.memset(qeT1[:], 0.0), [], [qeT1])

                def proj_tok(hT, col0, ps):
                    for kc in range(8):
                        op("pe", lambda e, kc=kc: e.matmul(ps[:, 0:512], lhsT=hT[:, kc, :],
                                                           rhs=w_in[:, kc, col0:col0 + 512],
                                                           start=(kc == 0), stop=(kc == 7)), [hT, w_in], [ps])

                tiles = [(x_seq[tt * 128:(tt + 1) * 128, :], tt) for tt in range(NT)] + [(x_smp, NT)]
                tiles = tiles[:int(os.environ.get("KNT", "33"))]
                for it, (x_ap, tt) in enumerate(tiles):
                    b = it % 2
                    smp = tt == NT
                    P.dma("sp", xt[b][:], x_ap, writes=[xt[b]])
                    rmsnorm_T(xt[b][:], xt[b], gmix0, hn[b], sq, ss[b], rstd[b], ps_tr[b], hnT[b][:], hnT[b])
                    hT = hnT[b]
                    kb = kv_sb[b]
                    if smp or tt >= NT - NKEEP:
                        ps = ps_mm[0]
                        proj_tok(hT, 512, ps)
                        op("dve", lambda e, ps=ps, kb=kb: e.tensor_copy(out=kb[:, 0:512], in_=ps[:]), [ps], [kb])
                    ps = ps_mm[1]
                    proj_tok(hT, 1024, ps)
                    if smp or tt >= NT - NKEEP:
                        op("act", lambda e, ps=ps, kb=kb: e.copy(out=kb[:, 512:1024], in_=ps[:]), [ps], [kb])
                        if smp:
                            P.dma("sp", o_ak_s[:], kb[:, 0:512], reads=[kb], writes=[o_ak_s])
                            P.dma("sp", o_av_s[:], kb[:, 512:1024], reads=[kb], writes=[o_av_s])
                        else:
                            r0 = (tt - (NT - NKEEP)) * 128
                            P.dma("sp", o_ak[r0:r0 + 128, :], kb[:, 0:512], reads=[kb], writes=[o_ak])
                            P.dma("sp", o_av[r0:r0 + 128, :], kb[:, 512:1024], reads=[kb], writes=[o_av])
                    vs = vp_sb[b]
                    if os.environ.get("KVP", "1") in ("1", "2"):
                      op("dve", lambda e, ps=ps, vs=vs: e.tensor_copy(
                        out=vs[:, :, 0:64], in_=ps[:].rearrange("p (h d) -> p h d", h=8)), [ps], [vs])
                    if os.environ.get("KVP", "1") in ("1", "3"):
                      P.dma("sp", Vp_d[tt], vs[:].rearrange("p h d -> p (h d)"), reads=[vs], writes=[Vp_d])
                    for which, col0 in (((0, 0), (1, 512)) if os.environ.get("KQK", "1") == "1" else ()):
                        ps = ps_mm[2 + which]
                        for pr in range(4):
                            for kc in range(8):
                                op("pe", lambda e, kc=kc, pr=pr, ps=ps, col0=col0, hT=hT: e.matmul(
                                    ps[:, pr * 128:(pr + 1) * 128],
                                    lhsT=w_in[:, kc, col0 + pr * 128:col0 + (pr + 1) * 128],
                                    rhs=hT[:, kc, :], start=(kc == 0), stop=(kc == 7)), [hT, w_in], [ps])
                        if which == 0:
                            qs = qT_sb[b]
                            op("act", lambda e, ps=ps, qs=qs: e.mul(out=qs[:].rearrange("p a b -> p (a b)"),
                                                                    in_=ps[:], mul=0.125), [ps], [qs])
                            for pr in range(4):
                                P.dma("sp", qT_d[pr, :, tt * 128:(tt + 1) * 128], qs[:, pr, :],
                                      reads=[qs], writes=[qT_d])
                        else:
                            ks = kT_sb[b]
                            op("act", lambda e, ps=ps, ks=ks: e.copy(out=ks[:].rearrange("p a b -> p (a b)"),
                                                                     in_=ps[:]), [ps], [ks])
                            for pr in range(4):
                                P.dma("sp", kT_d[pr, :, tt * 128:(tt + 1) * 128], ks[:, pr, :],
                                      reads=[ks], writes=[kT_d])
                    if os.environ.get("KHG", "1") == "0":
                        continue
                    c0 = 1536
                    if smp:
                        for i4 in range(4):
                            ps = ps_mm[i4]
                            proj_tok(hT, c0 + i4 * 512, ps)
                            hb_ = kv_sb[b]
                            op("act" if i4 % 2 else "dve",
                               (lambda e, ps=ps, hb_=hb_, i4=i4: e.copy(out=hb_[:, (i4 % 2) * 512:(i4 % 2 + 1) * 512],
                                                                        in_=ps[:])) if i4 % 2 else
                               (lambda e, ps=ps, hb_=hb_, i4=i4: e.tensor_copy(
                                   out=hb_[:, (i4 % 2) * 512:(i4 % 2 + 1) * 512], in_=ps[:])), [ps], [hb_])
                            P.dma("sp", hgpre_d[:, i4 * 512:(i4 + 1) * 512],
                                  hb_[:, (i4 % 2) * 512:(i4 % 2 + 1) * 512], reads=[hb_], writes=[hgpre_d])
                        continue
                    ps_q, ps_f = ps_mm[0], ps_mm[1]
                    proj_tok(hT, c0 + 512, ps_f)
                    op("act", lambda e, ps_f=ps_f: e.activation(out=sg[:], in_=ps_f[:], func=AF.Tanh, scale=0.5),
                       [ps_f], [sg])
                    op("dve", lambda e: e.tensor_scalar(out=sg[:], in0=sg[:], scalar1=0.5, scalar2=0.5,
                                                        op0=ALU.mult, op1=ALU.add), [sg], [sg])
                    op("dve", lambda e: e.tensor_tensor(out=fgate[:], in0=sg[:], in1=oml_t[:], op=ALU.mult),
                       [sg, oml_t], [fgate])
                    op("dve", lambda e: e.tensor_tensor(out=fgate[:], in0=fgate[:], in1=lb_t[:], op=ALU.add),
                       [fgate, lb_t], [fgate])
                    op("act", lambda e: e.activation(out=glog[:], in_=fgate[:], func=AF.Ln), [fgate], [glog])
                    op("dve", lambda e: e.tensor_scalar(out=kk[:], in0=fgate[:], scalar1=-1.0, scalar2=1.0,
                                                        op0=ALU.mult, op1=ALU.add), [fgate], [kk])
                    ps_c = ps_mm[2]
                    op("pe", lambda e, ps_c=ps_c: e.matmul(ps_c[:], lhsT=lt64[:], rhs=glog[:], start=True, stop=True),
                       [lt64, glog], [ps_c])
                    op("act", lambda e, ps_c=ps_c: e.activation(out=ecum[:], in_=ps_c[:], func=AF.Exp),
                       [ps_c], [ecum])
                    op("act", lambda e, ps_c=ps_c: e.activation(out=encum[:], in_=ps_c[:], func=AF.Exp, scale=-1.0),
                       [ps_c], [encum])
                    op("dve", lambda e: e.tensor_tensor(out=ke[:], in0=kk[:], in1=encum[:], op=ALU.mult),
                       [kk, encum], [ke])
                    proj_tok(hT, c0, ps_q)
                    op("act", lambda e, ps_q=ps_q: e.activation(out=qh[:], in_=ps_q[:], func=AF.Tanh, scale=0.5),
                       [ps_q], [qh])
                    op("dve", lambda e, ps_q=ps_q: e.scalar_tensor_tensor(out=qh[:], in0=qh[:], scalar=1.0,
                                                                          in1=ps_q[:], op0=ALU.add, op1=ALU.mult),
                       [qh, ps_q], [qh])
                    op("dve", lambda e: e.scalar_tensor_tensor(out=qe[:], in0=qh[:], scalar=0.0625, in1=ecum[:],
                                                               op0=ALU.mult, op1=ALU.mult), [qh, ecum], [qe])
                    ps_i = ps_mm[3]
                    proj_tok(hT, c0 + 1024, ps_i)
                    op("act", lambda e, ps_i=ps_i: e.copy(out=v_bf[:], in_=ps_i[:]), [ps_i], [v_bf])
                    ps_g = ps_mm[2]
                    proj_tok(hT, c0 + 1536, ps_g)
                    op("act", lambda e, ps_g=ps_g: e.activation(out=gsil[:], in_=ps_g[:], func=AF.Tanh, scale=0.5),
                       [ps_g], [gsil])
                    op("dve", lambda e, ps_g=ps_g: e.scalar_tensor_tensor(out=gsil[:], in0=gsil[:], scalar=1.0,
                                                                          in1=ps_g[:], op0=ALU.add, op1=ALU.mult),
                       [gsil, ps_g], [gsil])
                    ps_e = ps_mm[0]
                    for c in range(2):
                        for h in range(8):
                            op("pe", lambda e, c=c, h=h, ps_e=ps_e: e.matmul(
                                ps_e[0:64, c * 8 + h:c * 8 + h + 1],
                                lhsT=glog[c * 64:(c + 1) * 64, h * 64:(h + 1) * 64],
                                rhs=ones_f[c * 64:(c + 1) * 64, 0:1], start=True, stop=True),
                               [glog, ones_f], [ps_e])
                    op("act", lambda e, ps_e=ps_e: e.activation(out=elast[:], in_=ps_e[0:64, 0:16], func=AF.Exp),
                       [ps_e], [elast])
                    for src, dst, pt in ((qe, qeT, ps_tr[0]), (ke, keT, ps_tr[1])):
                        for h in range(8):
                            op("pe", lambda e, h=h, src=src, pt=pt: e.transpose(
                                out=pt[0:64, h * 128:(h + 1) * 128], in_=src[:, h * 64:(h + 1) * 64],
                                identity=ident[:]), [src, ident], [pt])
                        if dst is keT:
                            op("act", lambda e, dst=dst, pt=pt: e.copy(
                                out=dst[:].rearrange("p a b -> p (a b)"), in_=pt[0:64, :]), [pt], [dst])
                        else:
                            op("dve", lambda e, dst=dst, pt=pt: e.tensor_copy(
                                out=dst[:].rearrange("p a b -> p (a b)"), in_=pt[0:64, :]), [pt], [dst])
                    op("dve", lambda e: e.tensor_copy(out=qeT0[:, :, 0:64], in_=qeT[:, :, 0:64]), [qeT], [qeT0])
                    op("dve", lambda e: e.tensor_copy(out=qeT1[:, :, 64:128], in_=qeT[:, :, 64:128]), [qeT], [qeT1])
                    pw = ps_w[0]
                    for h in range(8):
                        op("pe", lambda e, h=h, pw=pw: e.matmul(pw[:, h * 128:(h + 1) * 128], lhsT=keT[:, h, :],
                                                                rhs=qeT[:, h, :], start=True, stop=True),
                           [keT, qeT], [pw])
                    op("dve", lambda e, pw=pw: e.tensor_tensor(out=attT[:], in0=pw[:], in1=mask64[:], op=ALU.mult),
                       [pw, mask64], [attT])
                    ps_s = ps_mm[1]
                    for c in range(2):
                        for h in range(8):
                            op("pe", lambda e, c=c, h=h, ps_s=ps_s: e.matmul(
                                ps_s[0:64, h * 64:(h + 1) * 64],
                                lhsT=ke[c * 64:(c + 1) * 64, h * 64:(h + 1) * 64],
                                rhs=v_bf[c * 64:(c + 1) * 64, h * 64:(h + 1) * 64], start=True, stop=True),
                               [ke, v_bf], [ps_s])
                        op("dve", lambda e, ps_s=ps_s: e.tensor_tensor(
                            out=S_tmp[:], in0=S_f[:], in1=ps_s[0:64, :].rearrange("p (h v) -> p h v", h=8),
                            op=ALU.add), [S_f, ps_s], [S_tmp])
                        op("dve", lambda e, c=c: e.tensor_tensor(
                            out=S_f[:], in0=S_tmp[:],
                            in1=elast[:, c * 8:(c + 1) * 8].unsqueeze(2).to_broadcast([64, 8, 64]),
                            op=ALU.mult), [S_tmp, elast], [S_f])
                        if c == 0:
                            op("act", lambda e: e.copy(out=S_bf[1][:], in_=S_f[:]), [S_f], [S_bf[1]])
                    ps_o = ps_mm[3]
                    for h in range(8):
                        osl = ps_o[:, h * 64:(h + 1) * 64]
                        op("pe", lambda e, h=h, osl=osl: e.matmul(osl, lhsT=attT[:, h * 128:(h + 1) * 128],
                                                                  rhs=v_bf[:, h * 64:(h + 1) * 64],
                                                                  start=True, stop=False), [attT, v_bf], [ps_o])
                        op("pe", lambda e, h=h, osl=osl: e.matmul(osl, lhsT=qeT0[:, h, :], rhs=S_bf[0][:, h, :],
                                                                  start=False, stop=False), [qeT0, S_bf[0]], [ps_o])
                        op("pe", lambda e, h=h, osl=osl: e.matmul(osl, lhsT=qeT1[:, h, :], rhs=S_bf[1][:, h, :],
                                                                  start=False, stop=True), [qeT1, S_bf[1]], [ps_o])
                    op("act", lambda e: e.copy(out=S_bf[0][:], in_=S_f[:]), [S_f], [S_bf[0]])
                    op("act", lambda e, ps_o=ps_o: e.copy(out=o_sb[:], in_=ps_o[:]), [ps_o], [o_sb])
                    op("dve", lambda e: e.tensor_tensor(out=o_sq[:], in0=o_sb[:], in1=o_sb[:], op=ALU.mult),
                       [o_sb], [o_sq])
                    op("dve", lambda e: e.reduce_sum(out=ssq[:], in_=o_sq[:].rearrange("p (h v) -> p h v", h=8),
                                                     axis=AX.X), [o_sq], [ssq])
                    op("dve", lambda e: e.tensor_scalar(out=ssq[:], in0=ssq[:], scalar1=1.0 / 64, scalar2=1e-6,
                                                        op0=ALU.mult, op1=ALU.add), [ssq], [ssq])
                    op("act", lambda e: e.sqrt(out=ssq[:], in_=ssq[:]), [ssq], [ssq])
                    op("dve", lambda e: e.reciprocal(out=ssq[:], in_=ssq[:]), [ssq], [ssq])
                    op("dve", lambda e: e.tensor_tensor(
                        out=o_sq[:].rearrange("p (h v) -> p h v", h=8),
                        in0=o_sb[:].rearrange("p (h v) -> p h v", h=8),
                        in1=ssq[:].unsqueeze(2).to_broadcast([128, 8, 64]), op=ALU.mult), [o_sb, ssq], [o_sq])
                    op("dve", lambda e: e.tensor_tensor(out=o_sq[:], in0=o_sq[:],
                                                        in1=gn_t[:].rearrange("p h v -> p (h v)"), op=ALU.mult),
                       [o_sq, gn_t], [o_sq])
                    op("dve", lambda e: e.scalar_tensor_tensor(out=ob_bf[:], in0=o_sq[:], scalar=0.5, in1=gsil[:],
                                                               op0=ALU.mult, op1=ALU.mult), [o_sq, gsil], [ob_bf])
                    P.dma("sp", mixed_d[tt * 128:(tt + 1) * 128, 512:1024], ob_bf[:], reads=[ob_bf],
                          writes=[mixed_d])
                P.dma("sp", o_hg.ap.rearrange("h k v -> k h v"), S_f[:], reads=[S_f], writes=[o_hg])
                P.barrier()
            P.emit()
            if MAXPH < 1:
                raise _Stop()

            dbg_idma("after0A")
            b_es = contextlib.ExitStack()
            with b_es:
                def TB(name, shape, dt=F32, psum=False):
                    return T(P, b_es, name, shape, dt, psum)
                dstrip = TB("dstrip", [128, STRIP])
                mstrip = TB("mstrip", [128, STRIP], BF16)
                P.dma("sp", dstrip[:], cst["dstrip"], writes=[dstrip])
                P.dma("sp", mstrip[:], cst["mstrip"], writes=[mstrip])
                qg = [TB(f"qg{i}", [128, 4, 512], BF16) for i in range(2)]
                kwin = [TB(f"kwin{i}", [128, 4, 2560], BF16) for i in range(2)]
                vwin = [TB(f"vwin{i}", [128, 20, 528], BF16) for i in range(2)]
                tmp = [TB(f"tmp{i}", [128, 512]) for i in range(2)]
                Eb = [TB(f"Eb{i}", [128, 512], BF16) for i in range(2)]
                PTb = [TB(f"PTb{i}", [128, 20, 512], BF16) for i in range(2)]
                rden = TB("rden", [128, 4])
                oa = [TB(f"oa{i}", [128, 4, 512], BF16) for i in range(2)]
                ps_s = [TB(f"ps_s{i}", [128, 512], F32, psum=True) for i in range(3)]
                ps_o = [TB(f"ps_o{i}", [128, 4, 128], F32, psum=True) for i in range(2)]
                blk = 0
                for Q in range(NT // 4):
                    qb_ = qg[Q % 2]
                    for pr in range(4):
                        P.dma("sp", qb_[:, pr, :], qT_d[pr, :, Q * 512:(Q + 1) * 512], reads=[qT_d], writes=[qb_])
                    oab = oa[Q % 2]
                    kts = list(range(max(0, 4 * Q - 16), 4 * Q + 4))
                    kw, vw = kwin[Q % 2], vwin[Q % 2]
                    k0 = kts[0]
                    for pr in range(4):
                        P.dma("sp", kw[:, pr, 0:len(kts) * 128], kT_d[pr, :, k0 * 128:(kts[-1] + 1) * 128],
                              reads=[kT_d], writes=[kw])
                    for kt in kts:
                        P.dma("sp", vw[:, kt - k0, :], Vp_d[kt], reads=[Vp_d], writes=[vw])
                    for h in range(8):
                        pr, r0 = h // 2, (h % 2) * 64
                        po = ps_o[h % 2]
                        valid = {j: [kt for kt in kts if kt <= 4 * Q + j and 4 * Q + j - kt <= 16] for j in range(4)}
                        ptb = PTb[h % 2]
                        for kt in kts:
                            Dk = 4 * Q - kt
                            c0 = (Dk + 3) * 128
                            ps = ps_s[blk % 3]
                            tm, eb = tmp[blk % 2], Eb[blk % 2]
                            blk += 1
                            op("pe", lambda e, ps=ps, kt=kt, pr=pr, r0=r0, qb_=qb_, kw=kw, k0=k0: e.matmul(
                                ps[:], lhsT=kw[r0:r0 + 64, pr, (kt - k0) * 128:(kt - k0 + 1) * 128],
                                rhs=qb_[r0:r0 + 64, pr, :], start=True, stop=True), [kw, qb_], [ps])
                            op("dve", lambda e, ps=ps, tm=tm, c0=c0, h=h: e.scalar_tensor_tensor(
                                out=tm[:], in0=dstrip[:, c0:c0 + 512], scalar=-A_SLOPES[h], in1=ps[:],
                                op0=ALU.mult, op1=ALU.add), [dstrip, ps], [tm])
                            op("act", lambda e, tm=tm, eb=eb: e.activation(out=eb[:], in_=tm[:], func=AF.Exp),
                               [tm], [eb])
                            op("pool", lambda e, eb=eb, ptb=ptb, c0=c0, kt=kt, k0=k0: e.tensor_tensor(
                                out=ptb[:, kt - k0, :], in0=eb[:], in1=mstrip[:, c0:c0 + 512], op=ALU.mult),
                               [eb, mstrip], [ptb])
                        for j in range(4):
                            v = valid[j]
                            for kt in v:
                                op("pe", lambda e, j=j, ptb=ptb, po=po, kt=kt, h=h, v=v, vw=vw, k0=k0: e.matmul(
                                    po[:, j, 0:66], lhsT=ptb[:, kt - k0, j * 128:(j + 1) * 128],
                                    rhs=vw[:, kt - k0, h * 66:(h + 1) * 66],
                                    start=(kt == v[0]), stop=(kt == v[-1])), [ptb, vw], [po])
                        op("dve", lambda e, po=po: e.reciprocal(out=rden[:], in_=po[:, :, 64]), [po], [rden])
                        op("dve", lambda e, po=po, oab=oab, h=h: e.tensor_tensor(
                            out=oab[:, :, h * 64:(h + 1) * 64], in0=po[:, :, 0:64],
                            in1=rden[:].unsqueeze(2).to_broadcast([128, 4, 64]), op=ALU.mult), [po, rden], [oab])
                    for j in range(4):
                        t0 = (4 * Q + j) * 128
                        P.dma("sp", mixed_d[t0:t0 + 128, 0:512], oab[:, j, :], reads=[oab], writes=[mixed_d])
                P.barrier()
            P.emit()
            if MAXPH < 2:
                raise _Stop()

        dbg_idma("after0B")
        if os.environ.get("KSMP", "1") == "1":
          s_es = contextlib.ExitStack()
          with s_es:
            def TS(name, shape, dt=F32, psum=False):
                return T(P, s_es, "s_" + name, shape, dt, psum)
            ident_f = TS("ident_f", [128, 128])
            P.dma("sp", ident_f[:], cst["ident_f"], writes=[ident_f])
            bias_a = TS("bias_a", [128, 33, 64])
            P.dma("sp", bias_a[:].rearrange("p a b -> p (a b)"), cst["bias_a"], writes=[bias_a])
            qTn = TS("qTn", [128, 4, 128], BF16); kTn = TS("kTn", [128, 4, 128], BF16)
            for pr in range(4):
                P.dma("sp", qTn[:, pr, :], qT_d[pr, :, SEQ:SEQ + 128], reads=[qT_d], writes=[qTn])
                P.dma("sp", kTn[:, pr, :], kT_d[pr, :, SEQ:SEQ + 128], reads=[kT_d], writes=[kTn])
            vS = [TS(f"vS{i}", [128, 17, 528], BF16) for i in range(2)]
            for i in range(2):
                op("pool", lambda e, i=i: e.memset(vS[i][:], 1.0), [], [vS[i]])
                P.dma("sp", vS[i][:, 16, :], Vp_d[NT], reads=[Vp_d], writes=[vS[i]])
            kc = [TS(f"kc{i}", [128, 512]) for i in range(2)]
            vcs = [TS(f"vcs{i}", [128, 512]) for i in range(2)]
            kTc = [TS(f"kTc{i}", [128, 4, 128], BF16) for i in range(2)]
            tmpa = [TS(f"tmpa{i}", [128, 64]) for i in range(2)]
            PTs = [TS(f"PTs{i}", [128, 17, 64], BF16) for i in range(2)]
            rdn = TS("rdn", [8, 8])
            oas = [TS(f"oas{i}", [8, 8, 64], BF16) for i in range(2)]
            ps_kt = [TS(f"ps_kt{i}", [128, 512], F32, psum=True) for i in range(2)]
            ps_sc = [TS(f"ps_sc{i}", [128, 64], F32, psum=True) for i in range(2)]
            ps_po = [TS(f"ps_po{i}", [128, 4, 128], F32, psum=True) for i in range(2)]
            it = 0
            for sq_ in range(16):
                vb, ptb = vS[sq_ % 2], PTs[sq_ % 2]
                for kt in range(17):
                    b = it % 2
                    it += 1
                    pss = ps_sc[b]
                    tm = tmpa[b]
                    if kt < 16:
                        P.dma("sp", kc[b][:], cache_a_k[sq_, kt * 128:(kt + 1) * 128, :], writes=[kc[b]])
                        P.dma("sp", vcs[b][:], cache_a_v[sq_, kt * 128:(kt + 1) * 128, :], writes=[vcs[b]])
                        op("pool", lambda e, b=b, vb=vb, kt=kt: e.tensor_copy(
                            out=vb[:, kt, :].rearrange("p (h d) -> p h d", d=66)[:, :, 0:64],
                            in_=vcs[b][:].rearrange("p (h d) -> p h d", d=64)), [vcs[b]], [vb])
                        pk = ps_kt[b]
                        for c4 in range(4):
                            op("pe", lambda e, c4=c4, pk=pk, b=b: e.transpose(
                                out=pk[:, c4 * 128:(c4 + 1) * 128], in_=kc[b][:, c4 * 128:(c4 + 1) * 128],
                                identity=ident_f[:]), [kc[b], ident_f], [pk])
                        kt_ = kTc[b]
                        op("act", lambda e, pk=pk, kt_=kt_: e.copy(out=kt_[:].rearrange("p a b -> p (a b)"),
                                                                   in_=pk[:]), [pk], [kt_])
                        bi = kt
                    else:
                        kt_ = kTn
                        bi = 16 + sq_
                    for h in range(8):
                        pr, r0 = h // 2, (h % 2) * 64
                        op("pe", lambda e, h=h, pr=pr, r0=r0, pss=pss, kt_=kt_, sq_=sq_: e.matmul(
                            pss[:, h * 8:(h + 1) * 8], lhsT=kt_[r0:r0 + 64, pr, :],
                            rhs=qTn[r0:r0 + 64, pr, sq_ * 8:(sq_ + 1) * 8], start=True, stop=True),
                           [kt_, qTn], [pss])
                    op("dve", lambda e, pss=pss, tm=tm, bi=bi: e.tensor_tensor(
                        out=tm[:], in0=pss[:], in1=bias_a[:, bi, :], op=ALU.add), [pss, bias_a], [tm])
                    op("act", lambda e, tm=tm, ptb=ptb, kt=kt: e.activation(out=ptb[:, kt, :], in_=tm[:],
                                                                           func=AF.Exp), [tm], [ptb])
                for h in range(8):
                    po = ps_po[h // 4]
                    for kt in range(17):
                        op("pe", lambda e, h=h, kt=kt, po=po, ptb=ptb, vb=vb: e.matmul(
                            po[0:8, h % 4, 0:66], lhsT=ptb[:, kt, h * 8:(h + 1) * 8],
                            rhs=vb[:, kt, h * 66:(h + 1) * 66], start=(kt == 0), stop=(kt == 16)),
                           [ptb, vb], [po])
                ob_ = oas[sq_ % 2]
                for half in range(2):
                    po = ps_po[half]
                    op("dve", lambda e, po=po, half=half: e.reciprocal(out=rdn[:, half * 4:(half + 1) * 4],
                                                                       in_=po[0:8, :, 64]), [po], [rdn])
                    op("dve", lambda e, po=po, half=half, ob_=ob_: e.tensor_tensor(
                        out=ob_[:, half * 4:(half + 1) * 4, :], in0=po[0:8, :, 0:64],
                        in1=rdn[:, half * 4:(half + 1) * 4].unsqueeze(2).to_broadcast([8, 4, 64]),
                        op=ALU.mult), [po, rdn], [ob_])
                P.dma("sp", mixed_d[SEQ + sq_ * 8:SEQ + sq_ * 8 + 8, 0:512], ob_[:].rearrange("p h d -> p (h d)"),
                      reads=[ob_], writes=[mixed_d])
            P.barrier()
          P.emit()

          h_es = contextlib.ExitStack()
          with h_es:
            def TH(name, shape, dt=F32, psum=False):
                return T(P, h_es, "hs_" + name, shape, dt, psum)
            lt8 = TH("lt8", [128, 128]); mask8 = TH("mask8", [128, 1024], BF16)
            seqm = TH("seqm", [128, 16]); seqmb = TH("seqmb", [128, 16], BF16)
            P.dma("sp", lt8[:], cst["lt8"], writes=[lt8])
            P.dma("sp", mask8[:], cst["mask8"], writes=[mask8])
            P.dma("sp", seqm[:], cst["seqmask"], writes=[seqm])
            P.dma("sp", seqmb[:], cst["seqmask_b"], writes=[seqmb])
            gn_t = TH("gn_t", [128, 8, 64])
            for h in range(8):
                P.dma("sp", gn_t[:, h, :], hgrn_gnorm[0:1, :].partition_broadcast(128), writes=[gn_t])
            lb3 = TH("lb3", [128, 3, 512])
            for r in range(3):
                P.dma("sp", lb3[:, r, :], hgrn_lb[r:r + 1, :].partition_broadcast(128), writes=[lb3])
            lb_t = TH("lb_t", [128, 512]); oml_t = TH("oml_t", [128, 512]); lbs = TH("lbs", [128, 512])
            op("act", lambda e: e.activation(out=lb3[:], in_=lb3[:], func=AF.Exp), [lb3], [lb3])
            op("dve", lambda e: e.tensor_tensor(out=lbs[:], in0=lb3[:, 0, :], in1=lb3[:, 1, :], op=ALU.add),
               [lb3], [lbs])
            op("dve", lambda e: e.tensor_tensor(out=lbs[:], in0=lbs[:], in1=lb3[:, 2, :], op=ALU.add),
               [lb3, lbs], [lbs])
            op("dve", lambda e: e.reciprocal(out=lbs[:], in_=lbs[:]), [lbs], [lbs])
            op("dve", lambda e: e.tensor_tensor(out=lb_t[:], in0=lb3[:, 0, :], in1=lbs[:], op=ALU.mult),
               [lb3, lbs], [lb_t])
            op("dve", lambda e: e.tensor_scalar(out=oml_t[:], in0=lb_t[:], scalar1=-1.0, scalar2=1.0,
                                                op0=ALU.mult, op1=ALU.add), [lb_t], [oml_t])
            pre = TH("pre", [128, 4, 512])
            P.dma("sp", pre[:].rearrange("p a b -> p (a b)"), hgpre_d[:, :], reads=[hgpre_d], writes=[pre])
            S0 = TH("S0", [64, 16, 8, 64]); S0b = TH("S0b", [64, 16, 8, 64], BF16)
            for sq_ in range(16):
                P.dma("sp", S0[:, sq_, :, :], state_hg[sq_].rearrange("h k v -> k h v"), writes=[S0])
            op("act", lambda e: e.copy(out=S0b[:], in_=S0[:]), [S0], [S0b])
            sg = TH("sg", [128, 512]); glog = TH("glog", [128, 512]); ecum = TH("ecum", [128, 512])
            encum = TH("encum", [128, 512]); kk = TH("kk", [128, 512]); qh = TH("qh", [128, 512])
            gsil = TH("gsil", [128, 512]); o_sb = TH("o_sb", [128, 512]); o_sq = TH("o_sq", [128, 512])
            ke = TH("ke", [128, 512], BF16); qe = TH("qe", [128, 512], BF16); v_bf = TH("v_bf", [128, 512], BF16)
            ob_bf = TH("ob_bf", [128, 512], BF16); ssq = TH("ssq", [128, 8])
            qeT = TH("qeT", [64, 8, 128], BF16); keT = TH("keT", [64, 8, 128], BF16)
            qeTx = TH("qeTx", [64, 8, 2176], BF16)
            attT = TH("attT", [128, 1024], BF16)
            vex = TH("vex", [128, 16, 64], BF16)
            els = TH("els", [64, 8, 16])
            Sn = TH("Sn", [64, 16, 8, 64])
            ps_a = [TH(f"ps_a{i}", [128, 512], F32, psum=True) for i in range(3)]
            ps_t2 = [TH(f"ps_t{i}", [128, 1024], BF16, psum=True) for i in range(2)]
            ps_w2 = TH("ps_w", [128, 1024], F32, psum=True)
            op("dve", lambda e: e.memset(qeTx[:], 0.0), [], [qeTx])
            op("act", lambda e: e.activation(out=sg[:], in_=pre[:, 1, :], func=AF.Tanh, scale=0.5), [pre], [sg])
            op("dve", lambda e: e.tensor_scalar(out=sg[:], in0=sg[:], scalar1=0.5, scalar2=0.5,
                                                op0=ALU.mult, op1=ALU.add), [sg], [sg])
            op("dve", lambda e: e.tensor_tensor(out=sg[:], in0=sg[:], in1=oml_t[:], op=ALU.mult), [sg, oml_t], [sg])
            op("dve", lambda e: e.tensor_tensor(out=sg[:], in0=sg[:], in1=lb_t[:], op=ALU.add), [sg, lb_t], [sg])
            op("act", lambda e: e.activation(out=glog[:], in_=sg[:], func=AF.Ln), [sg], [glog])
            op("dve", lambda e: e.tensor_scalar(out=kk[:], in0=sg[:], scalar1=-1.0, scalar2=1.0,
                                                op0=ALU.mult, op1=ALU.add), [sg], [kk])
            pc = ps_a[0]
            op("pe", lambda e: e.matmul(pc[:], lhsT=lt8[:], rhs=glog[:], start=True, stop=True), [lt8, glog], [pc])
            op("act", lambda e: e.activation(out=ecum[:], in_=pc[:], func=AF.Exp), [pc], [ecum])
            op("act", lambda e: e.activation(out=encum[:], in_=pc[:], func=AF.Exp, scale=-1.0), [pc], [encum])
            op("dve", lambda e: e.tensor_tensor(out=ke[:], in0=kk[:], in1=encum[:], op=ALU.mult), [kk, encum], [ke])
            op("act", lambda e: e.activation(out=qh[:], in_=pre[:, 0, :], func=AF.Tanh, scale=0.5), [pre], [qh])
            op("dve", lambda e: e.scalar_tensor_tensor(out=qh[:], in0=qh[:], scalar=1.0, in1=pre[:, 0, :],
                                                       op0=ALU.add, op1=ALU.mult), [qh, pre], [qh])
            op("dve", lambda e: e.scalar_tensor_tensor(out=qe[:], in0=qh[:], scalar=0.0625, in1=ecum[:],
                                                       op0=ALU.mult, op1=ALU.mult), [qh, ecum], [qe])
            op("act", lambda e: e.copy(out=v_bf[:], in_=pre[:, 2, :]), [pre], [v_bf])
            op("act", lambda e: e.activation(out=gsil[:], in_=pre[:, 3, :], func=AF.Tanh, scale=0.5), [pre], [gsil])
            op("dve", lambda e: e.scalar_tensor_tensor(out=gsil[:], in0=gsil[:], scalar=1.0, in1=pre[:, 3, :],
                                                       op0=ALU.add, op1=ALU.mult), [gsil, pre], [gsil])
            pe_ = ps_a[1]
            for h in range(8):
                op("pe", lambda e, h=h: e.matmul(pe_[0:64, h * 16:(h + 1) * 16], lhsT=glog[:, h * 64:(h + 1) * 64],
                                                 rhs=seqm[:], start=True, stop=True), [glog, seqm], [pe_])
            op("act", lambda e: e.activation(out=els[:].rearrange("p a b -> p (a b)"), in_=pe_[0:64, 0:128],
                                             func=AF.Exp), [pe_], [els])
            for src, dst, pt in ((qe, qeT, ps_t2[0]), (ke, keT, ps_t2[1])):
                for h in range(8):
                    op("pe", lambda e, h=h, src=src, pt=pt: e.transpose(
                        out=pt[0:64, h * 128:(h + 1) * 128], in_=src[:, h * 64:(h + 1) * 64], identity=ident[:]),
                       [src, ident], [pt])
                op("act", lambda e, dst=dst, pt=pt: e.copy(out=dst[:].rearrange("p a b -> p (a b)"),
                                                           in_=pt[0:64, :]), [pt], [dst])
            op("dve", lambda e: e.tensor_copy(
                out=qeTx[:].rearrange("p h (s x) -> p h s x", x=136)[:, :, :, 0:8],
                in_=qeT[:].rearrange("p h (s j) -> p h s j", j=8)), [qeT], [qeTx])
            for h in range(8):
                op("pe", lambda e, h=h: e.matmul(ps_w2[:, h * 128:(h + 1) * 128], lhsT=keT[:, h, :], rhs=qeT[:, h, :],
                                                 start=True, stop=True), [keT, qeT], [ps_w2])
            op("dve", lambda e: e.tensor_tensor(out=attT[:], in0=ps_w2[:], in1=mask8[:], op=ALU.mult),
               [ps_w2, mask8], [attT])
            po_ = ps_a[2]
            for h in range(8):
                osl = po_[:, h * 64:(h + 1) * 64]
                op("pe", lambda e, h=h, osl=osl: e.matmul(osl, lhsT=attT[:, h * 128:(h + 1) * 128],
                                                          rhs=v_bf[:, h * 64:(h + 1) * 64], start=True, stop=False),
                   [attT, v_bf], [po_])
                for sq_ in range(16):
                    op("pe", lambda e, h=h, osl=osl, sq_=sq_: e.matmul(
                        osl, lhsT=qeTx[:, h, sq_ * 128:(sq_ + 1) * 128], rhs=S0b[:, sq_, h, :],
                        start=False, stop=(sq_ == 15)), [qeTx, S0b], [po_])
            for h in range(8):
                op("dve", lambda e, h=h: e.tensor_tensor(
                    out=vex[:], in0=v_bf[:, h * 64:(h + 1) * 64].unsqueeze(1).to_broadcast([128, 16, 64]),
                    in1=seqmb[:].unsqueeze(2).to_broadcast([128, 16, 64]), op=ALU.mult), [v_bf, seqmb], [vex])
                for half in range(2):
                    op("pe", lambda e, h=h, half=half: e.matmul(
                        ps_w2[0:64, half * 512:(half + 1) * 512], lhsT=ke[:, h * 64:(h + 1) * 64],
                        rhs=vex[:, half * 8:(half + 1) * 8, :].rearrange("p s v -> p (s v)"),
                        start=True, stop=True), [ke, vex], [ps_w2])
                op("dve", lambda e, h=h: e.tensor_tensor(
                    out=Sn[:, :, h, :], in0=S0[:, :, h, :],
                    in1=ps_w2[0:64, :].rearrange("p (s v) -> p s v", s=16), op=ALU.add), [S0, ps_w2], [Sn])
                op("dve", lambda e, h=h: e.tensor_tensor(
                    out=Sn[:, :, h, :], in0=Sn[:, :, h, :],
                    in1=els[:, h, :].unsqueeze(2).to_broadcast([64, 16, 64]), op=ALU.mult), [Sn, els], [Sn])
            for sq_ in range(16):
                P.dma("sp", o_hg_s[sq_].rearrange("h k v -> k h v"), Sn[:, sq_, :, :], reads=[Sn], writes=[o_hg_s])
            op("act", lambda e: e.copy(out=o_sb[:], in_=po_[:]), [po_], [o_sb])
            op("dve", lambda e: e.tensor_tensor(out=o_sq[:], in0=o_sb[:], in1=o_sb[:], op=ALU.mult), [o_sb], [o_sq])
            op("dve", lambda e: e.reduce_sum(out=ssq[:], in_=o_sq[:].rearrange("p (h v) -> p h v", h=8), axis=AX.X),
               [o_sq], [ssq])
            op("dve", lambda e: e.tensor_scalar(out=ssq[:], in0=ssq[:], scalar1=1.0 / 64, scalar2=1e-6,
                                                op0=ALU.mult, op1=ALU.add), [ssq], [ssq])
            op("act", lambda e: e.sqrt(out=ssq[:], in_=ssq[:]), [ssq], [ssq])
            op("dve", lambda e: e.reciprocal(out=ssq[:], in_=ssq[:]), [ssq], [ssq])
            op("dve", lambda e: e.tensor_tensor(
                out=o_sq[:].rearrange("p (h v) -> p h v", h=8), in0=o_sb[:].rearrange("p (h v) -> p h v", h=8),
                in1=ssq[:].unsqueeze(2).to_broadcast([128, 8, 64]), op=ALU.mult), [o_sb, ssq], [o_sq])
            op("dve", lambda e: e.tensor_tensor(out=o_sq[:], in0=o_sq[:], in1=gn_t[:].rearrange("p h v -> p (h v)"),
                                                op=ALU.mult), [o_sq, gn_t], [o_sq])
            op("dve", lambda e: e.scalar_tensor_tensor(out=ob_bf[:], in0=o_sq[:], scalar=0.5, in1=gsil[:],
                                                       op0=ALU.mult, op1=ALU.mult), [o_sq, gsil], [ob_bf])
            P.dma("sp", mixed_d[SEQ:SEQ + 128, 512:1024], ob_bf[:], reads=[ob_bf], writes=[mixed_d])
            P.barrier()
          P.emit()

        dbg_idma("after0S")
        def out_proj_phase(tag, w_dram, mixed_src, xres_ap, xres_res, x_dst, ntiles):
            c_es = contextlib.ExitStack()
            with c_es:
                def TC(name, shape, dt=F32, psum=False):
                    return T(P, c_es, tag + name, shape, dt, psum)
                w_out = TC("w_out", [128, 8, D], BF16)
                load_w(w_out, w_dram.rearrange("(kc kp) n -> kp kc n", kp=128), 8)
                mx = [TC(f"mx{i}", [128, D], BF16) for i in range(2)]
                mxT = [TC(f"mxT{i}", [128, 8, 128], BF16) for i in range(2)]
                xr = [TC(f"xr{i}", [128, D]) for i in range(2)]
                x1t = [TC(f"x1t{i}", [128, D]) for i in range(2)]
                ps_tr = [TC(f"ps_tr{i}", [128, 1024], BF16, psum=True) for i in range(2)]
                ps_y = [TC(f"ps_y{i}", [128, 1024], F32, psum=True) for i in range(2)]
                for tt in range(ntiles):
                    b = tt % 2
                    P.dma("sp", mx[b][:], mixed_src[tt * 128:(tt + 1) * 128, :], reads=[mixed_src], writes=[mx[b]])
                    P.dma("sp", xr[b][:], xres_ap[tt * 128:(tt + 1) * 128, :],
                          reads=([xres_res] if xres_res is not None else []), writes=[xr[b]])
                    pt = ps_tr[b]
                    for c in range(8):
                        op("pe", lambda e, c=c, pt=pt, b=b: e.transpose(out=pt[:, c * 128:(c + 1) * 128],
                                                                        in_=mx[b][:, c * 128:(c + 1) * 128],
                                                                        identity=ident[:]), [mx[b], ident], [pt])
                    op("act", lambda e, pt=pt, b=b: e.copy(out=mxT[b][:].rearrange("p a b -> p (a b)"), in_=pt[:]),
                       [pt], [mxT[b]])
                    py = ps_y[b]
                    for half in range(2):
                        for kc in range(8):
                            op("pe", lambda e, kc=kc, half=half, py=py, b=b: e.matmul(
                                py[:, half * 512:(half + 1) * 512], lhsT=mxT[b][:, kc, :],
                                rhs=w_out[:, kc, half * 512:(half + 1) * 512], start=(kc == 0), stop=(kc == 7)),
                               [mxT[b], w_out], [py])
                    op("dve", lambda e, py=py, b=b: e.tensor_tensor(out=x1t[b][:], in0=py[:], in1=xr[b][:],
                                                                   op=ALU.add), [py, xr[b]], [x1t[b]])
                    P.dma("sp", x_dst[tt * 128:(tt + 1) * 128, :], x1t[b][:], reads=[x1t[b]], writes=[x_dst])
                P.barrier()
            P.emit()

        def ffn_phase(tag, wg_ap, wu_ap, wd_ap, g_row_ap, x_src, res_src, x_dst, groups, cw_src=None, ecol=0):
            d_es = contextlib.ExitStack()
            with d_es:
                def TD(name, shape, dt=F32, psum=False):
                    return T(P, d_es, tag + name, shape, dt, psum)
                wg = TD("wg", [128, 8, DFF], BF16)
                wu = TD("wu", [128, 8, DFF], BF16)
                wd = TD("wd", [128, NFF, D], BF16)
                load_w(wg, wg_ap.rearrange("(kc kp) n -> kp kc n", kp=128), 8)
                load_w(wu, wu_ap.rearrange("(kc kp) n -> kp kc n", kp=128), 8)
                load_w(wd, wd_ap.rearrange("(kc kp) n -> kp kc n", kp=128), NFF)
                gffn = TD("gffn", [128, D])
                P.dma("sp", gffn[:], g_row_ap.partition_broadcast(128), writes=[gffn])
                xg = TD("xg", [128, 4, D])
                rbuf = [TD(f"rbuf{i}", [128, 512]) for i in range(2)]
                cwg = TD("cwg", [128, 4, 8])
                hn = [TD(f"hn{i}", [128, D], BF16) for i in range(2)]
                hnTg = TD("hnTg", [128, 8, 512], BF16)
                sq = TD("sq", [128, D], BF16)
                ss = [TD(f"ss{i}", [128, 1]) for i in range(2)]
                rstd = [TD(f"rstd{i}", [128, 1]) for i in range(2)]
                hT = TD("hT", [128, NFF, 512], BF16)
                gs = [TD(f"gs{i}", [128, 512]) for i in range(2)]
                x2t = [TD(f"x2t{i}", [128, 512]) for i in range(2)]
                ps_tr = [TD(f"ps_tr{i}", [128, 1024], BF16, psum=True) for i in range(2)]
                ps_g = [TD(f"ps_g{i}", [128, 512], F32, psum=True) for i in range(2)]
                ps_u = [TD(f"ps_u{i}", [128, 512], F32, psum=True) for i in range(2)]
                ps_d = [TD(f"ps_d{i}", [128, 512], F32, psum=True) for i in range(2)]
                for tiles_g in groups:
                    ncol = len(tiles_g) * 128
                    for j, tt in enumerate(tiles_g):
                        P.dma("sp", xg[:, j, :], x_src[tt * 128:(tt + 1) * 128, :], reads=[x_src], writes=[xg])
                        if cw_src is not None:
                            P.dma("sp", cwg[:, j, :], cw_src[tt * 128:(tt + 1) * 128, :], reads=[cw_src],
                                  writes=[cwg])
                    for j, tt in enumerate(tiles_g):
                        b = j % 2
                        rmsnorm_T(xg[:, j, :], xg, gffn, hn[b], sq, ss[b], rstd[b], ps_tr[b],
                                  hnTg[:, :, j * 128:(j + 1) * 128], hnTg)
                    for fc in range(NFF):
                        pg, pu = ps_g[fc % 2], ps_u[fc % 2]
                        for kc in range(8):
                            op("pe", lambda e, kc=kc, fc=fc, pg=pg, ncol=ncol: e.matmul(
                                pg[:, 0:ncol], lhsT=wg[:, kc, fc * 128:(fc + 1) * 128], rhs=hnTg[:, kc, 0:ncol],
                                start=(kc == 0), stop=(kc == 7)), [wg, hnTg], [pg])
                        for kc in range(8):
                            op("pe", lambda e, kc=kc, fc=fc, pu=pu, ncol=ncol: e.matmul(
                                pu[:, 0:ncol], lhsT=wu[:, kc, fc * 128:(fc + 1) * 128], rhs=hnTg[:, kc, 0:ncol],
                                start=(kc == 0), stop=(kc == 7)), [wu, hnTg], [pu])
                        gsb = gs[fc % 2]
                        op("act", lambda e, pg=pg, gsb=gsb, ncol=ncol: e.activation(out=gsb[:, 0:ncol], in_=pg[:, 0:ncol], func=AF.Tanh,
                                                                         scale=0.5), [pg], [gsb])
                        op("dve", lambda e, pg=pg, gsb=gsb, ncol=ncol: e.scalar_tensor_tensor(
                            out=gsb[:, 0:ncol], in0=gsb[:, 0:ncol], scalar=1.0, in1=pg[:, 0:ncol], op0=ALU.add,
                            op1=ALU.mult),
                           [gsb, pg], [gsb])
                        op("dve", lambda e, pu=pu, gsb=gsb, fc=fc, ncol=ncol: e.scalar_tensor_tensor(
                            out=hT[:, fc, 0:ncol], in0=gsb[:, 0:ncol], scalar=0.5, in1=pu[:, 0:ncol], op0=ALU.mult,
                            op1=ALU.mult),
                           [gsb, pu], [hT])
                    for j, tt in enumerate(tiles_g):
                        for half in range(2):
                            pd = ps_d[(2 * j + half) % 2]
                            for fc in range(NFF):
                                op("pe", lambda e, fc=fc, j=j, half=half, pd=pd: e.matmul(
                                    pd[:], lhsT=hT[:, fc, j * 128:(j + 1) * 128],
                                    rhs=wd[:, fc, half * 512:(half + 1) * 512],
                                    start=(fc == 0), stop=(fc == NFF - 1)), [hT, wd], [pd])
                            xo = x2t[(2 * j + half) % 2]
                            rb_ = rbuf[(2 * j + half) % 2]
                            P.dma("sp", rb_[:], res_src[tt * 128:(tt + 1) * 128, half * 512:(half + 1) * 512],
                                  reads=[res_src], writes=[rb_])
                            if cw_src is None:
                                op("dve", lambda e, pd=pd, xo=xo, rb_=rb_: e.tensor_tensor(
                                    out=xo[:], in0=pd[:], in1=rb_[:], op=ALU.add), [pd, rb_], [xo])
                            else:
                                op("dve", lambda e, pd=pd, xo=xo, j=j, rb_=rb_: e.scalar_tensor_tensor(
                                    out=xo[:], in0=pd[:], scalar=cwg[:, j, ecol:ecol + 1],
                                    in1=rb_[:], op0=ALU.mult, op1=ALU.add), [pd, rb_, cwg], [xo])
                            P.dma("sp", x_dst[tt * 128:(tt + 1) * 128, half * 512:(half + 1) * 512], xo[:],
                                  reads=[xo], writes=[x_dst])
                P.barrier()
            P.emit()

        SMP = os.environ.get("KSMP", "1") == "1"
        TGROUPS = [[4 * g + j for j in range(4)] for g in range(NT // 4)] + ([[NT]] if SMP else [])
        NTS = NT + (1 if SMP else 0)

        class _SubDR:
            def __init__(self, base, r0):
                self.ap = base.ap[r0:, :]
                self.res = base.res

            def __getitem__(self, k):
                return self.ap[k]
        out_proj_phase("c_", w_out_even, mixed_d, x_seq, None, x1_d, NT)
        if os.environ.get("KSMP", "1") == "1":
            out_proj_phase("cs_", w_out_even, DR(mixed_d.ap[SEQ:SEQ + 128, :], "mx_s") if False else _SubDR(mixed_d, SEQ),
                           x_smp, None, _SubDR(x1_d, SEQ), 1)
        if MAXPH < 3:
            raise _Stop()
        ffn_phase("d_", ffn_wg, ffn_wu, ffn_wd, norm_ffn[0:1, :], x1_d, x1_d, x2_d, TGROUPS)
        if MAXPH < 4:
            raise _Stop()

        e_es = contextlib.ExitStack()
        with e_es:
            def TE(name, shape, dt=F32, psum=False):
                return T(P, e_es, name, shape, dt, psum)
            w_io = TE("w_io", [128, 8, 2048], BF16)
            load_w(w_io, w_in_odd.rearrange("(kc kp) n -> kp kc n", kp=128), 8)
            gmix1 = TE("gmix1", [128, D])
            P.dma("sp", gmix1[:], norm_mix[1:2, :].partition_broadcast(128), writes=[gmix1])
            xt = [TE(f"e_xt{i}", [128, D]) for i in range(2)]
            hn = [TE(f"e_hn{i}", [128, D], BF16) for i in range(2)]
            hnT = [TE(f"e_hnT{i}", [128, 8, 128], BF16) for i in range(2)]
            sq = TE("e_sq", [128, D], BF16)
            ss = [TE(f"e_ss{i}", [128, 1]) for i in range(2)]
            rstd = [TE(f"e_rstd{i}", [128, 1]) for i in range(2)]
            kv_sb = [TE(f"e_kv{i}", [128, 1024]) for i in range(2)]
            u_sb = [TE(f"e_u{i}", [128, 512]) for i in range(2)]
            vc_sb = [TE(f"e_vc{i}", [128, 4, 130], BF16) for i in range(2)]
            for i in range(2):
                op("dve", lambda e, i=i: e.memset(vc_sb[i][:], 1.0), [], [vc_sb[i]])
            fT_sb = [TE(f"e_fT{i}", [128, 4, 128], BF16) for i in range(6)]
            ps_tr = [TE(f"e_ps_tr{i}", [128, 1024], BF16, psum=True) for i in range(2)]
            ps_mm = [TE(f"e_ps_mm{i}", [128, 512], F32, psum=True) for i in range(4)]
            for tt in range(NTS):
                b = tt % 2
                P.dma("sp", xt[b][:], x2_d[tt * 128:(tt + 1) * 128, :], reads=[x2_d], writes=[xt[b]])
                rmsnorm_T(xt[b][:], xt[b], gmix1, hn[b], sq, ss[b], rstd[b], ps_tr[b], hnT[b][:], hnT[b])
                kb = kv_sb[b]
                for j, col0 in enumerate((512, 1024, 1536)):
                    ps = ps_mm[j]
                    for kc in range(8):
                        op("pe", lambda e, kc=kc, ps=ps, col0=col0, b=b: e.matmul(
                            ps[:], lhsT=hnT[b][:, kc, :], rhs=w_io[:, kc, col0:col0 + 512],
                            start=(kc == 0), stop=(kc == 7)), [hnT[b], w_io], [ps])
                    if j == 0:
                        op("dve", lambda e, ps=ps, kb=kb: e.tensor_copy(out=kb[:, 0:512], in_=ps[:]), [ps], [kb])
                    elif j == 1:
                        op("act", lambda e, ps=ps, kb=kb: e.copy(out=kb[:, 512:1024], in_=ps[:]), [ps], [kb])
                        vs = vc_sb[b]
                        op("dve", lambda e, ps=ps, vs=vs: e.tensor_copy(
                            out=vs[:, :, 0:128], in_=ps[:].rearrange("p (h d) -> p h d", h=4)), [ps], [vs])
                        P.dma("sp", Vc_d[tt], vs[:].rearrange("p h d -> p (h d)"), reads=[vs], writes=[Vc_d])
                    else:
                        ub = u_sb[b]
                        op("act", lambda e, ps=ps, ub=ub: e.copy(out=ub[:], in_=ps[:]), [ps], [ub])
                        P.dma("sp", ud_d[tt * 128:(tt + 1) * 128, :], ub[:], reads=[ub], writes=[ud_d])
                if tt < NT:
                    P.dma("sp", o_ck[tt * 128:(tt + 1) * 128, :], kb[:, 0:512], reads=[kb], writes=[o_ck])
                    P.dma("sp", o_cv[tt * 128:(tt + 1) * 128, :], kb[:, 512:1024], reads=[kb], writes=[o_cv])
                else:
                    P.dma("sp", o_ck_s[:], kb[:, 0:512], reads=[kb], writes=[o_ck_s])
                    P.dma("sp", o_cv_s[:], kb[:, 512:1024], reads=[kb], writes=[o_cv_s])
                for wi, (col0, dst, scl) in enumerate(((0, q2T_d, 0.125), (512, k2T_d, 1.0), (1536, uT_d, 1.0))):
                    ps = ps_mm[3] if wi != 1 else ps_mm[0]
                    for pr in range(4):
                        for kc in range(8):
                            op("pe", lambda e, kc=kc, pr=pr, ps=ps, col0=col0, b=b: e.matmul(
                                ps[:, pr * 128:(pr + 1) * 128],
                                lhsT=w_io[:, kc, col0 + pr * 128:col0 + (pr + 1) * 128],
                                rhs=hnT[b][:, kc, :], start=(kc == 0), stop=(kc == 7)), [hnT[b], w_io], [ps])
                    fs = fT_sb[(tt % 2) * 3 + wi]
                    op("act", lambda e, ps=ps, fs=fs, scl=scl: e.mul(out=fs[:].rearrange("p a b -> p (a b)"),
                                                                     in_=ps[:], mul=scl), [ps], [fs])
                    for pr in range(4):
                        P.dma("sp", dst[pr, :, tt * 128:(tt + 1) * 128], fs[:, pr, :], reads=[fs], writes=[dst])
            P.barrier()
        P.emit()
        if MAXPH < 5:
            raise _Stop()

        dbg_idma("after1A")
        LAM_INIT = 0.8 - 0.6 * float(np.exp(-0.3 * 1))
        C_SLOPES = [2.0 ** (-2.0 * (h + 1)) for h in range(4)]
        f_es = contextlib.ExitStack()
        with f_es:
            def TF(name, shape, dt=F32, psum=False):
                return T(P, f_es, "f_" + name, shape, dt, psum)
            NC2 = (NT + 3) * 128
            dstrip2 = TF("dstrip2", [128, NC2])
            P.dma("sp", dstrip2[:], cst["dstrip2"], writes=[dstrip2])
            k2 = TF("k2", [128, 4, SEQ], BF16)
            for pr in range(4):
                P.dma("sp", k2[:, pr, :], k2T_d[pr, :, 0:SEQ], reads=[k2T_d], writes=[k2])
            vc = TF("vc", [128, NT, 520], BF16)
            for kt in range(NT):
                P.dma("sp", vc[:, kt, :], Vc_d[kt], reads=[Vc_d], writes=[vc])
            lqk = TF("lqk", [128, 4, 64])
            for i, src in enumerate((dlq1, dlk1, dlq2, dlk2)):
                P.dma("sp", lqk[:, i, :], src[0:1, :].partition_broadcast(128), writes=[lqk])
            lam2 = TF("lam2", [128, 2])
            lprod = TF("lprod", [128, 2, 64])
            op("dve", lambda e: e.tensor_tensor(out=lprod[:, 0, :], in0=lqk[:, 0, :], in1=lqk[:, 1, :], op=ALU.mult),
               [lqk], [lprod])
            op("dve", lambda e: e.tensor_tensor(out=lprod[:, 1, :], in0=lqk[:, 2, :], in1=lqk[:, 3, :], op=ALU.mult),
               [lqk, lprod], [lprod])
            op("dve", lambda e: e.reduce_sum(out=lam2[:], in_=lprod[:], axis=AX.X), [lprod], [lam2])
            op("act", lambda e: e.activation(out=lam2[:], in_=lam2[:], func=AF.Exp), [lam2], [lam2])
            nlam = TF("nlam", [128, 1])
            op("dve", lambda e: e.tensor_tensor(out=nlam[:], in0=lam2[:, 1:2], in1=lam2[:, 0:1], op=ALU.subtract),
               [lam2], [nlam])
            op("dve", lambda e: e.tensor_scalar(out=nlam[:], in0=nlam[:], scalar1=-LAM_INIT, scalar2=None,
                                                op0=ALU.add), [nlam], [nlam])
            subw = TF("subw", [128, 128])
            P.dma("sp", subw[:], dsubln[0:1, :].partition_broadcast(128), writes=[subw])
            op("dve", lambda e: e.tensor_scalar(out=subw[:], in0=subw[:], scalar1=1.0 - LAM_INIT, scalar2=None,
                                                op0=ALU.mult), [subw], [subw])
            qg = [TF(f"qg{i}", [128, 4, 512], BF16) for i in range(2)]
            tmp = [TF(f"tmp{i}", [128, 512]) for i in range(2)]
            PTb = [TF(f"PTb{i}", [128, NT, 512], BF16) for i in range(2)]
            rden = TF("rden", [128, 2, 4])
            o1 = TF("o1", [128, 4, 128])
            o2 = TF("o2", [128, 4, 128])
            osq = TF("osq", [128, 4, 128])
            ssq = TF("ssq", [128, 4])
            oc = [TF(f"oc{i}", [128, 4, 512], BF16) for i in range(2)]
            ps_s = [TF(f"ps_s{i}", [128, 512], F32, psum=True) for i in range(3)]
            ps_o = [[TF(f"ps_o{m}{i}", [128, 2, 256], F32, psum=True) for i in range(2)] for m in range(2)]
            blk = 0
            for Q in range(NT // 4):
                qb_ = qg[Q % 2]
                for pr in range(4):
                    P.dma("sp", qb_[:, pr, :], q2T_d[pr, :, Q * 512:(Q + 1) * 512], reads=[q2T_d], writes=[qb_])
                ocb = oc[Q % 2]
                kts = list(range(0, 4 * Q + 4))
                for h in range(4):
                    for m in range(2):
                        ptb = PTb[m]
                        r0 = m * 64
                        for kt in kts:
                            c0 = (4 * Q - kt + 3) * 128
                            ps = ps_s[blk % 3]
                            tm = tmp[blk % 2]
                            blk += 1
                            op("pe", lambda e, ps=ps, kt=kt, h=h, r0=r0, qb_=qb_: e.matmul(
                                ps[:], lhsT=k2[r0:r0 + 64, h, kt * 128:(kt + 1) * 128],
                                rhs=qb_[r0:r0 + 64, h, :], start=True, stop=True), [k2, qb_], [ps])
                            op("dve", lambda e, ps=ps, tm=tm, c0=c0, h=h: e.scalar_tensor_tensor(
                                out=tm[:], in0=dstrip2[:, c0:c0 + 512], scalar=-C_SLOPES[h], in1=ps[:],
                                op0=ALU.mult, op1=ALU.add), [dstrip2, ps], [tm])
                            op("act", lambda e, tm=tm, ptb=ptb, kt=kt: e.activation(out=ptb[:, kt, :], in_=tm[:],
                                                                                   func=AF.Exp), [tm], [ptb])
                        for j in range(4):
                            po = ps_o[m][j // 2]
                            nk = 4 * Q + j + 1
                            for kt in range(nk):
                                op("pe", lambda e, j=j, ptb=ptb, po=po, kt=kt, h=h, nk=nk: e.matmul(
                                    po[:, j % 2, 0:130], lhsT=ptb[:, kt, j * 128:(j + 1) * 128],
                                    rhs=vc[:, kt, h * 130:(h + 1) * 130],
                                    start=(kt == 0), stop=(kt == nk - 1)), [ptb, vc], [po])
                    for m in range(2):
                        for jj in range(2):
                            po = ps_o[m][jj]
                            op("dve", lambda e, po=po, m=m, jj=jj: e.reciprocal(
                                out=rden[:, m, 2 * jj:2 * jj + 2], in_=po[:, :, 128]), [po], [rden])
                    op("dve", lambda e: e.tensor_scalar(out=rden[:, 1, :], in0=rden[:, 1, :], scalar1=nlam[:, 0:1],
                                                        scalar2=None, op0=ALU.mult), [rden, nlam], [rden])
                    for m, od in ((0, o1), (1, o2)):
                        for jj in range(2):
                            po = ps_o[m][jj]
                            op("dve", lambda e, po=po, m=m, jj=jj, od=od: e.tensor_tensor(
                                out=od[:, 2 * jj:2 * jj + 2, :], in0=po[:, :, 0:128],
                                in1=rden[:, m, 2 * jj:2 * jj + 2].unsqueeze(2).to_broadcast([128, 2, 128]),
                                op=ALU.mult), [po, rden], [od])
                    op("dve", lambda e: e.tensor_tensor(out=o1[:], in0=o1[:], in1=o2[:], op=ALU.add), [o1, o2], [o1])
                    op("dve", lambda e: e.tensor_tensor(out=osq[:], in0=o1[:], in1=o1[:], op=ALU.mult), [o1], [osq])
                    op("dve", lambda e: e.reduce_sum(out=ssq[:], in_=osq[:], axis=AX.X), [osq], [ssq])
                    op("dve", lambda e: e.tensor_scalar(out=ssq[:], in0=ssq[:], scalar1=1.0 / 128, scalar2=1e-6,
                                                        op0=ALU.mult, op1=ALU.add), [ssq], [ssq])
                    op("act", lambda e: e.sqrt(out=ssq[:], in_=ssq[:]), [ssq], [ssq])
                    op("dve", lambda e: e.reciprocal(out=ssq[:], in_=ssq[:]), [ssq], [ssq])
                    op("dve", lambda e: e.tensor_tensor(out=osq[:], in0=o1[:],
                                                        in1=ssq[:].unsqueeze(2).to_broadcast([128, 4, 128]),
                                                        op=ALU.mult), [o1, ssq], [osq])
                    op("dve", lambda e, h=h, ocb=ocb: e.tensor_tensor(
                        out=ocb[:, :, h * 128:(h + 1) * 128], in0=osq[:],
                        in1=subw[:].unsqueeze(1).to_broadcast([128, 4, 128]), op=ALU.mult), [osq, subw], [ocb])
                for j in range(4):
                    t0 = (4 * Q + j) * 128
                    P.dma("sp", mixed2_d[t0:t0 + 128, 0:512], ocb[:, j, :], reads=[ocb], writes=[mixed2_d])
            P.barrier()
        P.emit()
        if MAXPH < 6:
            raise _Stop()

        dbg_idma("after1B")
        if SMP:
          t_es = contextlib.ExitStack()
          with t_es:
            def TT(name, shape, dt=F32, psum=False):
                return T(P, t_es, "t_" + name, shape, dt, psum)
            ident_f = TT("ident_f", [128, 128])
            P.dma("sp", ident_f[:], cst["ident_f"], writes=[ident_f])
            bias_c = TT("bias_c", [128, 33, 64])
            P.dma("sp", bias_c[:].rearrange("p a b -> p (a b)"), cst["bias_c"], writes=[bias_c])
            kidx = TT("kidx", [128, 1])
            P.dma("sp", kidx[:], cst["kidx"], writes=[kidx])
            pti = TT("pti", [128, 256], I32); ptf = TT("ptf", [128, 256]); idxi = TT("idxi", [128, 256], I32)
            P.dma("sp", pti[:], ptab[0:1, :].partition_broadcast(128), writes=[pti])
            op("dve", lambda e: e.tensor_copy(out=ptf[:], in_=pti[:]), [pti], [ptf])
            op("dve", lambda e: e.tensor_scalar(out=ptf[:], in0=ptf[:], scalar1=128.0, scalar2=kidx[:, 0:1],
                                                op0=ALU.mult, op1=ALU.add), [ptf, kidx], [ptf])
            op("dve", lambda e: e.tensor_copy(out=idxi[:], in_=ptf[:]), [ptf], [idxi])
            lqk = TT("lqk", [128, 4, 64])
            for i, src_ in enumerate((dlq1, dlk1, dlq2, dlk2)):
                P.dma("sp", lqk[:, i, :], src_[0:1, :].partition_broadcast(128), writes=[lqk])
            lam2 = TT("lam2", [128, 2]); lprod = TT("lprod", [128, 2, 64]); nlam = TT("nlam", [128, 1])
            op("dve", lambda e: e.tensor_tensor(out=lprod[:, 0, :], in0=lqk[:, 0, :], in1=lqk[:, 1, :], op=ALU.mult),
               [lqk], [lprod])
            op("dve", lambda e: e.tensor_tensor(out=lprod[:, 1, :], in0=lqk[:, 2, :], in1=lqk[:, 3, :], op=ALU.mult),
               [lqk, lprod], [lprod])
            op("dve", lambda e: e.reduce_sum(out=lam2[:], in_=lprod[:], axis=AX.X), [lprod], [lam2])
            op("act", lambda e: e.activation(out=lam2[:], in_=lam2[:], func=AF.Exp), [lam2], [lam2])
            op("dve", lambda e: e.tensor_tensor(out=nlam[:], in0=lam2[:, 1:2], in1=lam2[:, 0:1], op=ALU.subtract),
               [lam2], [nlam])
            op("dve", lambda e: e.tensor_scalar(out=nlam[:], in0=nlam[:], scalar1=-LAM_INIT, scalar2=None,
                                                op0=ALU.add), [nlam], [nlam])
            subw = TT("subw", [128, 128])
            P.dma("sp", subw[:], dsubln[0:1, :].partition_broadcast(128), writes=[subw])
            op("dve", lambda e: e.tensor_scalar(out=subw[:], in0=subw[:], scalar1=1.0 - LAM_INIT, scalar2=None,
                                                op0=ALU.mult), [subw], [subw])
            q2n = TT("q2n", [128, 4, 128], BF16); k2n = TT("k2n", [128, 4, 128], BF16)
            for pr in range(4):
                P.dma("sp", q2n[:, pr, :], q2T_d[pr, :, SEQ:SEQ + 128], reads=[q2T_d], writes=[q2n])
                P.dma("sp", k2n[:, pr, :], k2T_d[pr, :, SEQ:SEQ + 128], reads=[k2T_d], writes=[k2n])
            vC = [TT(f"vC{i}", [128, 17, 520], BF16) for i in range(2)]
            for i in range(2):
                op("dve", lambda e, i=i: e.memset(vC[i][:], 1.0), [], [vC[i]])
                P.dma("sp", vC[i][:, 16, :], Vc_d[NT], reads=[Vc_d], writes=[vC[i]])
            kpg = [TT(f"kpg{i}", [128, 512]) for i in range(2)]
            vpg = [TT(f"vpg{i}", [128, 512]) for i in range(2)]
            k2c = [TT(f"k2c{i}", [128, 4, 128], BF16) for i in range(2)]
            tmpc = [TT(f"tmpc{i}", [128, 64]) for i in range(2)]
            PTc = [TT(f"PTc{i}", [128, 17, 64], BF16) for i in range(2)]
            rdc = TT("rdc", [8, 4, 2]); oc1 = TT("oc1", [8, 4, 128]); oc2 = TT("oc2", [8, 4, 128])
            osq = TT("osq", [8, 4, 128]); ssq = TT("ssq", [8, 4])
            ocs = [TT(f"ocs{i}", [8, 4, 128], BF16) for i in range(2)]
            ps_kt = [TT(f"ps_kt{i}", [128, 512], F32, psum=True) for i in range(2)]
            ps_sc = [TT(f"ps_sc{i}", [128, 64], F32, psum=True) for i in range(2)]
            ps_po = [TT(f"ps_po{i}", [128, 2, 256], F32, psum=True) for i in range(4)]
            it = 0
            for sq_ in range(16):
                vb, ptb = vC[sq_ % 2], PTc[sq_ % 2]
                for j in range(17):
                    b = it % 2
                    it += 1
                    pss, tm = ps_sc[b], tmpc[b]
                    if j < 16:
                        col = sq_ * 16 + j
                        P.idma(kpg[b][:], pool_k[:, :], idxi[:, col:col + 1], 2560 * 128, reads=[idxi],
                               writes=[kpg[b]])
                        P.idma(vpg[b][:], pool_v[:, :], idxi[:, col:col + 1], 2560 * 128, reads=[idxi],
                               writes=[vpg[b]])
                        op("dve", lambda e, b=b, vb=vb, j=j: e.tensor_copy(
                            out=vb[:, j, :].rearrange("p (h d) -> p h d", d=130)[:, :, 0:128],
                            in_=vpg[b][:].rearrange("p (h d) -> p h d", d=128)), [vpg[b]], [vb])
                        pk = ps_kt[b]
                        for c4 in range(4):
                            op("pe", lambda e, c4=c4, pk=pk, b=b: e.transpose(
                                out=pk[:, c4 * 128:(c4 + 1) * 128], in_=kpg[b][:, c4 * 128:(c4 + 1) * 128],
                                identity=ident_f[:]), [kpg[b], ident_f], [pk])
                        kt_ = k2c[b]
                        op("act", lambda e, pk=pk, kt_=kt_: e.copy(out=kt_[:].rearrange("p a b -> p (a b)"),
                                                                   in_=pk[:]), [pk], [kt_])
                        bi = j
                    else:
                        kt_ = k2n
                        bi = 16 + sq_
                    for h in range(4):
                        for m in range(2):
                            g8 = (h * 2 + m) * 8
                            op("pe", lambda e, h=h, m=m, g8=g8, pss=pss, kt_=kt_, sq_=sq_: e.matmul(
                                pss[:, g8:g8 + 8], lhsT=kt_[m * 64:(m + 1) * 64, h, :],
                                rhs=q2n[m * 64:(m + 1) * 64, h, sq_ * 8:(sq_ + 1) * 8], start=True, stop=True),
                               [kt_, q2n], [pss])
                    op("dve", lambda e, pss=pss, tm=tm, bi=bi: e.tensor_tensor(
                        out=tm[:], in0=pss[:], in1=bias_c[:, bi, :], op=ALU.add), [pss, bias_c], [tm])
                    op("act", lambda e, tm=tm, ptb=ptb, j=j: e.activation(out=ptb[:, j, :], in_=tm[:], func=AF.Exp),
                       [tm], [ptb])
                for h in range(4):
                    po = ps_po[h]
                    for m in range(2):
                        g8 = (h * 2 + m) * 8
                        for j in range(17):
                            op("pe", lambda e, h=h, m=m, g8=g8, j=j, po=po, ptb=ptb, vb=vb: e.matmul(
                                po[0:8, m, 0:130], lhsT=ptb[:, j, g8:g8 + 8], rhs=vb[:, j, h * 130:(h + 1) * 130],
                                start=(j == 0), stop=(j == 16)), [ptb, vb], [po])
                for h in range(4):
                    po = ps_po[h]
                    op("dve", lambda e, po=po, h=h: e.reciprocal(out=rdc[:, h, :], in_=po[0:8, :, 128]), [po], [rdc])
                op("dve", lambda e: e.tensor_scalar(out=rdc[:, :, 1], in0=rdc[:, :, 1], scalar1=nlam[0:8, 0:1],
                                                    scalar2=None, op0=ALU.mult), [rdc, nlam], [rdc])
                for h in range(4):
                    po = ps_po[h]
                    op("dve", lambda e, po=po, h=h: e.tensor_scalar(out=oc1[:, h, :], in0=po[0:8, 0, 0:128],
                                                                    scalar1=rdc[:, h, 0:1], scalar2=None,
                                                                    op0=ALU.mult), [po, rdc], [oc1])
                    op("dve", lambda e, po=po, h=h: e.tensor_scalar(out=oc2[:, h, :], in0=po[0:8, 1, 0:128],
                                                                    scalar1=rdc[:, h, 1:2], scalar2=None,
                                                                    op0=ALU.mult), [po, rdc], [oc2])
                op("dve", lambda e: e.tensor_tensor(out=oc1[:], in0=oc1[:], in1=oc2[:], op=ALU.add), [oc1, oc2], [oc1])
                op("dve", lambda e: e.tensor_tensor(out=osq[:], in0=oc1[:], in1=oc1[:], op=ALU.mult), [oc1], [osq])
                op("dve", lambda e: e.reduce_sum(out=ssq[:], in_=osq[:], axis=AX.X), [osq], [ssq])
                op("dve", lambda e: e.tensor_scalar(out=ssq[:], in0=ssq[:], scalar1=1.0 / 128, scalar2=1e-6,
                                                    op0=ALU.mult, op1=ALU.add), [ssq], [ssq])
                op("act", lambda e: e.sqrt(out=ssq[:], in_=ssq[:]), [ssq], [ssq])
                op("dve", lambda e: e.reciprocal(out=ssq[:], in_=ssq[:]), [ssq], [ssq])
                op("dve", lambda e: e.tensor_tensor(out=osq[:], in0=oc1[:],
                                                    in1=ssq[:].unsqueeze(2).to_broadcast([8, 4, 128]), op=ALU.mult),
                   [oc1, ssq], [osq])
                ob_ = ocs[sq_ % 2]
                op("dve", lambda e, ob_=ob_: e.tensor_tensor(
                    out=ob_[:], in0=osq[:], in1=subw[0:8, :].unsqueeze(1).to_broadcast([8, 4, 128]), op=ALU.mult),
                   [osq, subw], [ob_])
                P.dma("sp", mixed2_d[SEQ + sq_ * 8:SEQ + sq_ * 8 + 8, 0:512], ob_[:].rearrange("p h d -> p (h d)"),
                      reads=[ob_], writes=[mixed2_d])
            P.barrier()
          P.emit()

        TWO_PI = 2.0 * np.pi
        g_es = contextlib.ExitStack()
        with g_es:
            def TG(name, shape, dt=F32, psum=False):
                return T(P, g_es, "g_" + name, shape, dt, psum)
            idx1 = TG("idx1", [128, 1]); nidx1 = TG("nidx1", [128, 1])
            P.dma("sp", idx1[:], cst["idx1"], writes=[idx1])
            P.dma("sp", nidx1[:], cst["nidx1"], writes=[nidx1])
            lt128 = TG("lt128", [128, 128]); sel127 = TG("sel127", [128, 128]); gmask = TG("gmask", [128, 8])
            ident_f = TG("ident_f", [128, 128])
            P.dma("sp", lt128[:], cst["lt128"], writes=[lt128])
            P.dma("sp", sel127[:], cst["sel127"], writes=[sel127])
            P.dma("sp", gmask[:], cst["gmask"], writes=[gmask])
            P.dma("sp", ident_f[:], cst["ident_f"], writes=[ident_f])
            are_b = TG("are_b", [128, 32, 64]); aim_b = TG("aim_b", [128, 32, 64]); dt_b = TG("dt_b", [128, 32])
            P.dma("sp", are_b[:].rearrange("p g q -> p (g q)"),
                  s5_a_re.rearrange("g q -> (g q)").partition_broadcast(128), writes=[are_b])
            P.dma("sp", aim_b[:].rearrange("p g q -> p (g q)"),
                  s5_a_im.rearrange("g q -> (g q)").partition_broadcast(128), writes=[aim_b])
            P.dma("sp", dt_b[:], s5_log_dt[0:1, :].partition_broadcast(128), writes=[dt_b])
            op("act", lambda e: e.activation(out=dt_b[:], in_=dt_b[:], func=AF.Exp), [dt_b], [dt_b])
            dtb3 = dt_b[:].unsqueeze(2).to_broadcast([128, 32, 64])
            op("dve", lambda e: e.tensor_tensor(out=are_b[:], in0=are_b[:], in1=dtb3, op=ALU.mult),
               [are_b, dt_b], [are_b])
            op("dve", lambda e: e.tensor_tensor(out=aim_b[:], in0=aim_b[:], in1=dtb3, op=ALU.mult),
               [aim_b, dt_b], [aim_b])
            PNr = TG("PNr", [128, 32, 64]); PNi = TG("PNi", [128, 32, 64])
            PPr = TG("PPr", [128, 32, 64]); PPi = TG("PPi", [128, 32, 64])
            magn = TG("magn", [128, 32, 64]); ang = TG("ang", [128, 32, 64])
            ki32 = TG("ki32", [128, 32, 64], I32)
            kf32 = TG("kf32", [128, 32, 64])
            idx8 = TG("idx8", [128, 1]); nidx8 = TG("nidx8", [128, 1])
            P.dma("sp", idx8[:], cst["idx8"], writes=[idx8])
            P.dma("sp", nidx8[:], cst["nidx8"], writes=[nidx8])

            def sin_of(dst, x_t, shape_tiles):
                ki, kf = shape_tiles
                op("dve", lambda e: e.tensor_scalar(out=ki[:], in0=x_t[:], scalar1=float(1.0 / TWO_PI), scalar2=None,
                                                    op0=ALU.mult), [x_t], [ki])
                op("dve", lambda e: e.tensor_copy(out=kf[:], in_=ki[:]), [ki], [kf])
                op("dve", lambda e: e.scalar_tensor_tensor(out=x_t[:], in0=kf[:], scalar=float(-TWO_PI), in1=x_t[:],
                                                           op0=ALU.mult, op1=ALU.add), [kf, x_t], [x_t])
                op("dve", lambda e: e.tensor_scalar(out=kf[:], in0=x_t[:], scalar1=float(np.pi), scalar2=None,
                                                    op0=ALU.is_gt), [x_t], [kf])
                op("dve", lambda e: e.scalar_tensor_tensor(out=x_t[:], in0=kf[:], scalar=float(-TWO_PI), in1=x_t[:],
                                                           op0=ALU.mult, op1=ALU.add), [kf, x_t], [x_t])
                op("dve", lambda e: e.tensor_scalar(out=kf[:], in0=x_t[:], scalar1=float(-np.pi), scalar2=None,
                                                    op0=ALU.is_lt), [x_t], [kf])
                op("dve", lambda e: e.scalar_tensor_tensor(out=x_t[:], in0=kf[:], scalar=float(TWO_PI), in1=x_t[:],
                                                           op0=ALU.mult, op1=ALU.add), [kf, x_t], [x_t])
                op("act", lambda e: e.activation(out=dst[:], in_=x_t[:], func=AF.Sin), [x_t], [dst])

            def build_tables(idx_t, nidx_t):
                op("act", lambda e: e.activation(out=magn[:], in_=are_b[:], func=AF.Exp, scale=nidx_t[:, 0:1]),
                   [are_b, nidx_t], [magn])
                op("act", lambda e: e.activation(out=PPr[:], in_=are_b[:], func=AF.Exp, scale=idx_t[:, 0:1]),
                   [are_b, idx_t], [PPr])
                for shift, dst in ((0.0, PNi), (0.5 * np.pi, PNr)):
                    op("dve", lambda e, shift=shift: e.tensor_scalar(out=ang[:], in0=aim_b[:], scalar1=idx_t[:, 0:1],
                                                                     scalar2=float(shift), op0=ALU.mult, op1=ALU.add),
                       [aim_b, idx_t], [ang])
                    sin_of(dst, ang, (ki32, kf32))
                op("dve", lambda e: e.tensor_tensor(out=PPi[:], in0=PPr[:], in1=PNi[:], op=ALU.mult), [PPr, PNi], [PPi])
                op("dve", lambda e: e.tensor_tensor(out=PPr[:], in0=PPr[:], in1=PNr[:], op=ALU.mult), [PPr, PNr], [PPr])
                op("dve", lambda e: e.tensor_tensor(out=PNr[:], in0=magn[:], in1=PNr[:], op=ALU.mult), [magn, PNr], [PNr])
                op("dve", lambda e: e.scalar_tensor_tensor(out=PNi[:], in0=magn[:], scalar=-1.0, in1=PNi[:],
                                                           op0=ALU.mult, op1=ALU.mult), [magn, PNi], [PNi])

            build_tables(idx1, nidx1)
            BB = TG("BB", [128, 8, 512], BF16)
            Cm = TG("Cm", [128, 32, 16], BF16)
            pa = contextlib.ExitStack()
            with pa:
                def TP(name, shape, dt=F32, psum=False):
                    return T(P, pa, "gp_" + name, shape, dt, psum)
                ar = TP("ar", [128, 32]); ai = TP("ai", [128, 32]); dtp = TP("dtp", [128, 32])
                bre = TP("bre", [128, 32, 16]); bim = TP("bim", [128, 32, 16])
                for half in range(2):
                    sl = slice(half * 64, (half + 1) * 64)
                    P.dma("sp", ar[sl, :], s5_a_re.rearrange("g q -> q g"), writes=[ar], slow=True)
                    P.dma("sp", ai[sl, :], s5_a_im.rearrange("g q -> q g"), writes=[ai], slow=True)
                    P.dma("sp", bre[sl, :, :], s5_b_re.rearrange("g q c -> q g c"), writes=[bre])
                    P.dma("sp", bim[sl, :, :], s5_b_im.rearrange("g q c -> q g c"), writes=[bim])
                P.dma("sp", dtp[:], s5_log_dt[0:1, :].partition_broadcast(128), writes=[dtp])
                op("act", lambda e: e.activation(out=dtp[:], in_=dtp[:], func=AF.Exp), [dtp], [dtp])
                mg = TP("mg", [128, 32]); th = TP("th", [128, 32]); sn = TP("sn", [128, 32]); cs = TP("cs", [128, 32])
                den = TP("den", [128, 32]); zr = TP("zr", [128, 32]); zi = TP("zi", [128, 32])
                t1 = TP("t1", [128, 32]); t2 = TP("t2", [128, 32])
                op("dve", lambda e: e.tensor_tensor(out=mg[:], in0=ar[:], in1=dtp[:], op=ALU.mult), [ar, dtp], [mg])
                op("act", lambda e: e.activation(out=mg[:], in_=mg[:], func=AF.Exp), [mg], [mg])
                op("dve", lambda e: e.tensor_tensor(out=th[:], in0=ai[:], in1=dtp[:], op=ALU.mult), [ai, dtp], [th])
                ki_s = TP("ki_s", [128, 32], I32)
                kf_s = TP("kf_s", [128, 32])
                for shift, dst in ((0.0, sn), (0.5 * np.pi, cs)):
                    op("dve", lambda e, shift=shift: e.tensor_scalar(out=t1[:], in0=th[:], scalar1=float(shift),
                                                                     scalar2=None, op0=ALU.add), [th], [t1])
                    sin_of(dst, t1, (ki_s, kf_s))
                op("dve", lambda e: e.tensor_tensor(out=cs[:], in0=cs[:], in1=mg[:], op=ALU.mult), [cs, mg], [cs])
                op("dve", lambda e: e.tensor_scalar(out=cs[:], in0=cs[:], scalar1=-1.0, scalar2=None, op0=ALU.add),
                   [cs], [cs])
                op("dve", lambda e: e.tensor_tensor(out=sn[:], in0=sn[:], in1=mg[:], op=ALU.mult), [sn, mg], [sn])
                op("dve", lambda e: e.tensor_tensor(out=den[:], in0=ar[:], in1=ar[:], op=ALU.mult), [ar], [den])
                op("dve", lambda e: e.tensor_tensor(out=t1[:], in0=ai[:], in1=ai[:], op=ALU.mult), [ai], [t1])
                op("dve", lambda e: e.tensor_tensor(out=den[:], in0=den[:], in1=t1[:], op=ALU.add), [den, t1], [den])
                op("dve", lambda e: e.reciprocal(out=den[:], in_=den[:]), [den], [den])
                op("dve", lambda e: e.tensor_tensor(out=t1[:], in0=cs[:], in1=ar[:], op=ALU.mult), [cs, ar], [t1])
                op("dve", lambda e: e.tensor_tensor(out=t2[:], in0=sn[:], in1=ai[:], op=ALU.mult), [sn, ai], [t2])
                op("dve", lambda e: e.tensor_tensor(out=zr[:], in0=t1[:], in1=t2[:], op=ALU.add), [t1, t2], [zr])
                op("dve", lambda e: e.tensor_tensor(out=zr[:], in0=zr[:], in1=den[:], op=ALU.mult), [zr, den], [zr])
                op("dve", lambda e: e.tensor_tensor(out=t1[:], in0=sn[:], in1=ar[:], op=ALU.mult), [sn, ar], [t1])
                op("dve", lambda e: e.tensor_tensor(out=t2[:], in0=cs[:], in1=ai[:], op=ALU.mult), [cs, ai], [t2])
                op("dve", lambda e: e.tensor_tensor(out=zi[:], in0=t1[:], in1=t2[:], op=ALU.subtract), [t1, t2], [zi])
                op("dve", lambda e: e.tensor_tensor(out=zi[:], in0=zi[:], in1=den[:], op=ALU.mult), [zi, den], [zi])
                Mb = TP("Mb", [128, 32, 16]); tb = TP("tb", [128, 32, 16])
                zr3 = zr[:].unsqueeze(2).to_broadcast([128, 32, 16])
                zi3 = zi[:].unsqueeze(2).to_broadcast([128, 32, 16])
                lo, hi = slice(0, 64), slice(64, 128)
                op("dve", lambda e: e.tensor_tensor(out=Mb[lo], in0=bre[lo], in1=zr3[lo], op=ALU.mult),
                   [bre, zr], [Mb])
                op("dve", lambda e: e.tensor_tensor(out=tb[lo], in0=bim[lo], in1=zi3[lo], op=ALU.mult),
                   [bim, zi], [tb])
                op("dve", lambda e: e.tensor_tensor(out=Mb[lo], in0=Mb[lo], in1=tb[lo], op=ALU.subtract),
                   [Mb, tb], [Mb])
                op("dve", lambda e: e.tensor_tensor(out=Mb[hi], in0=bim[hi], in1=zr3[hi], op=ALU.mult),
                   [bim, zr, Mb], [Mb])
                op("dve", lambda e: e.tensor_tensor(out=tb[hi], in0=bre[hi], in1=zi3[hi], op=ALU.mult),
                   [bre, zi, tb], [tb])
                op("dve", lambda e: e.tensor_tensor(out=Mb[hi], in0=Mb[hi], in1=tb[hi], op=ALU.add),
                   [Mb, tb], [Mb])
                MT = TP("MT", [128, 128])
                ps_t = TP("ps_t", [128, 128], F32, psum=True)
                Cst = TP("Cst", [128, 128])
                for ch in range(4):
                    op("pe", lambda e, ch=ch: e.transpose(
                        out=ps_t[:], in_=Mb[:, ch * 8:(ch + 1) * 8, :].rearrange("p g c -> p (g c)"),
                        identity=ident_f[:]), [Mb, ident_f], [ps_t])
                    op("act", lambda e: e.copy(out=MT[:], in_=ps_t[:]), [ps_t], [MT])
                    for gl in range(8):
                        n, j = ch * 2 + gl // 4, gl % 4
                        op("dve", lambda e, n=n, j=j, gl=gl: e.tensor_scalar(
                            out=BB[:, n, j * 128:(j + 1) * 128], in0=MT[:], scalar1=gmask[:, gl:gl + 1],
                            scalar2=None, op0=ALU.mult), [MT, gmask], [BB])
                    P.dma("sp", Cst[:, 0:64], s5_c_re[ch * 128:(ch + 1) * 128, :], writes=[Cst])
                    P.dma("sp", Cst[:, 64:128], s5_c_im[ch * 128:(ch + 1) * 128, :], writes=[Cst])
                    op("pe", lambda e: e.transpose(out=ps_t[:], in_=Cst[:], identity=ident_f[:]),
                       [Cst, ident_f], [ps_t])
                    op("act", lambda e, ch=ch: e.copy(
                        out=Cm[0:64, ch * 8:(ch + 1) * 8, :].rearrange("p g c -> p (g c)"), in_=ps_t[0:64, :]),
                       [ps_t], [Cm])
                    op("act", lambda e, ch=ch: e.mul(
                        out=Cm[64:128, ch * 8:(ch + 1) * 8, :].rearrange("p g c -> p (g c)"), in_=ps_t[64:128, :],
                        mul=-1.0), [ps_t, Cm], [Cm])
                P.barrier()
            wglu = TG("wglu", [128, 4, 512], BF16)
            load_w(wglu, s5_w_glu.rearrange("(kc kp) n -> kp kc n", kp=128), 4)
            bglu = TG("bglu", [128, 512]); dsk = TG("dsk", [128, 512])
            P.dma("sp", bglu[:], s5_b_glu[0:1, :].partition_broadcast(128), writes=[bglu])
            P.dma("sp", dsk[:], s5_d[0:1, :].partition_broadcast(128), writes=[dsk])
            uTt = [TG(f"uTt{i}", [128, 4, 128], BF16) for i in range(2)]
            utok = [TG(f"utok{i}", [128, 512]) for i in range(2)]
            hblk = [TG(f"hblk{n}", [128, 4, 2, 64]) for n in range(8)]
            Wb = [TG(f"Wb{i}", [128, 4, 2, 64]) for i in range(2)]
            tA = [TG(f"tA{i}", [128, 4, 2, 64]) for i in range(2)]
            tB = [TG(f"tB{i}", [128, 4, 2, 64]) for i in range(2)]
            hTb = [TG(f"hTb{i}", [128, 4, 128], BF16) for i in range(2)]
            ysb = TG("ysb", [128, 512]); zsb = TG("zsb", [128, 512]); z2 = TG("z2", [128, 512])
            zbf = TG("zbf", [128, 512], BF16); zT = TG("zT", [128, 4, 128], BF16)
            od_bf = [TG(f"od{i}", [128, 512], BF16) for i in range(2)]
            ps_bu = [TG(f"ps_bu{i}", [128, 4, 2, 64], F32, psum=True) for i in range(2)]
            ps_z = [TG(f"ps_z{i}", [128, 4, 2, 64], F32, psum=True) for i in range(2)]
            ps_hT = [TG(f"ps_hT{i}", [128, 512], F32, psum=True) for i in range(2)]
            ps_y = TG("ps_y", [128, 512], F32, psum=True)
            ps_zt = TG("ps_zt", [128, 1024], BF16, psum=True)

            def cmul(dst, tbl_r, tbl_i, src, n, tA_, tB_, src_res):
                tr = tbl_r[:, 4 * n:4 * n + 4, :].unsqueeze(2).to_broadcast([128, 4, 2, 64])
                ti = tbl_i[:, 4 * n:4 * n + 4, :]
                op("dve", lambda e: e.tensor_tensor(out=tA_[:], in0=src[:], in1=tr, op=ALU.mult),
                   [src_res, tbl_r], [tA_])
                op("dve", lambda e: e.tensor_tensor(out=tB_[:, :, 0, :], in0=src[:, :, 1, :], in1=ti, op=ALU.mult),
                   [src_res, tbl_i], [tB_])
                op("dve", lambda e: e.tensor_tensor(out=tB_[:, :, 1, :], in0=src[:, :, 0, :], in1=ti, op=ALU.mult),
                   [src_res, tbl_i, tB_], [tB_])
                op("dve", lambda e: e.tensor_tensor(out=dst[:, :, 0, :], in0=tA_[:, :, 0, :], in1=tB_[:, :, 0, :],
                                                    op=ALU.subtract), [tA_, tB_], [dst])
                op("dve", lambda e: e.tensor_tensor(out=dst[:, :, 1, :], in0=tA_[:, :, 1, :], in1=tB_[:, :, 1, :],
                                                    op=ALU.add), [tA_, tB_, dst], [dst])

            lt8s = TG("lt8s", [128, 128]); seqselT = TG("seqselT", [16, 128]); sel8 = TG("sel8", [128, 16])
            P.dma("sp", lt8s[:], cst["lt8"], writes=[lt8s])
            P.dma("sp", seqselT[:], cst["seqselT"], writes=[seqselT])
            P.dma("sp", sel8[:], cst["sel8"], writes=[sel8])
            h0 = TG("h0", [16, 32, 2, 64])
            P.dma("sp", h0[:, :, 0, :], st_s5r, writes=[h0])
            P.dma("sp", h0[:, :, 1, :], st_s5i, writes=[h0])
            hfin = TG("hfin", [16, 8, 512])
            for tt in list(range(NT)) + ([NT] if SMP else []):
                b = tt % 2
                smp = tt == NT
                if smp:
                    build_tables(idx8, nidx8)
                ltm = lt8s if smp else lt128
                for ch in range(4):
                    P.dma("sp", uTt[b][:, ch, :], uT_d[ch, :, tt * 128:(tt + 1) * 128], reads=[uT_d], writes=[uTt[b]])
                P.dma("sp", utok[b][:], ud_d[tt * 128:(tt + 1) * 128, :], reads=[ud_d], writes=[utok[b]])
                for n in range(8):
                    pb, pz = ps_bu[n % 2], ps_z[n % 2]
                    wb, ta, tb_ = Wb[n % 2], tA[n % 2], tB[n % 2]
                    op("pe", lambda e, n=n, pb=pb, b=b: e.matmul(
                        pb[:].rearrange("p a r q -> p (a r q)"), lhsT=uTt[b][:, n // 2, :], rhs=BB[:, n, :],
                        start=True, stop=True), [uTt[b], BB], [pb])
                    cmul(wb, PNr, PNi, pb, n, ta, tb_, pb)
                    op("pe", lambda e, pz=pz, wb=wb, tt=tt, ltm=ltm: e.matmul(
                        pz[:].rearrange("p a r q -> p (a r q)"), lhsT=ltm[:],
                        rhs=wb[:].rearrange("p a r q -> p (a r q)"), start=True, stop=(tt == 0)),
                       [ltm, wb], [pz])
                    if smp:
                        op("pe", lambda e, pz=pz, n=n: e.matmul(
                            pz[:].rearrange("p a r q -> p (a r q)"), lhsT=seqselT[:],
                            rhs=h0[:, 4 * n:4 * n + 4, :, :].rearrange("p a r q -> p (a r q)"),
                            start=False, stop=True), [seqselT, h0], [pz])
                    elif tt > 0:
                        op("pe", lambda e, pz=pz, n=n: e.matmul(
                            pz[:].rearrange("p a r q -> p (a r q)"), lhsT=sel127[:],
                            rhs=hblk[n][:].rearrange("p a r q -> p (a r q)"), start=False, stop=True),
                           [sel127, hblk[n]], [pz])
                    cmul(hblk[n], PPr, PPi, pz, n, ta, tb_, pz)
                    if smp:
                        pf = ps_bu[n % 2]
                        op("pe", lambda e, pf=pf, n=n: e.matmul(
                            pf[0:16].rearrange("p a r q -> p (a r q)"), lhsT=sel8[:],
                            rhs=hblk[n][:].rearrange("p a r q -> p (a r q)"), start=True, stop=True),
                           [sel8, hblk[n]], [pf])
                        op("act", lambda e, pf=pf, n=n: e.copy(out=hfin[:, n, :],
                                                               in_=pf[0:16].rearrange("p a r q -> p (a r q)")),
                           [pf], [hfin])
                    ph = ps_hT[n % 2]
                    for j in range(4):
                        op("pe", lambda e, j=j, ph=ph, n=n: e.transpose(
                            out=ph[:, j * 128:(j + 1) * 128],
                            in_=hblk[n][:, j, :, :].rearrange("p r q -> p (r q)"), identity=ident_f[:]),
                           [hblk[n], ident_f], [ph])
                    hb = hTb[n % 2]
                    op("act", lambda e, ph=ph, hb=hb: e.copy(out=hb[:].rearrange("p a b -> p (a b)"), in_=ph[:]),
                       [ph], [hb])
                    for j in range(4):
                        g = 4 * n + j
                        op("pe", lambda e, j=j, g=g, hb=hb: e.matmul(
                            ps_y[:, g * 16:(g + 1) * 16], lhsT=hb[:, j, :], rhs=Cm[:, g, :], start=True, stop=True),
                           [hb, Cm], [ps_y])
                op("dve", lambda e, b=b: e.tensor_tensor(out=ysb[:], in0=utok[b][:], in1=dsk[:], op=ALU.mult),
                   [utok[b], dsk], [ysb])
                op("dve", lambda e: e.tensor_tensor(out=ysb[:], in0=ysb[:], in1=ps_y[:], op=ALU.add),
                   [ysb, ps_y], [ysb])
                op("dve", lambda e: e.tensor_tensor(out=z2[:], in0=ysb[:], in1=ysb[:], op=ALU.mult), [ysb], [z2])
                op("dve", lambda e: e.tensor_scalar(out=z2[:], in0=z2[:], scalar1=0.044715, scalar2=1.0,
                                                    op0=ALU.mult, op1=ALU.add), [z2], [z2])
                op("dve", lambda e: e.tensor_tensor(out=z2[:], in0=z2[:], in1=ysb[:], op=ALU.mult), [z2, ysb], [z2])
                op("act", lambda e: e.activation(out=z2[:], in_=z2[:], func=AF.Tanh, scale=0.7978845608028654),
                   [z2], [z2])
                op("dve", lambda e: e.scalar_tensor_tensor(out=zsb[:], in0=z2[:], scalar=1.0, in1=ysb[:],
                                                           op0=ALU.add, op1=ALU.mult), [z2, ysb], [zsb])
                op("dve", lambda e: e.tensor_scalar(out=zsb[:], in0=zsb[:], scalar1=0.5, scalar2=None,
                                                    op0=ALU.mult), [zsb], [zsb])
                op("act", lambda e: e.copy(out=zbf[:], in_=zsb[:]), [zsb], [zbf])
                for c in range(4):
                    op("pe", lambda e, c=c: e.transpose(out=ps_zt[:, c * 128:(c + 1) * 128],
                                                        in_=zbf[:, c * 128:(c + 1) * 128], identity=ident[:]),
                       [zbf, ident], [ps_zt])
                op("act", lambda e: e.copy(out=zT[:].rearrange("p a b -> p (a b)"), in_=ps_zt[:, 0:512]),
                   [ps_zt], [zT])
                pgl = ps_hT[0]
                for kc in range(4):
                    op("pe", lambda e, kc=kc, pgl=pgl: e.matmul(pgl[:], lhsT=zT[:, kc, :], rhs=wglu[:, kc, :],
                                                                start=(kc == 0), stop=(kc == 3)), [zT, wglu], [pgl])
                op("dve", lambda e, pgl=pgl: e.tensor_tensor(out=z2[:], in0=pgl[:], in1=bglu[:], op=ALU.add),
                   [pgl, bglu], [z2])
                op("act", lambda e: e.activation(out=z2[:], in_=z2[:], func=AF.Tanh, scale=0.5), [z2], [z2])
                op("dve", lambda e: e.scalar_tensor_tensor(out=z2[:], in0=z2[:], scalar=1.0, in1=zsb[:],
                                                           op0=ALU.add, op1=ALU.mult), [z2, zsb], [z2])
                ob = od_bf[b]
                op("dve", lambda e, ob=ob: e.tensor_scalar(out=ob[:], in0=z2[:], scalar1=0.5, scalar2=None,
                                                           op0=ALU.mult), [z2], [ob])
                P.dma("sp", mixed2_d[tt * 128:(tt + 1) * 128, 512:1024], ob[:], reads=[ob], writes=[mixed2_d])
                if tt == NT - 1:
                    for n in range(8):
                        P.dma("sp", o_s5r[4 * n:4 * n + 4, :], hblk[n][127:128, :, 0, :], reads=[hblk[n]],
                              writes=[o_s5r])
                        P.dma("sp", o_s5i[4 * n:4 * n + 4, :], hblk[n][127:128, :, 1, :], reads=[hblk[n]],
                              writes=[o_s5i])
            if SMP:
                hv = hfin[:].rearrange("p n (a r q) -> p n a r q", a=4, r=2)
                for n in range(8):
                    P.dma("sp", o_s5r_s[:, 4 * n:4 * n + 4, :], hv[:, n, :, 0, :], reads=[hfin], writes=[o_s5r_s])
                    P.dma("sp", o_s5i_s[:, 4 * n:4 * n + 4, :], hv[:, n, :, 1, :], reads=[hfin], writes=[o_s5i_s])
            for n in range(0):
                P.dma("sp", o_s5r[4 * n:4 * n + 4, :], hblk[n][127:128, :, 0, :], reads=[hblk[n]], writes=[o_s5r])
                P.dma("sp", o_s5i[4 * n:4 * n + 4, :], hblk[n][127:128, :, 1, :], reads=[hblk[n]], writes=[o_s5i])
            P.barrier()
        P.emit()
        if MAXPH < 7:
            raise _Stop()

        out_proj_phase("h_", w_out_odd, mixed2_d, x2_d.ap, x2_d, x3_d, NTS)
        if MAXPH < 8:
            raise _Stop()

        r_es = contextlib.ExitStack()
        with r_es:
            def TR(name, shape, dt=F32, psum=False):
                return T(P, r_es, "r_" + name, shape, dt, psum)
            wr = TR("wr", [128, 8, 8], BF16)
            load_w(wr, moe_rw.rearrange("(kc kp) n -> kp kc n", kp=128), 8)
            rb = TR("rb", [128, 8])
            P.dma("sp", rb[:], moe_rb[0:1, :].partition_broadcast(128), writes=[rb])
            gf1 = TR("gf1", [128, D])
            P.dma("sp", gf1[:], norm_ffn[1:2, :].partition_broadcast(128), writes=[gf1])
            xt = [TR(f"xt{i}", [128, D]) for i in range(2)]
            hn = [TR(f"hn{i}", [128, D], BF16) for i in range(2)]
            hnT = [TR(f"hnT{i}", [128, 8, 128], BF16) for i in range(2)]
            sq = TR("sq", [128, D], BF16)
            ss = [TR(f"ss{i}", [128, 1]) for i in range(2)]
            rstd = [TR(f"rstd{i}", [128, 1]) for i in range(2)]
            lg = TR("lg", [128, 8]); l2 = TR("l2", [128, 8]); m1 = TR("m1", [128, 1]); m2 = TR("m2", [128, 1])
            eq = TR("eq", [128, 8]); sm = TR("sm", [128, 1])
            cw = [TR(f"cw{i}", [128, 8]) for i in range(2)]
            ps_tr = [TR(f"ps_tr{i}", [128, 1024], BF16, psum=True) for i in range(2)]
            ps_l = [TR(f"ps_l{i}", [128, 8], F32, psum=True) for i in range(2)]
            for tt in range(NTS):
                b = tt % 2
                P.dma("sp", xt[b][:], x3_d[tt * 128:(tt + 1) * 128, :], reads=[x3_d], writes=[xt[b]])
                rmsnorm_T(xt[b][:], xt[b], gf1, hn[b], sq, ss[b], rstd[b], ps_tr[b], hnT[b][:], hnT[b])
                pl = ps_l[b]
                for kc in range(8):
                    op("pe", lambda e, kc=kc, pl=pl, b=b: e.matmul(pl[:], lhsT=hnT[b][:, kc, :], rhs=wr[:, kc, :],
                                                                   start=(kc == 0), stop=(kc == 7)),
                       [hnT[b], wr], [pl])
                op("dve", lambda e, pl=pl: e.tensor_tensor(out=lg[:], in0=pl[:], in1=rb[:], op=ALU.add),
                   [pl, rb], [lg])
                op("dve", lambda e: e.reduce_max(out=m1[:], in_=lg[:], axis=AX.X), [lg], [m1])
                op("dve", lambda e: e.tensor_scalar(out=eq[:], in0=lg[:], scalar1=m1[:, 0:1], scalar2=-1.0e30,
                                                    op0=ALU.is_equal, op1=ALU.mult), [lg, m1], [eq])
                op("dve", lambda e: e.tensor_tensor(out=l2[:], in0=lg[:], in1=eq[:], op=ALU.add), [lg, eq], [l2])
                op("dve", lambda e: e.reduce_max(out=m2[:], in_=l2[:], axis=AX.X), [l2], [m2])
                op("dve", lambda e: e.tensor_scalar(out=eq[:], in0=lg[:], scalar1=m2[:, 0:1], scalar2=None,
                                                    op0=ALU.is_ge), [lg, m2], [eq])
                op("dve", lambda e: e.tensor_scalar(out=l2[:], in0=lg[:], scalar1=m1[:, 0:1], scalar2=None,
                                                    op0=ALU.subtract), [lg, m1], [l2])
                op("act", lambda e: e.activation(out=l2[:], in_=l2[:], func=AF.Exp), [l2], [l2])
                op("dve", lambda e: e.tensor_tensor(out=l2[:], in0=l2[:], in1=eq[:], op=ALU.mult), [l2, eq], [l2])
                op("dve", lambda e: e.reduce_sum(out=sm[:], in_=l2[:], axis=AX.X), [l2], [sm])
                op("dve", lambda e: e.reciprocal(out=sm[:], in_=sm[:]), [sm], [sm])
                cb = cw[b]
                op("dve", lambda e, cb=cb: e.tensor_scalar(out=cb[:], in0=l2[:], scalar1=sm[:, 0:1], scalar2=None,
                                                           op0=ALU.mult), [l2, sm], [cb])
                P.dma("sp", cw_d[tt * 128:(tt + 1) * 128, :], cb[:], reads=[cb], writes=[cw_d])
            P.barrier()
        P.emit()
        if MAXPH < 9:
            raise _Stop()

        NOWN = NT // 2
        o_es = contextlib.ExitStack()
        with o_es:
            def TO(name, shape, dt=F32, psum=False):
                return T(P, o_es, "o_" + name, shape, dt, psum)
            oidx = TO("oidx", [128, NOWN], I32)
            P.dma("sp", oidx[:], own_rows, writes=[oidx])
            xo = [TO(f"xo{i}", [128, D]) for i in range(2)]
            co = [TO(f"co{i}", [128, 8]) for i in range(2)]
            for i in range(NOWN + (1 if SMP else 0)):
                b = i % 2
                if i < NOWN:
                    P.idma(xo[b][:], x3_d.ap[:, :], oidx[:, i:i + 1], SEQ + 128, reads=[oidx, x3_d], writes=[xo[b]])
                    P.idma(co[b][:], cw_d.ap[:, :], oidx[:, i:i + 1], SEQ + 128, reads=[oidx, cw_d], writes=[co[b]])
                else:
                    P.dma("sp", xo[b][:], x3_d[SEQ:SEQ + 128, :], reads=[x3_d], writes=[xo[b]])
                    P.dma("sp", co[b][:], cw_d[SEQ:SEQ + 128, :], reads=[cw_d], writes=[co[b]])
                P.dma("sp", x3o_d[i * 128:(i + 1) * 128, :], xo[b][:], reads=[xo[b]], writes=[x3o_d])
                P.dma("sp", cwo_d[i * 128:(i + 1) * 128, :], co[b][:], reads=[co[b]], writes=[cwo_d])
            P.barrier()
        P.emit()

        NEXP = int(os.environ.get("KNEXP", "8"))
        OGROUPS = [list(range(g, min(g + 4, NOWN))) for g in range(0, NOWN, 4)] + ([[NOWN]] if SMP else [])
        HF = NFF // 2
        m_es = contextlib.ExitStack()
        with m_es:
            def TM(name, shape, dt=F32, psum=False):
                return T(P, m_es, "m_" + name, shape, dt, psum)
            wgb = [TM(f"wg{i}", [128, 8, HF * 128], BF16) for i in range(2)]
            wub = [TM(f"wu{i}", [128, 8, HF * 128], BF16) for i in range(2)]
            wdb = [TM(f"wd{i}", [128, HF, D], BF16) for i in range(2)]
            mstg = [TM(f"stg{i}", [128, 512], F32) for i in range(4)]
            mctr = [0]

            def load_half(ex, half, i2):
                def ld(dst, view, nk, ncols):
                    for kc in range(nk):
                        for c0 in range(0, ncols, 512):
                            w = min(512, ncols - c0)
                            st = mstg[mctr[0] % 4]
                            mctr[0] += 1
                            P.dma("sp", st[:, 0:w], view[:, kc, c0:c0 + w], writes=[st])
                            op("pool", lambda e, st=st, kc=kc, c0=c0, w=w, dst=dst: e.tensor_copy(
                                out=dst[:, kc, c0:c0 + w], in_=st[:, 0:w]), [st], [dst])
                f0 = half * HF * 128
                ld(wgb[i2], moe_wg[ex].rearrange("(kc kp) n -> kp kc n", kp=128)[:, :, f0:f0 + HF * 128], 8, HF * 128)
                ld(wub[i2], moe_wu[ex].rearrange("(kc kp) n -> kp kc n", kp=128)[:, :, f0:f0 + HF * 128], 8, HF * 128)
                ld(wdb[i2], moe_wd[ex][f0:f0 + HF * 128, :].rearrange("(kc kp) n -> kp kc n", kp=128), HF, D)

            gffn = TM("gffn", [128, D])
            P.dma("sp", gffn[:], norm_ffn[1:2, :].partition_broadcast(128), writes=[gffn])
            xg = TM("xg", [128, 4, D])
            rbuf = [TM(f"rbuf{i}", [128, 512]) for i in range(2)]
            cwg = TM("cwg", [128, 4, 8])
            hn = [TM(f"hn{i}", [128, D], BF16) for i in range(2)]
            hnTg = TM("hnTg", [128, 8, 512], BF16)
            sq = TM("sq", [128, D], BF16)
            ss = [TM(f"ss{i}", [128, 1]) for i in range(2)]
            rstd = [TM(f"rstd{i}", [128, 1]) for i in range(2)]
            hT = TM("hT", [128, HF, 512], BF16)
            gs = [TM(f"gs{i}", [128, 512]) for i in range(2)]
            x2t = [TM(f"x2t{i}", [128, 512]) for i in range(2)]
            ps_tr = [TM(f"ps_tr{i}", [128, 1024], BF16, psum=True) for i in range(2)]
            ps_g = [TM(f"ps_g{i}", [128, 512], F32, psum=True) for i in range(2)]
            ps_u = [TM(f"ps_u{i}", [128, 512], F32, psum=True) for i in range(2)]
            ps_d = [TM(f"ps_d{i}", [128, 512], F32, psum=True) for i in range(2)]
            passes = [(ex, half) for ex in range(NEXP) for half in range(2)]
            load_half(passes[0][0], passes[0][1], 0)
            src = x3o_d
            for k, (ex, half) in enumerate(passes):
                i2 = k % 2
                if k + 1 < len(passes):
                    load_half(passes[k + 1][0], passes[k + 1][1], (k + 1) % 2)
                wg, wu, wd = wgb[i2], wub[i2], wdb[i2]
                dst = ya_d if k % 2 == 0 else yb_d
                for tiles_g in OGROUPS:
                    ncol = len(tiles_g) * 128
                    for j, tt in enumerate(tiles_g):
                        P.dma("act", xg[:, j, :], x3o_d[tt * 128:(tt + 1) * 128, :], reads=[x3o_d], writes=[xg])
                        P.dma("act", cwg[:, j, :], cwo_d[tt * 128:(tt + 1) * 128, :], reads=[cwo_d], writes=[cwg])
                    for j, tt in enumerate(tiles_g):
                        b = j % 2
                        rmsnorm_T(xg[:, j, :], xg, gffn, hn[b], sq, ss[b], rstd[b], ps_tr[b],
                                  hnTg[:, :, j * 128:(j + 1) * 128], hnTg)
                    for fc in range(HF):
                        pg, pu = ps_g[fc % 2], ps_u[fc % 2]
                        for kc in range(8):
                            op("pe", lambda e, kc=kc, fc=fc, pg=pg, ncol=ncol, wg=wg: e.matmul(
                                pg[:, 0:ncol], lhsT=wg[:, kc, fc * 128:(fc + 1) * 128], rhs=hnTg[:, kc, 0:ncol],
                                start=(kc == 0), stop=(kc == 7)), [wg, hnTg], [pg])
                        for kc in range(8):
                            op("pe", lambda e, kc=kc, fc=fc, pu=pu, ncol=ncol, wu=wu: e.matmul(
                                pu[:, 0:ncol], lhsT=wu[:, kc, fc * 128:(fc + 1) * 128], rhs=hnTg[:, kc, 0:ncol],
                                start=(kc == 0), stop=(kc == 7)), [wu, hnTg], [pu])
                        gsb = gs[fc % 2]
                        op("act", lambda e, pg=pg, gsb=gsb, ncol=ncol: e.activation(
                            out=gsb[:, 0:ncol], in_=pg[:, 0:ncol], func=AF.Tanh, scale=0.5), [pg], [gsb])
                        op("dve", lambda e, pg=pg, gsb=gsb, ncol=ncol: e.scalar_tensor_tensor(
                            out=gsb[:, 0:ncol], in0=gsb[:, 0:ncol], scalar=1.0, in1=pg[:, 0:ncol], op0=ALU.add,
                            op1=ALU.mult), [gsb, pg], [gsb])
                        op("dve", lambda e, pu=pu, gsb=gsb, fc=fc, ncol=ncol: e.scalar_tensor_tensor(
                            out=hT[:, fc, 0:ncol], in0=gsb[:, 0:ncol], scalar=0.5, in1=pu[:, 0:ncol], op0=ALU.mult,
                            op1=ALU.mult), [gsb, pu], [hT])
                    for j, tt in enumerate(tiles_g):
                        for hh in range(2):
                            pd = ps_d[(2 * j + hh) % 2]
                            for fc in range(HF):
                                op("pe", lambda e, fc=fc, j=j, hh=hh, pd=pd, wd=wd: e.matmul(
                                    pd[:], lhsT=hT[:, fc, j * 128:(j + 1) * 128],
                                    rhs=wd[:, fc, hh * 512:(hh + 1) * 512],
                                    start=(fc == 0), stop=(fc == HF - 1)), [hT, wd], [pd])
                            xo_ = x2t[(2 * j + hh) % 2]
                            rb_ = rbuf[(2 * j + hh) % 2]
                            P.dma("act", rb_[:], src[tt * 128:(tt + 1) * 128, hh * 512:(hh + 1) * 512],
                                  reads=[src], writes=[rb_])
                            op("dve", lambda e, pd=pd, xo_=xo_, j=j, rb_=rb_, ex=ex: e.scalar_tensor_tensor(
                                out=xo_[:], in0=pd[:], scalar=cwg[:, j, ex:ex + 1], in1=rb_[:],
                                op0=ALU.mult, op1=ALU.add), [pd, rb_, cwg], [xo_])
                            P.dma("act", dst[tt * 128:(tt + 1) * 128, hh * 512:(hh + 1) * 512], xo_[:],
                                  reads=[xo_], writes=[dst])
                src = dst
            P.barrier()
        P.emit()
        if MAXPH < 10:
            raise _Stop()

        n_es = contextlib.ExitStack()
        with n_es:
            def TN(name, shape, dt=F32, psum=False):
                return T(P, n_es, "n_" + name, shape, dt, psum)
            gfin = TN("gfin", [128, D])
            P.dma("sp", gfin[:], norm_final[0:1, :].partition_broadcast(128), writes=[gfin])
            xt = [TN(f"xt{i}", [128, D]) for i in range(2)]
            yt = [TN(f"yt{i}", [128, D]) for i in range(2)]
            sq = TN("sq", [128, D], BF16)
            ss = [TN(f"ss{i}", [128, 1]) for i in range(2)]
            for tt in range(NOWN + (1 if SMP else 0)):
                b = tt % 2
                P.dma("sp", xt[b][:], src[tt * 128:(tt + 1) * 128, :], reads=[src], writes=[xt[b]])
                op("act", lambda e, b=b: e.activation(out=sq[:], in_=xt[b][:], func=AF.Square, accum_out=ss[b][:]),
                   [xt[b]], [sq, ss[b]])
                op("dve", lambda e, b=b: e.tensor_scalar(out=ss[b][:], in0=ss[b][:], scalar1=1.0 / D, scalar2=1e-6,
                                                         op0=ALU.mult, op1=ALU.add), [ss[b]], [ss[b]])
                op("act", lambda e, b=b: e.sqrt(out=ss[b][:], in_=ss[b][:]), [ss[b]], [ss[b]])
                op("dve", lambda e, b=b: e.reciprocal(out=ss[b][:], in_=ss[b][:]), [ss[b]], [ss[b]])
                op("dve", lambda e, b=b: e.scalar_tensor_tensor(out=yt[b][:], in0=xt[b][:], scalar=ss[b][:, 0:1],
                                                                in1=gfin[:], op0=ALU.mult, op1=ALU.mult),
                   [xt[b], ss[b], gfin], [yt[b]])
                if tt < NOWN:
                    P.dma("sp", o_yo[tt * 128:(tt + 1) * 128, :], yt[b][:], reads=[yt[b]], writes=[o_yo])
                else:
                    P.dma("sp", o_y_s[:], yt[b][:], reads=[yt[b]], writes=[o_y_s])
            P.barrier()
        P.emit()
    return nc


_CACHE = {}


def kernel(**inputs):
    f32 = np.float32
    if "nc" not in _CACHE:
        _CACHE["nc"] = build_program()
    nc = _CACHE["nc"]
    xp = np.asarray(inputs["x_prompt"], f32)
    xs = np.asarray(inputs["x_sample"], f32)
    hc = host_consts()

    def A(name, idx=None):
        a = np.asarray(inputs[name], f32)
        if idx is not None:
            a = a[idx]
        return np.ascontiguousarray(a)

    shared = {
        "norm_mix": A("norm_mix"), "norm_ffn": A("norm_ffn"),
        "w_in_even": A("w_in_even", 0), "w_out_even": A("w_out_even", 0),
        "hgrn_lb": A("hgrn_lb"), "hgrn_gnorm": A("hgrn_gnorm"),
        "ffn_w_gate": A("ffn_w_gate", 0), "ffn_w_up": A("ffn_w_up", 0), "ffn_w_down": A("ffn_w_down", 0),
        "w_in_odd": A("w_in_odd", 0), "w_out_odd": A("w_out_odd", 0),
        "norm_final": A("norm_final").reshape(1, D),
        "diff_lq1": A("diff_lq1"), "diff_lk1": A("diff_lk1"), "diff_lq2": A("diff_lq2"), "diff_lk2": A("diff_lk2"),
        "diff_subln": A("diff_subln"),
        "s5_a_re": A("s5_a_re", 0), "s5_a_im": A("s5_a_im", 0), "s5_log_dt": A("s5_log_dt"),
        "s5_b_re": A("s5_b_re", 0), "s5_b_im": A("s5_b_im", 0),
        "s5_c_re": A("s5_c_re", 0).reshape(512, 64), "s5_c_im": A("s5_c_im", 0).reshape(512, 64),
        "s5_d": A("s5_d", 0).reshape(1, 512), "s5_w_glu": A("s5_w_glu", 0), "s5_b_glu": A("s5_b_glu"),
        "moe_router_w": A("moe_router_w", 0), "moe_router_b": A("moe_router_b"),
        "moe_w_gate": A("moe_w_gate", 0), "moe_w_up": A("moe_w_up", 0), "moe_w_down": A("moe_w_down", 0),
    }
    for k, v in hc.items():
        shared["c_" + k] = v
    shared["pool_k"] = np.ascontiguousarray(np.asarray(inputs["cache_c_k"], f32)[0]).reshape(2560 * 128, 512)
    shared["pool_v"] = np.ascontiguousarray(np.asarray(inputs["cache_c_v"], f32)[0]).reshape(2560 * 128, 512)
    in_maps = []
    for c in range(NCORES):
        m = dict(shared)
        m["own_rows"] = np.ascontiguousarray(
            ((c % 2) * (SEQ // 2) + np.arange(SEQ // 2, dtype=np.int32)).reshape(NT // 2, 128).T)
        m["page_table"] = np.ascontiguousarray(np.asarray(inputs["page_table"], np.int32)[16 * c:16 * c + 16]).reshape(1, 256)
        m["x_seq"] = np.ascontiguousarray(xp[c // 2][:SEQ])
        m["x_smp"] = np.ascontiguousarray(xs[16 * c:16 * c + 16].reshape(128, D))
        m["cache_a_k"] = np.ascontiguousarray(np.asarray(inputs["cache_a_k"], f32)[0, 16 * c:16 * c + 16]).reshape(16, 2048, 512)
        m["cache_a_v"] = np.ascontiguousarray(np.asarray(inputs["cache_a_v"], f32)[0, 16 * c:16 * c + 16]).reshape(16, 2048, 512)
        m["state_s5_re"] = np.ascontiguousarray(np.asarray(inputs["state_s5_re"], f32)[0, 16 * c:16 * c + 16])
        m["state_s5_im"] = np.ascontiguousarray(np.asarray(inputs["state_s5_im"], f32)[0, 16 * c:16 * c + 16])
        m["state_hgrn"] = np.ascontiguousarray(np.asarray(inputs["state_hgrn"], f32)[0, 16 * c:16 * c + 16])
        in_maps.append(m)
    res = run_bass_kernel_spmd(nc, in_maps, core_ids=list(range(NCORES))).results

    B, DB, DS = 4, 128, 8
    y_prompt = np.stack([np.concatenate([res[2 * b]["o_yo"], res[2 * b + 1]["o_yo"]]) for b in range(B)])
    y_sample = np.concatenate([res[c]["o_y_s"].reshape(16, 8, D) for c in range(NCORES)])
    a_k = np.stack([res[2 * b]["o_ak"].reshape(NKEEP * 128, 8, 64) for b in range(B)])[None]
    a_v = np.stack([res[2 * b]["o_av"].reshape(NKEEP * 128, 8, 64) for b in range(B)])[None]
    hgrn = np.stack([res[2 * b]["o_hg"] for b in range(B)])[None]
    c_k = np.stack([res[2 * b]["o_ck"].reshape(SEQ, 4, 128) for b in range(B)])[None]
    c_v = np.stack([res[2 * b]["o_cv"].reshape(SEQ, 4, 128) for b in range(B)])[None]
    s5r = np.stack([res[2 * b]["o_s5r"] for b in range(B)])[None]
    s5i = np.stack([res[2 * b]["o_s5i"] for b in range(B)])[None]
    a_k_s = np.concatenate([res[c]["o_ak_s"].reshape(16, 8, 8, 64) for c in range(NCORES)])[None]
    a_v_s = np.concatenate([res[c]["o_av_s"].reshape(16, 8, 8, 64) for c in range(NCORES)])[None]
    hgrn_s = np.concatenate([res[c]["o_hg_s"] for c in range(NCORES)])[None]
    c_k_s = np.concatenate([res[c]["o_ck_s"].reshape(16, 8, 4, 128) for c in range(NCORES)])[None]
    c_v_s = np.concatenate([res[c]["o_cv_s"].reshape(16, 8, 4, 128) for c in range(NCORES)])[None]
    s5r_s = np.concatenate([res[c]["o_s5r_s"] for c in range(NCORES)])[None]
    s5i_s = np.concatenate([res[c]["o_s5i_s"] for c in range(NCORES)])[None]
    return (y_prompt, y_sample, a_k, a_v, hgrn, c_k, c_v, s5r, s5i,
            a_k_s, a_v_s, hgrn_s, c_k_s, c_v_s, s5r_s, s5i_s)
```

```python
import contextlib
import os
import numpy as np
import ml_dtypes
import concourse.bass as bass
import concourse.mybir as mybir
from concourse.bass_utils import run_bass_kernel_spmd

F32 = mybir.dt.float32
BF16 = mybir.dt.bfloat16
I32 = mybir.dt.int32
ALU = mybir.AluOpType
AF = mybir.ActivationFunctionType
AX = mybir.AxisListType

NCORES = 8
D = 1024
SEQ = int(os.environ.get("KSEQ", "4096"))
NKEEP = min(2048, SEQ) // 128
NT = SEQ // 128
EVEN_IN = 3584
DFF = 2816
NFF = DFF // 128
EPOCH = 12000
NDSEM = 32
STRIP = 2944
A_SLOPES = [2.0 ** (-(h + 1)) for h in range(8)]


MAXPH = int(os.environ.get("KMAXPH", "99"))


class _Stop(Exception):
    pass


class Res:
    __slots__ = ("name", "w", "r", "excl")

    def __init__(self, name, excl=False):
        self.name = name
        self.w = None
        self.r = {}
        self.excl = excl


def _res(x):
    return x.res if hasattr(x, "res") else x


class Prog:
    def __init__(self, nc, es):
        self.nc = nc
        self.es = es
        self.q = {e: [] for e in ("pe", "act", "dve", "pool", "sp")}
        self.cnt = {e: 0 for e in ("pe", "act", "dve", "pool")}
        self.esems = {e: [] for e in self.cnt}
        self.known = {e: {} for e in self.q}
        self.dsems = [es.enter_context(nc.semaphore(f"dsem{i}")) for i in range(NDSEM)]
        self.dval = [0] * NDSEM
        self.dnext = 0
        self.dnext_sw = NDSEM - 8

    def _esem(self, E, idx):
        ep = idx // EPOCH
        while len(self.esems[E]) <= ep:
            self.esems[E].append(self.es.enter_context(self.nc.semaphore(f"es_{E}{len(self.esems[E])}")))
        return self.esems[E][ep], idx % EPOCH + 1

    def _wait(self, E, tok):
        if tok is None:
            return
        if tok[0] == "eng":
            _, F, idx = tok
            if F == E and E == "pe":
                return
            if self.known[E].get(F, -1) >= idx:
                return
            self.known[E][F] = idx
            sem, val = self._esem(F, idx)
        else:
            _, si, val = tok
            key = ("d", si)
            if self.known[E].get(key, 0) >= val:
                return
            self.known[E][key] = val
            sem = self.dsems[si]
        self.q[E].append(lambda e, sem=sem, val=val: e.wait_ge(sem, val))

    def _deps(self, E, reads, writes):
        for r in reads:
            self._wait(E, r.w)
            if r.excl:
                for t in list(r.r.values()):
                    if not (t[0] == "eng" and t[1] == E):
                        self._wait(E, t)
        for w in writes:
            self._wait(E, w.w)
            for t in list(w.r.values()):
                self._wait(E, t)

    def _mark(self, tok, reads, writes):
        key = tok[1] if tok[0] == "eng" else ("d", tok[1])
        for r in reads:
            r.r[key] = tok
        for w in writes:
            w.w = tok
            w.r = {}

    def op(self, E, fn, reads=(), writes=()):
        reads = [_res(r) for r in reads]
        writes = [_res(r) for r in writes]
        self._deps(E, reads, writes)
        idx = self.cnt[E]
        self.cnt[E] += 1
        sem, _ = self._esem(E, idx)
        self.q[E].append(lambda e, fn=fn, sem=sem: fn(e).then_inc(sem, 1))
        self._mark(("eng", E, idx), reads, writes)

    def dma(self, Q, out, in_, reads=(), writes=(), slow=False):
        reads = [_res(r) for r in reads]
        writes = [_res(r) for r in writes]
        self._deps(Q, reads, writes)
        if Q == "pool":
            si = self.dnext_sw
            self.dnext_sw = NDSEM - 8 + (self.dnext_sw - (NDSEM - 8) + 1) % 8
        else:
            si = self.dnext
            self.dnext = (self.dnext + 1) % (NDSEM - 8)
        if self.dval[si] > 0:
            self._wait(Q, ("dma", si, self.dval[si]))
        self.dval[si] += 16
        sem = self.dsems[si]
        kw = {'allow_slow_non_contiguous': True} if slow else {}
        self.q[Q].append(lambda e, sem=sem, out=out, in_=in_, kw=kw: e.dma_start(out=out, in_=in_, **kw).then_inc(sem, 16))
        self._mark(("dma", si, self.dval[si]), reads, writes)

    def idma(self, out, in_, idx_ap, nrows, reads=(), writes=()):
        Q = "pool"
        reads = [_res(r) for r in reads]
        writes = [_res(r) for r in writes]
        self._deps(Q, reads, writes)
        si = self.dnext_sw
        self.dnext_sw = NDSEM - 8 + (self.dnext_sw - (NDSEM - 8) + 1) % 8
        if self.dval[si] > 0:
            self._wait(Q, ("dma", si, self.dval[si]))
        self.dval[si] += 16
        sem = self.dsems[si]
        self.q[Q].append(lambda e, sem=sem: e.indirect_dma_start(
            out=out, out_offset=None, in_=in_, in_offset=bass.IndirectOffsetOnAxis(ap=idx_ap, axis=0),
            ).then_inc(sem, 16))
        self._mark(("dma", si, self.dval[si]), reads, writes)

    def barrier(self):
        for E in self.q:
            for si in range(NDSEM):
                if self.dval[si] > 0:
                    self._wait(E, ("dma", si, self.dval[si]))
            for F in self.cnt:
                if self.cnt[F] > 0 and F != E:
                    self._wait(E, ("eng", F, self.cnt[F] - 1))

    def emit(self):
        nc = self.nc
        q = self.q
        with nc.Block() as block:
            @block.sync
            def _(e):
                for f in q["sp"]:
                    f(e)

            @block.tensor
            def _(e):
                for f in q["pe"]:
                    f(e)

            @block.scalar
            def _(e):
                for f in q["act"]:
                    f(e)

            @block.vector
            def _(e):
                for f in q["dve"]:
                    f(e)

            @block.gpsimd
            def _(e):
                for f in q["pool"]:
                    f(e)
        self.q = {e: [] for e in q}


class T:
    def __init__(self, P, es, name, shape, dtype, psum=False):
        ctx = P.nc.psum_tensor(name, shape, dtype) if psum else P.nc.sbuf_tensor(name, shape, dtype)
        self.t = es.enter_context(ctx)
        self.res = Res(name, excl=psum)
        self.shape = shape

    def __getitem__(self, k):
        return self.t[k]


class DR:
    def __init__(self, ap, name):
        self.ap = ap
        self.res = Res(name)

    def __getitem__(self, k):
        return self.ap[k]


def host_consts():
    c = {}
    c["ident"] = np.eye(128, dtype=np.float32).astype(ml_dtypes.bfloat16)
    s = np.arange(128)[:, None]
    t = np.arange(128)[None, :]
    lt = ((s // 64 == t // 64) & (s <= t)).astype(np.float32)
    c["lt64"] = lt
    c["mask64"] = np.tile(lt, (1, 8)).astype(ml_dtypes.bfloat16)
    ki = np.arange(128)[:, None]
    cc = np.arange(STRIP)[None, :]
    delta = cc - ki - 384
    mult = ((delta >= 0) & (delta <= 128)).astype(np.float32) \
        + ((delta >= 0) & (delta <= 512) & (delta % 4 == 0)) \
        + ((delta >= 0) & (delta <= 2048) & (delta % 16 == 0))
    c["dstrip"] = np.where(mult > 0, delta, 1.0e6).astype(np.float32)
    c["mstrip"] = mult.astype(ml_dtypes.bfloat16)
    c["ones_f"] = np.ones((128, 1), np.float32)
    ncol = (SEQ // 128 + 3) * 128
    cc2 = np.arange(ncol)[None, :]
    d2 = cc2 - ki - 384
    c["dstrip2"] = np.where(d2 >= 0, d2, 1.0e6).astype(np.float32)
    c["idx1"] = (np.arange(128, dtype=np.float32) + 1.0)[:, None]
    c["nidx1"] = -c["idx1"]
    c["lt128"] = (s <= t).astype(np.float32)
    sel = np.zeros((128, 128), np.float32)
    sel[127, :] = 1.0
    c["sel127"] = sel
    gm = np.zeros((128, 8), np.float32)
    gm[np.arange(128), np.arange(128) // 16] = 1.0
    c["gmask"] = gm
    c["ident_f"] = np.eye(128, dtype=np.float32)
    lt8 = ((s // 8 == t // 8) & (s <= t)).astype(np.float32)
    c["lt8"] = lt8
    c["mask8"] = np.tile(lt8, (1, 8)).astype(ml_dtypes.bfloat16)
    sm = np.zeros((128, 16), np.float32)
    sm[np.arange(128), np.arange(128) // 8] = 1.0
    c["seqmask"] = sm
    c["seqmask_b"] = sm.astype(ml_dtypes.bfloat16)
    c["seqselT"] = np.ascontiguousarray(sm.T)
    c["idx8"] = ((np.arange(128) % 8).astype(np.float32) + 1.0)[:, None]
    c["nidx8"] = -c["idx8"]
    s8 = np.zeros((128, 16), np.float32)
    s8[np.arange(16) * 8 + 7, np.arange(16)] = 1.0
    c["sel8"] = s8

    def mult_of(d):
        return ((d >= 0) & (d <= 128)).astype(np.float64) + ((d >= 0) & (d <= 512) & (d % 4 == 0)) \
            + ((d >= 0) & (d <= 2048) & (d % 16 == 0))
    ba = np.full((128, 33, 8, 8), -30000.0, np.float64)
    kk_ = np.arange(128)[:, None, None]
    qi = np.arange(8)[None, None, :]
    sl = np.array(A_SLOPES)[None, :, None]
    for kt in range(16):
        d = 2048 + qi - (128 * kt + kk_)
        m = mult_of(d)
        val = -sl * d + np.log(np.maximum(m, 1e-30))
        ba[:, kt] = np.where(m > 0, val, -30000.0)
    for sq_ in range(16):
        d = qi - (kk_ % 8)
        m = mult_of(d) * ((kk_ // 8) == sq_)
        val = -sl * d + np.log(np.maximum(m, 1e-30))
        ba[:, 16 + sq_] = np.where(m > 0, val, -30000.0)
    c["bias_a"] = ba.reshape(128, 33 * 64).astype(np.float32)
    bc = np.full((128, 33, 4, 2, 8), -30000.0, np.float64)
    slc = np.array([2.0 ** (-2.0 * (h + 1)) for h in range(4)])[None, :, None, None]
    kq = np.arange(128)[:, None, None, None]
    qq = np.arange(8)[None, None, None, :]
    for j in range(16):
        bc[:, j] = -slc * (2048 + qq - (128 * j + kq)) + np.zeros((1, 1, 2, 1))
    for sq_ in range(16):
        d = qq - (kq % 8)
        ok = (d >= 0) & ((kq // 8) == sq_)
        bc[:, 16 + sq_] = np.where(ok, -slc * d, -30000.0) + np.zeros((1, 1, 2, 1))
    c["bias_c"] = bc.reshape(128, 33 * 64).astype(np.float32)
    c["kidx"] = np.arange(128, dtype=np.float32)[:, None]
    return c


def build_program():
    nc = bass.Bass("TRN2", target_bir_lowering=False)
    es = contextlib.ExitStack()
    hc = host_consts()

    def din(name, shape, dt=F32):
        return nc.dram_tensor(name, list(shape), dt, kind="ExternalInput").ap()

    def dout(name, shape, dt=F32):
        return DR(nc.dram_tensor(name, list(shape), dt, kind="ExternalOutput").ap(), name)

    def dscr(name, shape, dt=F32):
        return DR(nc.dram_tensor(name, list(shape), dt).ap(), name)

    x_seq = din("x_seq", [SEQ, D])
    x_smp = din("x_smp", [128, D])
    norm_mix = din("norm_mix", [2, D])
    norm_ffn = din("norm_ffn", [2, D])
    w_in_even = din("w_in_even", [D, EVEN_IN])
    w_out_even = din("w_out_even", [D, D])
    hgrn_lb = din("hgrn_lb", [3, 512])
    hgrn_gnorm = din("hgrn_gnorm", [1, 64])
    ffn_wg = din("ffn_w_gate", [D, DFF])
    ffn_wu = din("ffn_w_up", [D, DFF])
    ffn_wd = din("ffn_w_down", [DFF, D])
    w_in_odd = din("w_in_odd", [D, 2048])
    w_out_odd = din("w_out_odd", [D, D])
    norm_final = din("norm_final", [1, D])
    dlq1 = din("diff_lq1", [1, 64]); dlk1 = din("diff_lk1", [1, 64])
    dlq2 = din("diff_lq2", [1, 64]); dlk2 = din("diff_lk2", [1, 64])
    dsubln = din("diff_subln", [1, 128])
    s5_a_re = din("s5_a_re", [32, 64]); s5_a_im = din("s5_a_im", [32, 64])
    s5_log_dt = din("s5_log_dt", [1, 32])
    s5_b_re = din("s5_b_re", [32, 64, 16]); s5_b_im = din("s5_b_im", [32, 64, 16])
    s5_c_re = din("s5_c_re", [512, 64]); s5_c_im = din("s5_c_im", [512, 64])
    s5_d = din("s5_d", [1, 512])
    s5_w_glu = din("s5_w_glu", [512, 512]); s5_b_glu = din("s5_b_glu", [1, 512])
    moe_rw = din("moe_router_w", [D, 8]); moe_rb = din("moe_router_b", [1, 8])
    moe_wg = din("moe_w_gate", [8, D, DFF]); moe_wu = din("moe_w_up", [8, D, DFF])
    moe_wd = din("moe_w_down", [8, DFF, D])
    cache_a_k = din("cache_a_k", [16, 2048, 512]); cache_a_v = din("cache_a_v", [16, 2048, 512])
    state_hg = din("state_hgrn", [16, 8, 64, 64])
    pool_k = din("pool_k", [2560 * 128, 512]); pool_v = din("pool_v", [2560 * 128, 512])
    ptab = din("page_table", [1, 256], I32)
    own_rows = din("own_rows", [128, NT // 2], I32)
    st_s5r = din("state_s5_re", [16, 32, 64]); st_s5i = din("state_s5_im", [16, 32, 64])
    cst = {k: din("c_" + k, v.shape, BF16 if v.dtype == ml_dtypes.bfloat16 else F32) for k, v in hc.items()}

    o_ak = dout("o_ak", [NKEEP * 128, 512])
    o_av = dout("o_av", [NKEEP * 128, 512])
    o_hg = dout("o_hg", [8, 64, 64])
    o_ck = dout("o_ck", [SEQ, 512])
    o_cv = dout("o_cv", [SEQ, 512])
    o_s5r = dout("o_s5r", [32, 64])
    o_s5i = dout("o_s5i", [32, 64])
    o_hg_s = dout("o_hg_s", [16, 8, 64, 64])
    o_s5r_s = dout("o_s5r_s", [16, 32, 64])
    o_s5i_s = dout("o_s5i_s", [16, 32, 64])
    o_y_s = dout("o_y_s", [128, D])
    o_yo = dout("o_yo", [SEQ // 2, D])
    o_ck_s = dout("o_ck_s", [128, 512])
    o_cv_s = dout("o_cv_s", [128, 512])
    o_ak_s = dout("o_ak_s", [128, 512])
    o_av_s = dout("o_av_s", [128, 512])

    qT_d = dscr("qT_d", [4, 128, SEQ + 128], BF16)
    kT_d = dscr("kT_d", [4, 128, SEQ + 128], BF16)
    Vp_d = dscr("Vp_d", [NT + 1, 128, 528], BF16)
    hgpre_d = dscr("hgpre_d", [128, 2048], F32)
    mixed_d = dscr("mixed_d", [SEQ + 128, D], BF16)
    x1_d = dscr("x1_d", [SEQ + 128, D], F32)
    x2_d = dscr("x2_d", [SEQ + 128, D], F32)
    x3_d = dscr("x3_d", [SEQ + 128, D], F32)
    ya_d = dscr("ya_d", [SEQ + 128, D], F32)
    yb_d = dscr("yb_d", [SEQ + 128, D], F32)
    cw_d = dscr("cw_d", [SEQ + 128, 8], F32)
    x3o_d = dscr("x3o_d", [SEQ // 2 + 128, D], F32)
    cwo_d = dscr("cwo_d", [SEQ // 2 + 128, 8], F32)
    q2T_d = dscr("q2T_d", [4, 128, SEQ + 128], BF16)
    k2T_d = dscr("k2T_d", [4, 128, SEQ + 128], BF16)
    uT_d = dscr("uT_d", [4, 128, SEQ + 128], BF16)
    Vc_d = dscr("Vc_d", [NT + 1, 128, 520], BF16)
    ud_d = dscr("ud_d", [SEQ + 128, 512], F32)
    mixed2_d = dscr("mixed2_d", [SEQ + 128, D], BF16)

    try:
        _build_body(nc, es, hc, locals())
    except _Stop:
        pass
    return nc


def _build_body(nc, es, hc, L):
    globals().update({k: v for k, v in L.items() if k not in ("nc", "es", "hc")})
    with es:
        P = Prog(nc, es)

        def op(E, fn, r=(), w=()):
            P.op(E, fn, r, w)

        ident = T(P, es, "ident", [128, 128], BF16)
        P.dma("sp", ident[:], cst["ident"], writes=[ident])

        def dbg_idma(tag):
            if os.environ.get("KDBG", "") != tag:
                return
            dd = contextlib.ExitStack()
            with dd:
                t_ = T(P, dd, "dbg_t" + tag, [128, 512], F32)
                i_ = T(P, dd, "dbg_i" + tag, [128, 4], I32)
                op("dve", lambda e: e.memset(i_[:], 0), [], [i_])
                for rep in range(int(os.environ.get("KDBGN", "1"))):
                    P.idma(t_[:], pool_k[:, :], i_[:, 0:1], 2560 * 128, reads=[i_], writes=[t_])
                    if rep % 30 == 29:
                        P.emit()
                P.barrier()
                P.emit()
            print("DBG idma ok at", tag)
            raise _Stop()

        wstg = [T(P, es, f"wstg{i}", [128, 512], F32) for i in range(3)]
        wctr = [0]

        def load_w(dst, src_view, nk):
            ncols = dst.shape[2]
            for kc in range(nk):
                for c0 in range(0, ncols, 512):
                    w = min(512, ncols - c0)
                    i = wctr[0] % 3
                    wctr[0] += 1
                    st = wstg[i]
                    P.dma("sp", st[:, 0:w], src_view[:, kc, c0:c0 + w], writes=[st])
                    eng = ("pool", "act", "dve")[i]
                    if eng == "act":
                        op(eng, lambda e, st=st, kc=kc, c0=c0, w=w: e.copy(out=dst[:, kc, c0:c0 + w], in_=st[:, 0:w]),
                           [st], [dst])
                    else:
                        op(eng, lambda e, st=st, kc=kc, c0=c0, w=w: e.tensor_copy(out=dst[:, kc, c0:c0 + w],
                                                                                 in_=st[:, 0:w]), [st], [dst])

        def rmsnorm_T(x_ap, x_res, g_t, hn_, sq_, ss_, rstd_, pt_, hnT_ap, hnT_res):
            op("act", lambda e: e.activation(out=sq_[:], in_=x_ap, func=AF.Square, accum_out=ss_[:]),
               [x_res], [sq_, ss_])
            op("dve", lambda e: e.tensor_scalar(out=rstd_[:], in0=ss_[:], scalar1=1.0 / D, scalar2=1e-6,
                                                op0=ALU.mult, op1=ALU.add), [ss_], [rstd_])
            op("act", lambda e: e.sqrt(out=rstd_[:], in_=rstd_[:]), [rstd_], [rstd_])
            op("dve", lambda e: e.reciprocal(out=rstd_[:], in_=rstd_[:]), [rstd_], [rstd_])
            op("dve", lambda e: e.scalar_tensor_tensor(out=hn_[:], in0=x_ap, scalar=rstd_[:, 0:1], in1=g_t[:],
                                                       op0=ALU.mult, op1=ALU.mult), [x_res, rstd_, g_t], [hn_])
            for c in range(8):
                op("pe", lambda e, c=c: e.transpose(out=pt_[:, c * 128:(c + 1) * 128],
                                                    in_=hn_[:, c * 128:(c + 1) * 128], identity=ident[:]),
                   [hn_, ident], [pt_])
            op("act", lambda e: e.copy(out=hnT_ap, in_=pt_[:].rearrange("p (a b) -> p a b", a=8)),
               [pt_], [hnT_res])

        dbg_idma("start")
        pes = contextlib.ExitStack()
        with pes:

            a_es = contextlib.ExitStack()
            with a_es:
                def TA(name, shape, dt=F32, psum=False):
                    return T(P, a_es, name, shape, dt, psum)
                w_in = TA("w_in", [128, 8, EVEN_IN], BF16)
                load_w(w_in, w_in_even.rearrange("(kc kp) n -> kp kc n", kp=128), 8)
                gmix0 = TA("gmix0", [128, D])
                P.dma("sp", gmix0[:], norm_mix[0:1, :].partition_broadcast(128), writes=[gmix0])
                lt64 = TA("lt64", [128, 128])
                P.dma("sp", lt64[:], cst["lt64"], writes=[lt64])
                mask64 = TA("mask64", [128, 1024], BF16)
                P.dma("sp", mask64[:], cst["mask64"], writes=[mask64])
                ones_f = TA("ones_f", [128, 1])
                P.dma("sp", ones_f[:], cst["ones_f"], writes=[ones_f])
                gn_t = TA("gn_t", [128, 8, 64])
                for h in range(8):
                    P.dma("sp", gn_t[:, h, :], hgrn_gnorm[0:1, :].partition_broadcast(128), writes=[gn_t])
                lb3 = TA("lb3", [128, 3, 512])
                for r in range(3):
                    P.dma("sp", lb3[:, r, :], hgrn_lb[r:r + 1, :].partition_broadcast(128), writes=[lb3])
                lb_t = TA("lb_t", [128, 512])
                oml_t = TA("oml_t", [128, 512])
                lbs = TA("lbs", [128, 512])
                op("act", lambda e: e.activation(out=lb3[:], in_=lb3[:], func=AF.Exp), [lb3], [lb3])
                op("dve", lambda e: e.tensor_tensor(out=lbs[:], in0=lb3[:, 0, :], in1=lb3[:, 1, :], op=ALU.add),
                   [lb3], [lbs])
                op("dve", lambda e: e.tensor_tensor(out=lbs[:], in0=lbs[:], in1=lb3[:, 2, :], op=ALU.add),
                   [lb3, lbs], [lbs])
                op("dve", lambda e: e.reciprocal(out=lbs[:], in_=lbs[:]), [lbs], [lbs])
                op("dve", lambda e: e.tensor_tensor(out=lb_t[:], in0=lb3[:, 0, :], in1=lbs[:], op=ALU.mult),
                   [lb3, lbs], [lb_t])
                op("dve", lambda e: e.tensor_scalar(out=oml_t[:], in0=lb_t[:], scalar1=-1.0, scalar2=1.0,
                                                    op0=ALU.mult, op1=ALU.add), [lb_t], [oml_t])

                xt = [TA(f"xt{i}", [128, D]) for i in range(2)]
                hn = [TA(f"hn{i}", [128, D], BF16) for i in range(2)]
                hnT = [TA(f"hnT{i}", [128, 8, 128], BF16) for i in range(2)]
                sq = TA("sq", [128, D], BF16)
                ss = [TA(f"ss{i}", [128, 1]) for i in range(2)]
                rstd = [TA(f"rstd{i}", [128, 1]) for i in range(2)]
                kv_sb = [TA(f"kv_sb{i}", [128, 1024]) for i in range(2)]
                qT_sb = [TA(f"qT_sb{i}", [128, 4, 128], BF16) for i in range(2)]
                kT_sb = [TA(f"kT_sb{i}", [128, 4, 128], BF16) for i in range(2)]
                vp_sb = [TA(f"vp_sb{i}", [128, 8, 66], BF16) for i in range(2)]
                for i in range(2):
                    op("dve", lambda e, i=i: e.memset(vp_sb[i][:], 1.0), [], [vp_sb[i]])
                ps_tr = [TA(f"ps_tr{i}", [128, 1024], BF16, psum=True) for i in range(2)]
                ps_mm = [TA(f"ps_mm{i}", [128, 512], F32, psum=True) for i in range(4)]
                ps_w = [TA(f"ps_w{i}", [128, 1024], F32, psum=True) for i in range(1)]
                sg = TA("sg", [128, 512])
                fgate = TA("fgate", [128, 512])
                glog = TA("glog", [128, 512])
                ecum = TA("ecum", [128, 512])
                encum = TA("encum", [128, 512])
                kk = TA("kk", [128, 512])
                ke = TA("ke", [128, 512], BF16)
                qh = TA("qh", [128, 512])
                qe = TA("qe", [128, 512], BF16)
                v_bf = TA("v_bf", [128, 512], BF16)
                gsil = TA("gsil", [128, 512])
                qeT = TA("qeT", [64, 8, 128], BF16)
                qeT0 = TA("qeT0", [64, 8, 128], BF16)
                qeT1 = TA("qeT1", [64, 8, 128], BF16)
                keT = TA("keT", [64, 8, 128], BF16)
                attT = TA("attT", [128, 1024], BF16)
                elast = TA("elast", [64, 16])
                S_f = TA("S_f", [64, 8, 64])
                S_tmp = TA("S_tmp", [64, 8, 64])
                S_bf = [TA(f"S_bf{i}", [64, 8, 64], BF16) for i in range(2)]
                o_sb = TA("o_sb", [128, 512])
                o_sq = TA("o_sq", [128, 512])
                ssq = TA("ssq", [128, 8])
                ob_bf = TA("ob_bf", [128, 512], BF16)

                op("dve", lambda e: e.memset(S_f[:], 0.0), [], [S_f])
                op("dve", lambda e: e.memset(S_bf[0][:], 0.0), [], [S_bf[0]])
                op("dve", lambda e: e.memset(qeT0[:], 0.0), [], [qeT0])
                op("dve", lambda e: e.memset(qeT1[:], 0.0), [], [qeT1])

                def proj_tok(hT, col0, ps):
                    for kc in range(8):
                        op("pe", lambda e, kc=kc: e.matmul(ps[:, 0:512], lhsT=hT[:, kc, :],
                                                           rhs=w_in[:, kc, col0:col0 + 512],
                                                           start=(kc == 0), stop=(kc == 7)), [hT, w_in], [ps])

                tiles = [(x_seq[tt * 128:(tt + 1) * 128, :], tt) for tt in range(NT)] + [(x_smp, NT)]
                tiles = tiles[:int(os.environ.get("KNT", "33"))]
                for it, (x_ap, tt) in enumerate(tiles):
                    b = it % 2
                    smp = tt == NT
                    P.dma("sp", xt[b][:], x_ap, writes=[xt[b]])
                    rmsnorm_T(xt[b][:], xt[b], gmix0, hn[b], sq, ss[b], rstd[b], ps_tr[b], hnT[b][:], hnT[b])
                    hT = hnT[b]
                    kb = kv_sb[b]
                    if smp or tt >= NT - NKEEP:
                        ps = ps_mm[0]
                        proj_tok(hT, 512, ps)
                        op("dve", lambda e, ps=ps, kb=kb: e.tensor_copy(out=kb[:, 0:512], in_=ps[:]), [ps], [kb])
                    ps = ps_mm[1]
                    proj_tok(hT, 1024, ps)
                    if smp or tt >= NT - NKEEP:
                        op("act", lambda e, ps=ps, kb=kb: e.copy(out=kb[:, 512:1024], in_=ps[:]), [ps], [kb])
                        if smp:
                            P.dma("sp", o_ak_s[:], kb[:, 0:512], reads=[kb], writes=[o_ak_s])
                            P.dma("sp", o_av_s[:], kb[:, 512:1024], reads=[kb], writes=[o_av_s])
                        else:
                            r0 = (tt - (NT - NKEEP)) * 128
                            P.dma("sp", o_ak[r0:r0 + 128, :], kb[:, 0:512], reads=[kb], writes=[o_ak])
                            P.dma("sp", o_av[r0:r0 + 128, :], kb[:, 512:1024], reads=[kb], writes=[o_av])
                    vs = vp_sb[b]
                    if os.environ.get("KVP", "1") in ("1", "2"):
                      op("dve", lambda e, ps=ps, vs=vs: e.tensor_copy(
                        out=vs[:, :, 0:64], in_=ps[:].rearrange("p (h d) -> p h d", h=8)), [ps], [vs])
                    if os.environ.get("KVP", "1") in ("1", "3"):
                      P.dma("sp", Vp_d[tt], vs[:].rearrange("p h d -> p (h d)"), reads=[vs], writes=[Vp_d])
                    for which, col0 in (((0, 0), (1, 512)) if os.environ.get("KQK", "1") == "1" else ()):
                        ps = ps_mm[2 + which]
                        for pr in range(4):
                            for kc in range(8):
                                op("pe", lambda e, kc=kc, pr=pr, ps=ps, col0=col0, hT=hT: e.matmul(
                                    ps[:, pr * 128:(pr + 1) * 128],
                                    lhsT=w_in[:, kc, col0 + pr * 128:col0 + (pr + 1) * 128],
                                    rhs=hT[:, kc, :], start=(kc == 0), stop=(kc == 7)), [hT, w_in], [ps])
                        if which == 0:
                            qs = qT_sb[b]
                            op("act", lambda e, ps=ps, qs=qs: e.mul(out=qs[:].rearrange("p a b -> p (a b)"),
                                                                    in_=ps[:], mul=0.125), [ps], [qs])
                            for pr in range(4):
                                P.dma("sp", qT_d[pr, :, tt * 128:(tt + 1) * 128], qs[:, pr, :],
                                      reads=[qs], writes=[qT_d])
                        else:
                            ks = kT_sb[b]
                            op("act", lambda e, ps=ps, ks=ks: e.copy(out=ks[:].rearrange("p a b -> p (a b)"),
                                                                     in_=ps[:]), [ps], [ks])
                            for pr in range(4):
                                P.dma("sp", kT_d[pr, :, tt * 128:(tt + 1) * 128], ks[:, pr, :],
                                      reads=[ks], writes=[kT_d])
                    if os.environ.get("KHG", "1") == "0":
                        continue
                    c0 = 1536
                    if smp:
                        for i4 in range(4):
                            ps = ps_mm[i4]
                            proj_tok(hT, c0 + i4 * 512, ps)
                            hb_ = kv_sb[b]
                            op("act" if i4 % 2 else "dve",
                               (lambda e, ps=ps, hb_=hb_, i4=i4: e.copy(out=hb_[:, (i4 % 2) * 512:(i4 % 2 + 1) * 512],
                                                                        in_=ps[:])) if i4 % 2 else
                               (lambda e, ps=ps, hb_=hb_, i4=i4: e.tensor_copy(
                                   out=hb_[:, (i4 % 2) * 512:(i4 % 2 + 1) * 512], in_=ps[:])), [ps], [hb_])
                            P.dma("sp", hgpre_d[:, i4 * 512:(i4 + 1) * 512],
                                  hb_[:, (i4 % 2) * 512:(i4 % 2 + 1) * 512], reads=[hb_], writes=[hgpre_d])
                        continue
                    ps_q, ps_f = ps_mm[0], ps_mm[1]
                    proj_tok(hT, c0 + 512, ps_f)
                    op("act", lambda e, ps_f=ps_f: e.activation(out=sg[:], in_=ps_f[:], func=AF.Tanh, scale=0.5),
                       [ps_f], [sg])
                    op("dve", lambda e: e.tensor_scalar(out=sg[:], in0=sg[:], scalar1=0.5, scalar2=0.5,
                                                        op0=ALU.mult, op1=ALU.add), [sg], [sg])
                    op("dve", lambda e: e.tensor_tensor(out=fgate[:], in0=sg[:], in1=oml_t[:], op=ALU.mult),
                       [sg, oml_t], [fgate])
                    op("dve", lambda e: e.tensor_tensor(out=fgate[:], in0=fgate[:], in1=lb_t[:], op=ALU.add),
                       [fgate, lb_t], [fgate])
                    op("act", lambda e: e.activation(out=glog[:], in_=fgate[:], func=AF.Ln), [fgate], [glog])
                    op("dve", lambda e: e.tensor_scalar(out=kk[:], in0=fgate[:], scalar1=-1.0, scalar2=1.0,
                                                        op0=ALU.mult, op1=ALU.add), [fgate], [kk])
                    ps_c = ps_mm[2]
                    op("pe", lambda e, ps_c=ps_c: e.matmul(ps_c[:], lhsT=lt64[:], rhs=glog[:], start=True, stop=True),
                       [lt64, glog], [ps_c])
                    op("act", lambda e, ps_c=ps_c: e.activation(out=ecum[:], in_=ps_c[:], func=AF.Exp),
                       [ps_c], [ecum])
                    op("act", lambda e, ps_c=ps_c: e.activation(out=encum[:], in_=ps_c[:], func=AF.Exp, scale=-1.0),
                       [ps_c], [encum])
                    op("dve", lambda e: e.tensor_tensor(out=ke[:], in0=kk[:], in1=encum[:], op=ALU.mult),
                       [kk, encum], [ke])
                    proj_tok(hT, c0, ps_q)
                    op("act", lambda e, ps_q=ps_q: e.activation(out=qh[:], in_=ps_q[:], func=AF.Tanh, scale=0.5),
                       [ps_q], [qh])
                    op("dve", lambda e, ps_q=ps_q: e.scalar_tensor_tensor(out=qh[:], in0=qh[:], scalar=1.0,
                                                                          in1=ps_q[:], op0=ALU.add, op1=ALU.mult),
                       [qh, ps_q], [qh])
                    op("dve", lambda e: e.scalar_tensor_tensor(out=qe[:], in0=qh[:], scalar=0.0625, in1=ecum[:],
                                                               op0=ALU.mult, op1=ALU.mult), [qh, ecum], [qe])
                    ps_i = ps_mm[3]
                    proj_tok(hT, c0 + 1024, ps_i)
                    op("act", lambda e, ps_i=ps_i: e.copy(out=v_bf[:], in_=ps_i[:]), [ps_i], [v_bf])
                    ps_g = ps_mm[2]
                    proj_tok(hT, c0 + 1536, ps_g)
                    op("act", lambda e, ps_g=ps_g: e.activation(out=gsil[:], in_=ps_g[:], func=AF.Tanh, scale=0.5),
                       [ps_g], [gsil])
                    op("dve", lambda e, ps_g=ps_g: e.scalar_tensor_tensor(out=gsil[:], in0=gsil[:], scalar=1.0,
                                                                          in1=ps_g[:], op0=ALU.add, op1=ALU.mult),
                       [gsil, ps_g], [gsil])
                    ps_e = ps_mm[0]
                    for c in range(2):
                        for h in range(8):
                            op("pe", lambda e, c=c, h=h, ps_e=ps_e: e.matmul(
                                ps_e[0:64, c * 8 + h:c * 8 + h + 1],
                                lhsT=glog[c * 64:(c + 1) * 64, h * 64:(h + 1) * 64],
                                rhs=ones_f[c * 64:(c + 1) * 64, 0:1], start=True, stop=True),
                               [glog, ones_f], [ps_e])
                    op("act", lambda e, ps_e=ps_e: e.activation(out=elast[:], in_=ps_e[0:64, 0:16], func=AF.Exp),
                       [ps_e], [elast])
                    for src, dst, pt in ((qe, qeT, ps_tr[0]), (ke, keT, ps_tr[1])):
                        for h in range(8):
                            op("pe", lambda e, h=h, src=src, pt=pt: e.transpose(
                                out=pt[0:64, h * 128:(h + 1) * 128], in_=src[:, h * 64:(h + 1) * 64],
                                identity=ident[:]), [src, ident], [pt])
                        if dst is keT:
                            op("act", lambda e, dst=dst, pt=pt: e.copy(
                                out=dst[:].rearrange("p a b -> p (a b)"), in_=pt[0:64, :]), [pt], [dst])
                        else:
                            op("dve", lambda e, dst=dst, pt=pt: e.tensor_copy(
                                out=dst[:].rearrange("p a b -> p (a b)"), in_=pt[0:64, :]), [pt], [dst])
                    op("dve", lambda e: e.tensor_copy(out=qeT0[:, :, 0:64], in_=qeT[:, :, 0:64]), [qeT], [qeT0])
                    op("dve", lambda e: e.tensor_copy(out=qeT1[:, :, 64:128], in_=qeT[:, :, 64:128]), [qeT], [qeT1])
                    pw = ps_w[0]
                    for h in range(8):
                        op("pe", lambda e, h=h, pw=pw: e.matmul(pw[:, h * 128:(h + 1) * 128], lhsT=keT[:, h, :],
                                                                rhs=qeT[:, h, :], start=True, stop=True),
                           [keT, qeT], [pw])
                    op("dve", lambda e, pw=pw: e.tensor_tensor(out=attT[:], in0=pw[:], in1=mask64[:], op=ALU.mult),
                       [pw, mask64], [attT])
                    ps_s = ps_mm[1]
                    for c in range(2):
                        for h in range(8):
                            op("pe", lambda e, c=c, h=h, ps_s=ps_s: e.matmul(
                                ps_s[0:64, h * 64:(h + 1) * 64],
                                lhsT=ke[c * 64:(c + 1) * 64, h * 64:(h + 1) * 64],
                                rhs=v_bf[c * 64:(c + 1) * 64, h * 64:(h + 1) * 64], start=True, stop=True),
                               [ke, v_bf], [ps_s])
                        op("dve", lambda e, ps_s=ps_s: e.tensor_tensor(
                            out=S_tmp[:], in0=S_f[:], in1=ps_s[0:64, :].rearrange("p (h v) -> p h v", h=8),
                            op=ALU.add), [S_f, ps_s], [S_tmp])
                        op("dve", lambda e, c=c: e.tensor_tensor(
                            out=S_f[:], in0=S_tmp[:],
                            in1=elast[:, c * 8:(c + 1) * 8].unsqueeze(2).to_broadcast([64, 8, 64]),
                            op=ALU.mult), [S_tmp, elast], [S_f])
                        if c == 0:
                            op("act", lambda e: e.copy(out=S_bf[1][:], in_=S_f[:]), [S_f], [S_bf[1]])
                    ps_o = ps_mm[3]
                    for h in range(8):
                        osl = ps_o[:, h * 64:(h + 1) * 64]
                        op("pe", lambda e, h=h, osl=osl: e.matmul(osl, lhsT=attT[:, h * 128:(h + 1) * 128],
                                                                  rhs=v_bf[:, h * 64:(h + 1) * 64],
                                                                  start=True, stop=False), [attT, v_bf], [ps_o])
                        op("pe", lambda e, h=h, osl=osl: e.matmul(osl, lhsT=qeT0[:, h, :], rhs=S_bf[0][:, h, :],
                                                                  start=False, stop=False), [qeT0, S_bf[0]], [ps_o])
                        op("pe", lambda e, h=h, osl=osl: e.matmul(osl, lhsT=qeT1[:, h, :], rhs=S_bf[1][:, h, :],
                                                                  start=False, stop=True), [qeT1, S_bf[1]], [ps_o])
                    op("act", lambda e: e.copy(out=S_bf[0][:], in_=S_f[:]), [S_f], [S_bf[0]])
                    op("act", lambda e, ps_o=ps_o: e.copy(out=o_sb[:], in_=ps_o[:]), [ps_o], [o_sb])
                    op("dve", lambda e: e.tensor_tensor(out=o_sq[:], in0=o_sb[:], in1=o_sb[:], op=ALU.mult),
                       [o_sb], [o_sq])
                    op("dve", lambda e: e.reduce_sum(out=ssq[:], in_=o_sq[:].rearrange("p (h v) -> p h v", h=8),
                                                     axis=AX.X), [o_sq], [ssq])
                    op("dve", lambda e: e.tensor_scalar(out=ssq[:], in0=ssq[:], scalar1=1.0 / 64, scalar2=1e-6,
                                                        op0=ALU.mult, op1=ALU.add), [ssq], [ssq])
                    op("act", lambda e: e.sqrt(out=ssq[:], in_=ssq[:]), [ssq], [ssq])
                    op("dve", lambda e: e.reciprocal(out=ssq[:], in_=ssq[:]), [ssq], [ssq])
                    op("dve", lambda e: e.tensor_tensor(
                        out=o_sq[:].rearrange("p (h v) -> p h v", h=8),
                        in0=o_sb[:].rearrange("p (h v) -> p h v", h=8),
                        in1=ssq[:].unsqueeze(2).to_broadcast([128, 8, 64]), op=ALU.mult), [o_sb, ssq], [o_sq])
                    op("dve", lambda e: e.tensor_tensor(out=o_sq[:], in0=o_sq[:],
                                                        in1=gn_t[:].rearrange("p h v -> p (h v)"), op=ALU.mult),
                       [o_sq, gn_t], [o_sq])
                    op("dve", lambda e: e.scalar_tensor_tensor(out=ob_bf[:], in0=o_sq[:], scalar=0.5, in1=gsil[:],
                                                               op0=ALU.mult, op1=ALU.mult), [o_sq, gsil], [ob_bf])
                    P.dma("sp", mixed_d[tt * 128:(tt + 1) * 128, 512:1024], ob_bf[:], reads=[ob_bf],
                          writes=[mixed_d])
                P.dma("sp", o_hg.ap.rearrange("h k v -> k h v"), S_f[:], reads=[S_f], writes=[o_hg])
                P.barrier()
            P.emit()
            if MAXPH < 1:
                raise _Stop()

            dbg_idma("after0A")
            b_es = contextlib.ExitStack()
            with b_es:
                def TB(name, shape, dt=F32, psum=False):
                    return T(P, b_es, name, shape, dt, psum)
                dstrip = TB("dstrip", [128, STRIP])
                mstrip = TB("mstrip", [128, STRIP], BF16)
                P.dma("sp", dstrip[:], cst["dstrip"], writes=[dstrip])
                P.dma("sp", mstrip[:], cst["mstrip"], writes=[mstrip])
                qg = [TB(f"qg{i}", [128, 4, 512], BF16) for i in range(2)]
                kwin = [TB(f"kwin{i}", [128, 4, 2560], BF16) for i in range(2)]
                vwin = [TB(f"vwin{i}", [128, 20, 528], BF16) for i in range(2)]
                tmp = [TB(f"tmp{i}", [128, 512]) for i in range(2)]
                Eb = [TB(f"Eb{i}", [128, 512], BF16) for i in range(2)]
                PTb = [TB(f"PTb{i}", [128, 20, 512], BF16) for i in range(2)]
                rden = TB("rden", [128, 4])
                oa = [TB(f"oa{i}", [128, 4, 512], BF16) for i in range(2)]
                ps_s = [TB(f"ps_s{i}", [128, 512], F32, psum=True) for i in range(3)]
                ps_o = [TB(f"ps_o{i}", [128, 4, 128], F32, psum=True) for i in range(2)]
                blk = 0
                for Q in range(NT // 4):
                    qb_ = qg[Q % 2]
                    for pr in range(4):
                        P.dma("sp", qb_[:, pr, :], qT_d[pr, :, Q * 512:(Q + 1) * 512], reads=[qT_d], writes=[qb_])
                    oab = oa[Q % 2]
                    kts = list(range(max(0, 4 * Q - 16), 4 * Q + 4))
                    kw, vw = kwin[Q % 2], vwin[Q % 2]
                    k0 = kts[0]
                    for pr in range(4):
                        P.dma("sp", kw[:, pr, 0:len(kts) * 128], kT_d[pr, :, k0 * 128:(kts[-1] + 1) * 128],
                              reads=[kT_d], writes=[kw])
                    for kt in kts:
                        P.dma("sp", vw[:, kt - k0, :], Vp_d[kt], reads=[Vp_d], writes=[vw])
                    for h in range(8):
                        pr, r0 = h // 2, (h % 2) * 64
                        po = ps_o[h % 2]
                        valid = {j: [kt for kt in kts if kt <= 4 * Q + j and 4 * Q + j - kt <= 16] for j in range(4)}
                        ptb = PTb[h % 2]
                        for kt in kts:
                            Dk = 4 * Q - kt
                            c0 = (Dk + 3) * 128
                            ps = ps_s[blk % 3]
                            tm, eb = tmp[blk % 2], Eb[blk % 2]
                            blk += 1
                            op("pe", lambda e, ps=ps, kt=kt, pr=pr, r0=r0, qb_=qb_, kw=kw, k0=k0: e.matmul(
                                ps[:], lhsT=kw[r0:r0 + 64, pr, (kt - k0) * 128:(kt - k0 + 1) * 128],
                                rhs=qb_[r0:r0 + 64, pr, :], start=True, stop=True), [kw, qb_], [ps])
                            op("dve", lambda e, ps=ps, tm=tm, c0=c0, h=h: e.scalar_tensor_tensor(
                                out=tm[:], in0=dstrip[:, c0:c0 + 512], scalar=-A_SLOPES[h], in1=ps[:],
                                op0=ALU.mult, op1=ALU.add), [dstrip, ps], [tm])
                            op("act", lambda e, tm=tm, eb=eb: e.activation(out=eb[:], in_=tm[:], func=AF.Exp),
                               [tm], [eb])
                            op("pool", lambda e, eb=eb, ptb=ptb, c0=c0, kt=kt, k0=k0: e.tensor_tensor(
                                out=ptb[:, kt - k0, :], in0=eb[:], in1=mstrip[:, c0:c0 + 512], op=ALU.mult),
                               [eb, mstrip], [ptb])
                        for j in range(4):
                            v = valid[j]
                            for kt in v:
                                op("pe", lambda e, j=j, ptb=ptb, po=po, kt=kt, h=h, v=v, vw=vw, k0=k0: e.matmul(
                                    po[:, j, 0:66], lhsT=ptb[:, kt - k0, j * 128:(j + 1) * 128],
                                    rhs=vw[:, kt - k0, h * 66:(h + 1) * 66],
                                    start=(kt == v[0]), stop=(kt == v[-1])), [ptb, vw], [po])
                        op("dve", lambda e, po=po: e.reciprocal(out=rden[:], in_=po[:, :, 64]), [po], [rden])
                        op("dve", lambda e, po=po, oab=oab, h=h: e.tensor_tensor(
                            out=oab[:, :, h * 64:(h + 1) * 64], in0=po[:, :, 0:64],
                            in1=rden[:].unsqueeze(2).to_broadcast([128, 4, 64]), op=ALU.mult), [po, rden], [oab])
                    for j in range(4):
                        t0 = (4 * Q + j) * 128
                        P.dma("sp", mixed_d[t0:t0 + 128, 0:512], oab[:, j, :], reads=[oab], writes=[mixed_d])
                P.barrier()
            P.emit()
            if MAXPH < 2:
                raise _Stop()

        dbg_idma("after0B")
        if os.environ.get("KSMP", "1") == "1":
          s_es = contextlib.ExitStack()
          with s_es:
            def TS(name, shape, dt=F32, psum=False):
                return T(P, s_es, "s_" + name, shape, dt, psum)
            ident_f = TS("ident_f", [128, 128])
            P.dma("sp", ident_f[:], cst["ident_f"], writes=[ident_f])
            bias_a = TS("bias_a", [128, 33, 64])
            P.dma("sp", bias_a[:].rearrange("p a b -> p (a b)"), cst["bias_a"], writes=[bias_a])
            qTn = TS("qTn", [128, 4, 128], BF16); kTn = TS("kTn", [128, 4, 128], BF16)
            for pr in range(4):
                P.dma("sp", qTn[:, pr, :], qT_d[pr, :, SEQ:SEQ + 128], reads=[qT_d], writes=[qTn])
                P.dma("sp", kTn[:, pr, :], kT_d[pr, :, SEQ:SEQ + 128], reads=[kT_d], writes=[kTn])
            vS = [TS(f"vS{i}", [128, 17, 528], BF16) for i in range(2)]
            for i in range(2):
                op("pool", lambda e, i=i: e.memset(vS[i][:], 1.0), [], [vS[i]])
                P.dma("sp", vS[i][:, 16, :], Vp_d[NT], reads=[Vp_d], writes=[vS[i]])
            kc = [TS(f"kc{i}", [128, 512]) for i in range(2)]
            vcs = [TS(f"vcs{i}", [128, 512]) for i in range(2)]
            kTc = [TS(f"kTc{i}", [128, 4, 128], BF16) for i in range(2)]
            tmpa = [TS(f"tmpa{i}", [128, 64]) for i in range(2)]
            PTs = [TS(f"PTs{i}", [128, 17, 64], BF16) for i in range(2)]
            rdn = TS("rdn", [8, 8])
            oas = [TS(f"oas{i}", [8, 8, 64], BF16) for i in range(2)]
            ps_kt = [TS(f"ps_kt{i}", [128, 512], F32, psum=True) for i in range(2)]
            ps_sc = [TS(f"ps_sc{i}", [128, 64], F32, psum=True) for i in range(2)]
            ps_po = [TS(f"ps_po{i}", [128, 4, 128], F32, psum=True) for i in range(2)]
            it = 0
            for sq_ in range(16):
                vb, ptb = vS[sq_ % 2], PTs[sq_ % 2]
                for kt in range(17):
                    b = it % 2
                    it += 1
                    pss = ps_sc[b]
                    tm = tmpa[b]
                    if kt < 16:
                        P.dma("sp", kc[b][:], cache_a_k[sq_, kt * 128:(kt + 1) * 128, :], writes=[kc[b]])
                        P.dma("sp", vcs[b][:], cache_a_v[sq_, kt * 128:(kt + 1) * 128, :], writes=[vcs[b]])
                        op("pool", lambda e, b=b, vb=vb, kt=kt: e.tensor_copy(
                            out=vb[:, kt, :].rearrange("p (h d) -> p h d", d=66)[:, :, 0:64],
                            in_=vcs[b][:].rearrange("p (h d) -> p h d", d=64)), [vcs[b]], [vb])
                        pk = ps_kt[b]
                        for c4 in range(4):
                            op("pe", lambda e, c4=c4, pk=pk, b=b: e.transpose(
                                out=pk[:, c4 * 128:(c4 + 1) * 128], in_=kc[b][:, c4 * 128:(c4 + 1) * 128],
                                identity=ident_f[:]), [kc[b], ident_f], [pk])
                        kt_ = kTc[b]
                        op("act", lambda e, pk=pk, kt_=kt_: e.copy(out=kt_[:].rearrange("p a b -> p (a b)"),
                                                                   in_=pk[:]), [pk], [kt_])
                        bi = kt
                    else:
                        kt_ = kTn
                        bi = 16 + sq_
                    for h in range(8):
                        pr, r0 = h // 2, (h % 2) * 64
                        op("pe", lambda e, h=h, pr=pr, r0=r0, pss=pss, kt_=kt_, sq_=sq_: e.matmul(
                            pss[:, h * 8:(h + 1) * 8], lhsT=kt_[r0:r0 + 64, pr, :],
                            rhs=qTn[r0:r0 + 64, pr, sq_ * 8:(sq_ + 1) * 8], start=True, stop=True),
                           [kt_, qTn], [pss])
                    op("dve", lambda e, pss=pss, tm=tm, bi=bi: e.tensor_tensor(
                        out=tm[:], in0=pss[:], in1=bias_a[:, bi, :], op=ALU.add), [pss, bias_a], [tm])
                    op("act", lambda e, tm=tm, ptb=ptb, kt=kt: e.activation(out=ptb[:, kt, :], in_=tm[:],
                                                                           func=AF.Exp), [tm], [ptb])
                for h in range(8):
                    po = ps_po[h // 4]
                    for kt in range(17):
                        op("pe", lambda e, h=h, kt=kt, po=po, ptb=ptb, vb=vb: e.matmul(
                            po[0:8, h % 4, 0:66], lhsT=ptb[:, kt, h * 8:(h + 1) * 8],
                            rhs=vb[:, kt, h * 66:(h + 1) * 66], start=(kt == 0), stop=(kt == 16)),
                           [ptb, vb], [po])
                ob_ = oas[sq_ % 2]
                for half in range(2):
                    po = ps_po[half]
                    op("dve", lambda e, po=po, half=half: e.reciprocal(out=rdn[:, half * 4:(half + 1) * 4],
                                                                       in_=po[0:8, :, 64]), [po], [rdn])
                    op("dve", lambda e, po=po, half=half, ob_=ob_: e.tensor_tensor(
                        out=ob_[:, half * 4:(half + 1) * 4, :], in0=po[0:8, :, 0:64],
                        in1=rdn[:, half * 4:(half + 1) * 4].unsqueeze(2).to_broadcast([8, 4, 64]),
                        op=ALU.mult), [po, rdn], [ob_])
                P.dma("sp", mixed_d[SEQ + sq_ * 8:SEQ + sq_ * 8 + 8, 0:512], ob_[:].rearrange("p h d -> p (h d)"),
                      reads=[ob_], writes=[mixed_d])
            P.barrier()
          P.emit()

          h_es = contextlib.ExitStack()
          with h_es:
            def TH(name, shape, dt=F32, psum=False):
                return T(P, h_es, "hs_" + name, shape, dt, psum)
            lt8 = TH("lt8", [128, 128]); mask8 = TH("mask8", [128, 1024], BF16)
            seqm = TH("seqm", [128, 16]); seqmb = TH("seqmb", [128, 16], BF16)
            P.dma("sp", lt8[:], cst["lt8"], writes=[lt8])
            P.dma("sp", mask8[:], cst["mask8"], writes=[mask8])
            P.dma("sp", seqm[:], cst["seqmask"], writes=[seqm])
            P.dma("sp", seqmb[:], cst["seqmask_b"], writes=[seqmb])
            gn_t = TH("gn_t", [128, 8, 64])
            for h in range(8):
                P.dma("sp", gn_t[:, h, :], hgrn_gnorm[0:1, :].partition_broadcast(128), writes=[gn_t])
            lb3 = TH("lb3", [128, 3, 512])
            for r in range(3):
                P.dma("sp", lb3[:, r, :], hgrn_lb[r:r + 1, :].partition_broadcast(128), writes=[lb3])
            lb_t = TH("lb_t", [128, 512]); oml_t = TH("oml_t", [128, 512]); lbs = TH("lbs", [128, 512])
            op("act", lambda e: e.activation(out=lb3[:], in_=lb3[:], func=AF.Exp), [lb3], [lb3])
            op("dve", lambda e: e.tensor_tensor(out=lbs[:], in0=lb3[:, 0, :], in1=lb3[:, 1, :], op=ALU.add),
               [lb3], [lbs])
            op("dve", lambda e: e.tensor_tensor(out=lbs[:], in0=lbs[:], in1=lb3[:, 2, :], op=ALU.add),
               [lb3, lbs], [lbs])
            op("dve", lambda e: e.reciprocal(out=lbs[:], in_=lbs[:]), [lbs], [lbs])
            op("dve", lambda e: e.tensor_tensor(out=lb_t[:], in0=lb3[:, 0, :], in1=lbs[:], op=ALU.mult),
               [lb3, lbs], [lb_t])
            op("dve", lambda e: e.tensor_scalar(out=oml_t[:], in0=lb_t[:], scalar1=-1.0, scalar2=1.0,
                                                op0=ALU.mult, op1=ALU.add), [lb_t], [oml_t])
            pre = TH("pre", [128, 4, 512])
            P.dma("sp", pre[:].rearrange("p a b -> p (a b)"), hgpre_d[:, :], reads=[hgpre_d], writes=[pre])
            S0 = TH("S0", [64, 16, 8, 64]); S0b = TH("S0b", [64, 16, 8, 64], BF16)
            for sq_ in range(16):
                P.dma("sp", S0[:, sq_, :, :], state_hg[sq_].rearrange("h k v -> k h v"), writes=[S0])
            op("act", lambda e: e.copy(out=S0b[:], in_=S0[:]), [S0], [S0b])
            sg = TH("sg", [128, 512]); glog = TH("glog", [128, 512]); ecum = TH("ecum", [128, 512])
            encum = TH("encum", [128, 512]); kk = TH("kk", [128, 512]); qh = TH("qh", [128, 512])
            gsil = TH("gsil", [128, 512]); o_sb = TH("o_sb", [128, 512]); o_sq = TH("o_sq", [128, 512])
            ke = TH("ke", [128, 512], BF16); qe = TH("qe", [128, 512], BF16); v_bf = TH("v_bf", [128, 512], BF16)
            ob_bf = TH("ob_bf", [128, 512], BF16); ssq = TH("ssq", [128, 8])
            qeT = TH("qeT", [64, 8, 128], BF16); keT = TH("keT", [64, 8, 128], BF16)
            qeTx = TH("qeTx", [64, 8, 2176], BF16)
            attT = TH("attT", [128, 1024], BF16)
            vex = TH("vex", [128, 16, 64], BF16)
            els = TH("els", [64, 8, 16])
            Sn = TH("Sn", [64, 16, 8, 64])
            ps_a = [TH(f"ps_a{i}", [128, 512], F32, psum=True) for i in range(3)]
            ps_t2 = [TH(f"ps_t{i}", [128, 1024], BF16, psum=True) for i in range(2)]
            ps_w2 = TH("ps_w", [128, 1024], F32, psum=True)
            op("dve", lambda e: e.memset(qeTx[:], 0.0), [], [qeTx])
            op("act", lambda e: e.activation(out=sg[:], in_=pre[:, 1, :], func=AF.Tanh, scale=0.5), [pre], [sg])
            op("dve", lambda e: e.tensor_scalar(out=sg[:], in0=sg[:], scalar1=0.5, scalar2=0.5,
                                                op0=ALU.mult, op1=ALU.add), [sg], [sg])
            op("dve", lambda e: e.tensor_tensor(out=sg[:], in0=sg[:], in1=oml_t[:], op=ALU.mult), [sg, oml_t], [sg])
            op("dve", lambda e: e.tensor_tensor(out=sg[:], in0=sg[:], in1=lb_t[:], op=ALU.add), [sg, lb_t], [sg])
            op("act", lambda e: e.activation(out=glog[:], in_=sg[:], func=AF.Ln), [sg], [glog])
            op("dve", lambda e: e.tensor_scalar(out=kk[:], in0=sg[:], scalar1=-1.0, scalar2=1.0,
                                                op0=ALU.mult, op1=ALU.add), [sg], [kk])
            pc = ps_a[0]
            op("pe", lambda e: e.matmul(pc[:], lhsT=lt8[:], rhs=glog[:], start=True, stop=True), [lt8, glog], [pc])
            op("act", lambda e: e.activation(out=ecum[:], in_=pc[:], func=AF.Exp), [pc], [ecum])
            op("act", lambda e: e.activation(out=encum[:], in_=pc[:], func=AF.Exp, scale=-1.0), [pc], [encum])
            op("dve", lambda e: e.tensor_tensor(out=ke[:], in0=kk[:], in1=encum[:], op=ALU.mult), [kk, encum], [ke])
            op("act", lambda e: e.activation(out=qh[:], in_=pre[:, 0, :], func=AF.Tanh, scale=0.5), [pre], [qh])
            op("dve", lambda e: e.scalar_tensor_tensor(out=qh[:], in0=qh[:], scalar=1.0, in1=pre[:, 0, :],
                                                       op0=ALU.add, op1=ALU.mult), [qh, pre], [qh])
            op("dve", lambda e: e.scalar_tensor_tensor(out=qe[:], in0=qh[:], scalar=0.0625, in1=ecum[:],
                                                       op0=ALU.mult, op1=ALU.mult), [qh, ecum], [qe])
            op("act", lambda e: e.copy(out=v_bf[:], in_=pre[:, 2, :]), [pre], [v_bf])
            op("act", lambda e: e.activation(out=gsil[:], in_=pre[:, 3, :], func=AF.Tanh, scale=0.5), [pre], [gsil])
            op("dve", lambda e: e.scalar_tensor_tensor(out=gsil[:], in0=gsil[:], scalar=1.0, in1=pre[:, 3, :],
                                                       op0=ALU.add, op1=ALU.mult), [gsil, pre], [gsil])
            pe_ = ps_a[1]
            for h in range(8):
                op("pe", lambda e, h=h: e.matmul(pe_[0:64, h * 16:(h + 1) * 16], lhsT=glog[:, h * 64:(h + 1) * 64],
                                                 rhs=seqm[:], start=True, stop=True), [glog, seqm], [pe_])
            op("act", lambda e: e.activation(out=els[:].rearrange("p a b -> p (a b)"), in_=pe_[0:64, 0:128],
                                             func=AF.Exp), [pe_], [els])
            for src, dst, pt in ((qe, qeT, ps_t2[0]), (ke, keT, ps_t2[1])):
                for h in range(8):
                    op("pe", lambda e, h=h, src=src, pt=pt: e.transpose(
                        out=pt[0:64, h * 128:(h + 1) * 128], in_=src[:, h * 64:(h + 1) * 64], identity=ident[:]),
                       [src, ident], [pt])
                op("act", lambda e, dst=dst, pt=pt: e.copy(out=dst[:].rearrange("p a b -> p (a b)"),
                                                           in_=pt[0:64, :]), [pt], [dst])
            op("dve", lambda e: e.tensor_copy(
                out=qeTx[:].rearrange("p h (s x) -> p h s x", x=136)[:, :, :, 0:8],
                in_=qeT[:].rearrange("p h (s j) -> p h s j", j=8)), [qeT], [qeTx])
            for h in range(8):
                op("pe", lambda e, h=h: e.matmul(ps_w2[:, h * 128:(h + 1) * 128], lhsT=keT[:, h, :], rhs=qeT[:, h, :],
                                                 start=True, stop=True), [keT, qeT], [ps_w2])
            op("dve", lambda e: e.tensor_tensor(out=attT[:], in0=ps_w2[:], in1=mask8[:], op=ALU.mult),
               [ps_w2, mask8], [attT])
            po_ = ps_a[2]
            for h in range(8):
                osl = po_[:, h * 64:(h + 1) * 64]
                op("pe", lambda e, h=h, osl=osl: e.matmul(osl, lhsT=attT[:, h * 128:(h + 1) * 128],
                                                          rhs=v_bf[:, h * 64:(h + 1) * 64], start=True, stop=False),
                   [attT, v_bf], [po_])
                for sq_ in range(16):
                    op("pe", lambda e, h=h, osl=osl, sq_=sq_: e.matmul(
                        osl, lhsT=qeTx[:, h, sq_ * 128:(sq_ + 1) * 128], rhs=S0b[:, sq_, h, :],
                        start=False, stop=(sq_ == 15)), [qeTx, S0b], [po_])
            for h in range(8):
                op("dve", lambda e, h=h: e.tensor_tensor(
                    out=vex[:], in0=v_bf[:, h * 64:(h + 1) * 64].unsqueeze(1).to_broadcast([128, 16, 64]),
                    in1=seqmb[:].unsqueeze(2).to_broadcast([128, 16, 64]), op=ALU.mult), [v_bf, seqmb], [vex])
                for half in range(2):
                    op("pe", lambda e, h=h, half=half: e.matmul(
                        ps_w2[0:64, half * 512:(half + 1) * 512], lhsT=ke[:, h * 64:(h + 1) * 64],
                        rhs=vex[:, half * 8:(half + 1) * 8, :].rearrange("p s v -> p (s v)"),
                        start=True, stop=True), [ke, vex], [ps_w2])
                op("dve", lambda e, h=h: e.tensor_tensor(
                    out=Sn[:, :, h, :], in0=S0[:, :, h, :],
                    in1=ps_w2[0:64, :].rearrange("p (s v) -> p s v", s=16), op=ALU.add), [S0, ps_w2], [Sn])
                op("dve", lambda e, h=h: e.tensor_tensor(
                    out=Sn[:, :, h, :], in0=Sn[:, :, h, :],
                    in1=els[:, h, :].unsqueeze(2).to_broadcast([64, 16, 64]), op=ALU.mult), [Sn, els], [Sn])
            for sq_ in range(16):
                P.dma("sp", o_hg_s[sq_].rearrange("h k v -> k h v"), Sn[:, sq_, :, :], reads=[Sn], writes=[o_hg_s])
            op("act", lambda e: e.copy(out=o_sb[:], in_=po_[:]), [po_], [o_sb])
            op("dve", lambda e: e.tensor_tensor(out=o_sq[:], in0=o_sb[:], in1=o_sb[:], op=ALU.mult), [o_sb], [o_sq])
            op("dve", lambda e: e.reduce_sum(out=ssq[:], in_=o_sq[:].rearrange("p (h v) -> p h v", h=8), axis=AX.X),
               [o_sq], [ssq])
            op("dve", lambda e: e.tensor_scalar(out=ssq[:], in0=ssq[:], scalar1=1.0 / 64, scalar2=1e-6,
                                                op0=ALU.mult, op1=ALU.add), [ssq], [ssq])
            op("act", lambda e: e.sqrt(out=ssq[:], in_=ssq[:]), [ssq], [ssq])
            op("dve", lambda e: e.reciprocal(out=ssq[:], in_=ssq[:]), [ssq], [ssq])
            op("dve", lambda e: e.tensor_tensor(
                out=o_sq[:].rearrange("p (h v) -> p h v", h=8), in0=o_sb[:].rearrange("p (h v) -> p h v", h=8),
                in1=ssq[:].unsqueeze(2).to_broadcast([128, 8, 64]), op=ALU.mult), [o_sb, ssq], [o_sq])
            op("dve", lambda e: e.tensor_tensor(out=o_sq[:], in0=o_sq[:], in1=gn_t[:].rearrange("p h v -> p (h v)"),
                                                op=ALU.mult), [o_sq, gn_t], [o_sq])
            op("dve", lambda e: e.scalar_tensor_tensor(out=ob_bf[:], in0=o_sq[:], scalar=0.5, in1=gsil[:],
                                                       op0=ALU.mult, op1=ALU.mult), [o_sq, gsil], [ob_bf])
            P.dma("sp", mixed_d[SEQ:SEQ + 128, 512:1024], ob_bf[:], reads=[ob_bf], writes=[mixed_d])
            P.barrier()
          P.emit()

        dbg_idma("after0S")
        def out_proj_phase(tag, w_dram, mixed_src, xres_ap, xres_res, x_dst, ntiles):
            c_es = contextlib.ExitStack()
            with c_es:
                def TC(name, shape, dt=F32, psum=False):
                    return T(P, c_es, tag + name, shape, dt, psum)
                w_out = TC("w_out", [128, 8, D], BF16)
                load_w(w_out, w_dram.rearrange("(kc kp) n -> kp kc n", kp=128), 8)
                mx = [TC(f"mx{i}", [128, D], BF16) for i in range(2)]
                mxT = [TC(f"mxT{i}", [128, 8, 128], BF16) for i in range(2)]
                xr = [TC(f"xr{i}", [128, D]) for i in range(2)]
                x1t = [TC(f"x1t{i}", [128, D]) for i in range(2)]
                ps_tr = [TC(f"ps_tr{i}", [128, 1024], BF16, psum=True) for i in range(2)]
                ps_y = [TC(f"ps_y{i}", [128, 1024], F32, psum=True) for i in range(2)]
                for tt in range(ntiles):
                    b = tt % 2
                    P.dma("sp", mx[b][:], mixed_src[tt * 128:(tt + 1) * 128, :], reads=[mixed_src], writes=[mx[b]])
                    P.dma("sp", xr[b][:], xres_ap[tt * 128:(tt + 1) * 128, :],
                          reads=([xres_res] if xres_res is not None else []), writes=[xr[b]])
                    pt = ps_tr[b]
                    for c in range(8):
                        op("pe", lambda e, c=c, pt=pt, b=b: e.transpose(out=pt[:, c * 128:(c + 1) * 128],
                                                                        in_=mx[b][:, c * 128:(c + 1) * 128],
                                                                        identity=ident[:]), [mx[b], ident], [pt])
                    op("act", lambda e, pt=pt, b=b: e.copy(out=mxT[b][:].rearrange("p a b -> p (a b)"), in_=pt[:]),
                       [pt], [mxT[b]])
                    py = ps_y[b]
                    for half in range(2):
                        for kc in range(8):
                            op("pe", lambda e, kc=kc, half=half, py=py, b=b: e.matmul(
                                py[:, half * 512:(half + 1) * 512], lhsT=mxT[b][:, kc, :],
                                rhs=w_out[:, kc, half * 512:(half + 1) * 512], start=(kc == 0), stop=(kc == 7)),
                               [mxT[b], w_out], [py])
                    op("dve", lambda e, py=py, b=b: e.tensor_tensor(out=x1t[b][:], in0=py[:], in1=xr[b][:],
                                                                   op=ALU.add), [py, xr[b]], [x1t[b]])
                    P.dma("sp", x_dst[tt * 128:(tt + 1) * 128, :], x1t[b][:], reads=[x1t[b]], writes=[x_dst])
                P.barrier()
            P.emit()

        def ffn_phase(tag, wg_ap, wu_ap, wd_ap, g_row_ap, x_src, res_src, x_dst, groups, cw_src=None, ecol=0):
            d_es = contextlib.ExitStack()
            with d_es:
                def TD(name, shape, dt=F32, psum=False):
                    return T(P, d_es, tag + name, shape, dt, psum)
                wg = TD("wg", [128, 8, DFF], BF16)
                wu = TD("wu", [128, 8, DFF], BF16)
                wd = TD("wd", [128, NFF, D], BF16)
                load_w(wg, wg_ap.rearrange("(kc kp) n -> kp kc n", kp=128), 8)
                load_w(wu, wu_ap.rearrange("(kc kp) n -> kp kc n", kp=128), 8)
                load_w(wd, wd_ap.rearrange("(kc kp) n -> kp kc n", kp=128), NFF)
                gffn = TD("gffn", [128, D])
                P.dma("sp", gffn[:], g_row_ap.partition_broadcast(128), writes=[gffn])
                xg = TD("xg", [128, 4, D])
                rbuf = [TD(f"rbuf{i}", [128, 512]) for i in range(2)]
                cwg = TD("cwg", [128, 4, 8])
                hn = [TD(f"hn{i}", [128, D], BF16) for i in range(2)]
                hnTg = TD("hnTg", [128, 8, 512], BF16)
                sq = TD("sq", [128, D], BF16)
                ss = [TD(f"ss{i}", [128, 1]) for i in range(2)]
                rstd = [TD(f"rstd{i}", [128, 1]) for i in range(2)]
                hT = TD("hT", [128, NFF, 512], BF16)
                gs = [TD(f"gs{i}", [128, 512]) for i in range(2)]
                x2t = [TD(f"x2t{i}", [128, 512]) for i in range(2)]
                ps_tr = [TD(f"ps_tr{i}", [128, 1024], BF16, psum=True) for i in range(2)]
                ps_g = [TD(f"ps_g{i}", [128, 512], F32, psum=True) for i in range(2)]
                ps_u = [TD(f"ps_u{i}", [128, 512], F32, psum=True) for i in range(2)]
                ps_d = [TD(f"ps_d{i}", [128, 512], F32, psum=True) for i in range(2)]
                for tiles_g in groups:
                    ncol = len(tiles_g) * 128
                    for j, tt in enumerate(tiles_g):
                        P.dma("sp", xg[:, j, :], x_src[tt * 128:(tt + 1) * 128, :], reads=[x_src], writes=[xg])
                        if cw_src is not None:
                            P.dma("sp", cwg[:, j, :], cw_src[tt * 128:(tt + 1) * 128, :], reads=[cw_src],
                                  writes=[cwg])
                    for j, tt in enumerate(tiles_g):
                        b = j % 2
                        rmsnorm_T(xg[:, j, :], xg, gffn, hn[b], sq, ss[b], rstd[b], ps_tr[b],
                                  hnTg[:, :, j * 128:(j + 1) * 128], hnTg)
                    for fc in range(NFF):
                        pg, pu = ps_g[fc % 2], ps_u[fc % 2]
                        for kc in range(8):
                            op("pe", lambda e, kc=kc, fc=fc, pg=pg, ncol=ncol: e.matmul(
                                pg[:, 0:ncol], lhsT=wg[:, kc, fc * 128:(fc + 1) * 128], rhs=hnTg[:, kc, 0:ncol],
                                start=(kc == 0), stop=(kc == 7)), [wg, hnTg], [pg])
                        for kc in range(8):
                            op("pe", lambda e, kc=kc, fc=fc, pu=pu, ncol=ncol: e.matmul(
                                pu[:, 0:ncol], lhsT=wu[:, kc, fc * 128:(fc + 1) * 128], rhs=hnTg[:, kc, 0:ncol],
                                start=(kc == 0), stop=(kc == 7)), [wu, hnTg], [pu])
                        gsb = gs[fc % 2]
                        op("act", lambda e, pg=pg, gsb=gsb, ncol=ncol: e.activation(out=gsb[:, 0:ncol], in_=pg[:, 0:ncol], func=AF.Tanh,
                                                                         scale=0.5), [pg], [gsb])
                        op("dve", lambda e, pg=pg, gsb=gsb, ncol=ncol: e.scalar_tensor_tensor(
                            out=gsb[:, 0:ncol], in0=gsb[:, 0:ncol], scalar=1.0, in1=pg[:, 0:ncol], op0=ALU.add,
                            op1=ALU.mult),
                           [gsb, pg], [gsb])
                        op("dve", lambda e, pu=pu, gsb=gsb, fc=fc, ncol=ncol: e.scalar_tensor_tensor(
                            out=hT[:, fc, 0:ncol], in0=gsb[:, 0:ncol], scalar=0.5, in1=pu[:, 0:ncol], op0=ALU.mult,
                            op1=ALU.mult),
                           [gsb, pu], [hT])
                    for j, tt in enumerate(tiles_g):
                        for half in range(2):
                            pd = ps_d[(2 * j + half) % 2]
                            for fc in range(NFF):
                                op("pe", lambda e, fc=fc, j=j, half=half, pd=pd: e.matmul(
                                    pd[:], lhsT=hT[:, fc, j * 128:(j + 1) * 128],
                                    rhs=wd[:, fc, half * 512:(half + 1) * 512],
                                    start=(fc == 0), stop=(fc == NFF - 1)), [hT, wd], [pd])
                            xo = x2t[(2 * j + half) % 2]
                            rb_ = rbuf[(2 * j + half) % 2]
                            P.dma("sp", rb_[:], res_src[tt * 128:(tt + 1) * 128, half * 512:(half + 1) * 512],
                                  reads=[res_src], writes=[rb_])
                            if cw_src is None:
                                op("dve", lambda e, pd=pd, xo=xo, rb_=rb_: e.tensor_tensor(
                                    out=xo[:], in0=pd[:], in1=rb_[:], op=ALU.add), [pd, rb_], [xo])
                            else:
                                op("dve", lambda e, pd=pd, xo=xo, j=j, rb_=rb_: e.scalar_tensor_tensor(
                                    out=xo[:], in0=pd[:], scalar=cwg[:, j, ecol:ecol + 1],
                                    in1=rb_[:], op0=ALU.mult, op1=ALU.add), [pd, rb_, cwg], [xo])
                            P.dma("sp", x_dst[tt * 128:(tt + 1) * 128, half * 512:(half + 1) * 512], xo[:],
                                  reads=[xo], writes=[x_dst])
                P.barrier()
            P.emit()

        SMP = os.environ.get("KSMP", "1") == "1"
        TGROUPS = [[4 * g + j for j in range(4)] for g in range(NT // 4)] + ([[NT]] if SMP else [])
        NTS = NT + (1 if SMP else 0)

        class _SubDR:
            def __init__(self, base, r0):
                self.ap = base.ap[r0:, :]
                self.res = base.res

            def __getitem__(self, k):
                return self.ap[k]
        out_proj_phase("c_", w_out_even, mixed_d, x_seq, None, x1_d, NT)
        if os.environ.get("KSMP", "1") == "1":
            out_proj_phase("cs_", w_out_even, DR(mixed_d.ap[SEQ:SEQ + 128, :], "mx_s") if False else _SubDR(mixed_d, SEQ),
                           x_smp, None, _SubDR(x1_d, SEQ), 1)
        if MAXPH < 3:
            raise _Stop()
        ffn_phase("d_", ffn_wg, ffn_wu, ffn_wd, norm_ffn[0:1, :], x1_d, x1_d, x2_d, TGROUPS)
        if MAXPH < 4:
            raise _Stop()

        e_es = contextlib.ExitStack()
        with e_es:
            def TE(name, shape, dt=F32, psum=False):
                return T(P, e_es, name, shape, dt, psum)
            w_io = TE("w_io", [128, 8, 2048], BF16)
            load_w(w_io, w_in_odd.rearrange("(kc kp) n -> kp kc n", kp=128), 8)
            gmix1 = TE("gmix1", [128, D])
            P.dma("sp", gmix1[:], norm_mix[1:2, :].partition_broadcast(128), writes=[gmix1])
            xt = [TE(f"e_xt{i}", [128, D]) for i in range(2)]
            hn = [TE(f"e_hn{i}", [128, D], BF16) for i in range(2)]
            hnT = [TE(f"e_hnT{i}", [128, 8, 128], BF16) for i in range(2)]
            sq = TE("e_sq", [128, D], BF16)
            ss = [TE(f"e_ss{i}", [128, 1]) for i in range(2)]
            rstd = [TE(f"e_rstd{i}", [128, 1]) for i in range(2)]
            kv_sb = [TE(f"e_kv{i}", [128, 1024]) for i in range(2)]
            u_sb = [TE(f"e_u{i}", [128, 512]) for i in range(2)]
            vc_sb = [TE(f"e_vc{i}", [128, 4, 130], BF16) for i in range(2)]
            for i in range(2):
                op("dve", lambda e, i=i: e.memset(vc_sb[i][:], 1.0), [], [vc_sb[i]])
            fT_sb = [TE(f"e_fT{i}", [128, 4, 128], BF16) for i in range(6)]
            ps_tr = [TE(f"e_ps_tr{i}", [128, 1024], BF16, psum=True) for i in range(2)]
            ps_mm = [TE(f"e_ps_mm{i}", [128, 512], F32, psum=True) for i in range(4)]
            for tt in range(NTS):
                b = tt % 2
                P.dma("sp", xt[b][:], x2_d[tt * 128:(tt + 1) * 128, :], reads=[x2_d], writes=[xt[b]])
                rmsnorm_T(xt[b][:], xt[b], gmix1, hn[b], sq, ss[b], rstd[b], ps_tr[b], hnT[b][:], hnT[b])
                kb = kv_sb[b]
                for j, col0 in enumerate((512, 1024, 1536)):
                    ps = ps_mm[j]
                    for kc in range(8):
                        op("pe", lambda e, kc=kc, ps=ps, col0=col0, b=b: e.matmul(
                            ps[:], lhsT=hnT[b][:, kc, :], rhs=w_io[:, kc, col0:col0 + 512],
                            start=(kc == 0), stop=(kc == 7)), [hnT[b], w_io], [ps])
                    if j == 0:
                        op("dve", lambda e, ps=ps, kb=kb: e.tensor_copy(out=kb[:, 0:512], in_=ps[:]), [ps], [kb])
                    elif j == 1:
                        op("act", lambda e, ps=ps, kb=kb: e.copy(out=kb[:, 512:1024], in_=ps[:]), [ps], [kb])
                        vs = vc_sb[b]
                        op("dve", lambda e, ps=ps, vs=vs: e.tensor_copy(
                            out=vs[:, :, 0:128], in_=ps[:].rearrange("p (h d) -> p h d", h=4)), [ps], [vs])
                        P.dma("sp", Vc_d[tt], vs[:].rearrange("p h d -> p (h d)"), reads=[vs], writes=[Vc_d])
                    else:
                        ub = u_sb[b]
                        op("act", lambda e, ps=ps, ub=ub: e.copy(out=ub[:], in_=ps[:]), [ps], [ub])
                        P.dma("sp", ud_d[tt * 128:(tt + 1) * 128, :], ub[:], reads=[ub], writes=[ud_d])
                if tt < NT:
                    P.dma("sp", o_ck[tt * 128:(tt + 1) * 128, :], kb[:, 0:512], reads=[kb], writes=[o_ck])
                    P.dma("sp", o_cv[tt * 128:(tt + 1) * 128, :], kb[:, 512:1024], reads=[kb], writes=[o_cv])
                else:
                    P.dma("sp", o_ck_s[:], kb[:, 0:512], reads=[kb], writes=[o_ck_s])
                    P.dma("sp", o_cv_s[:], kb[:, 512:1024], reads=[kb], writes=[o_cv_s])
                for wi, (col0, dst, scl) in enumerate(((0, q2T_d, 0.125), (512, k2T_d, 1.0), (1536, uT_d, 1.0))):
                    ps = ps_mm[3] if wi != 1 else ps_mm[0]
                    for pr in range(4):
                        for kc in range(8):
                            op("pe", lambda e, kc=kc, pr=pr, ps=ps, col0=col0, b=b: e.matmul(
                                ps[:, pr * 128:(pr + 1) * 128],
                                lhsT=w_io[:, kc, col0 + pr * 128:col0 + (pr + 1) * 128],
                                rhs=hnT[b][:, kc, :], start=(kc == 0), stop=(kc == 7)), [hnT[b], w_io], [ps])
                    fs = fT_sb[(tt % 2) * 3 + wi]
                    op("act", lambda e, ps=ps, fs=fs, scl=scl: e.mul(out=fs[:].rearrange("p a b -> p (a b)"),
                                                                     in_=ps[:], mul=scl), [ps], [fs])
                    for pr in range(4):
                        P.dma("sp", dst[pr, :, tt * 128:(tt + 1) * 128], fs[:, pr, :], reads=[fs], writes=[dst])
            P.barrier()
        P.emit()
        if MAXPH < 5:
            raise _Stop()

        dbg_idma("after1A")
        LAM_INIT = 0.8 - 0.6 * float(np.exp(-0.3 * 1))
        C_SLOPES = [2.0 ** (-2.0 * (h + 1)) for h in range(4)]
        f_es = contextlib.ExitStack()
        with f_es:
            def TF(name, shape, dt=F32, psum=False):
                return T(P, f_es, "f_" + name, shape, dt, psum)
            NC2 = (NT + 3) * 128
            dstrip2 = TF("dstrip2", [128, NC2])
            P.dma("sp", dstrip2[:], cst["dstrip2"], writes=[dstrip2])
            k2 = TF("k2", [128, 4, SEQ], BF16)
            for pr in range(4):
                P.dma("sp", k2[:, pr, :], k2T_d[pr, :, 0:SEQ], reads=[k2T_d], writes=[k2])
            vc = TF("vc", [128, NT, 520], BF16)
            for kt in range(NT):
                P.dma("sp", vc[:, kt, :], Vc_d[kt], reads=[Vc_d], writes=[vc])
            lqk = TF("lqk", [128, 4, 64])
            for i, src in enumerate((dlq1, dlk1, dlq2, dlk2)):
                P.dma("sp", lqk[:, i, :], src[0:1, :].partition_broadcast(128), writes=[lqk])
            lam2 = TF("lam2", [128, 2])
            lprod = TF("lprod", [128, 2, 64])
            op("dve", lambda e: e.tensor_tensor(out=lprod[:, 0, :], in0=lqk[:, 0, :], in1=lqk[:, 1, :], op=ALU.mult),
               [lqk], [lprod])
            op("dve", lambda e: e.tensor_tensor(out=lprod[:, 1, :], in0=lqk[:, 2, :], in1=lqk[:, 3, :], op=ALU.mult),
               [lqk, lprod], [lprod])
            op("dve", lambda e: e.reduce_sum(out=lam2[:], in_=lprod[:], axis=AX.X), [lprod], [lam2])
            op("act", lambda e: e.activation(out=lam2[:], in_=lam2[:], func=AF.Exp), [lam2], [lam2])
            nlam = TF("nlam", [128, 1])
            op("dve", lambda e: e.tensor_tensor(out=nlam[:], in0=lam2[:, 1:2], in1=lam2[:, 0:1], op=ALU.subtract),
               [lam2], [nlam])
            op("dve", lambda e: e.tensor_scalar(out=nlam[:], in0=nlam[:], scalar1=-LAM_INIT, scalar2=None,
                                                op0=ALU.add), [nlam], [nlam])
            subw = TF("subw", [128, 128])
            P.dma("sp", subw[:], dsubln[0:1, :].partition_broadcast(128), writes=[subw])
            op("dve", lambda e: e.tensor_scalar(out=subw[:], in0=subw[:], scalar1=1.0 - LAM_INIT, scalar2=None,
                                                op0=ALU.mult), [subw], [subw])
            qg = [TF(f"qg{i}", [128, 4, 512], BF16) for i in range(2)]
            tmp = [TF(f"tmp{i}", [128, 512]) for i in range(2)]
            PTb = [TF(f"PTb{i}", [128, NT, 512], BF16) for i in range(2)]
            rden = TF("rden", [128, 2, 4])
            o1 = TF("o1", [128, 4, 128])
            o2 = TF("o2", [128, 4, 128])
            osq = TF("osq", [128, 4, 128])
            ssq = TF("ssq", [128, 4])
            oc = [TF(f"oc{i}", [128, 4, 512], BF16) for i in range(2)]
            ps_s = [TF(f"ps_s{i}", [128, 512], F32, psum=True) for i in range(3)]
            ps_o = [[TF(f"ps_o{m}{i}", [128, 2, 256], F32, psum=True) for i in range(2)] for m in range(2)]
            blk = 0
            for Q in range(NT // 4):
                qb_ = qg[Q % 2]
                for pr in range(4):
                    P.dma("sp", qb_[:, pr, :], q2T_d[pr, :, Q * 512:(Q + 1) * 512], reads=[q2T_d], writes=[qb_])
                ocb = oc[Q % 2]
                kts = list(range(0, 4 * Q + 4))
                for h in range(4):
                    for m in range(2):
                        ptb = PTb[m]
                        r0 = m * 64
                        for kt in kts:
                            c0 = (4 * Q - kt + 3) * 128
                            ps = ps_s[blk % 3]
                            tm = tmp[blk % 2]
                            blk += 1
                            op("pe", lambda e, ps=ps, kt=kt, h=h, r0=r0, qb_=qb_: e.matmul(
                                ps[:], lhsT=k2[r0:r0 + 64, h, kt * 128:(kt + 1) * 128],
                                rhs=qb_[r0:r0 + 64, h, :], start=True, stop=True), [k2, qb_], [ps])
                            op("dve", lambda e, ps=ps, tm=tm, c0=c0, h=h: e.scalar_tensor_tensor(
                                out=tm[:], in0=dstrip2[:, c0:c0 + 512], scalar=-C_SLOPES[h], in1=ps[:],
                                op0=ALU.mult, op1=ALU.add), [dstrip2, ps], [tm])
                            op("act", lambda e, tm=tm, ptb=ptb, kt=kt: e.activation(out=ptb[:, kt, :], in_=tm[:],
                                                                                   func=AF.Exp), [tm], [ptb])
                        for j in range(4):
                            po = ps_o[m][j // 2]
                            nk = 4 * Q + j + 1
                            for kt in range(nk):
                                op("pe", lambda e, j=j, ptb=ptb, po=po, kt=kt, h=h, nk=nk: e.matmul(
                                    po[:, j % 2, 0:130], lhsT=ptb[:, kt, j * 128:(j + 1) * 128],
                                    rhs=vc[:, kt, h * 130:(h + 1) * 130],
                                    start=(kt == 0), stop=(kt == nk - 1)), [ptb, vc], [po])
                    for m in range(2):
                        for jj in range(2):
                            po = ps_o[m][jj]
                            op("dve", lambda e, po=po, m=m, jj=jj: e.reciprocal(
                                out=rden[:, m, 2 * jj:2 * jj + 2], in_=po[:, :, 128]), [po], [rden])
                    op("dve", lambda e: e.tensor_scalar(out=rden[:, 1, :], in0=rden[:, 1, :], scalar1=nlam[:, 0:1],
                                                        scalar2=None, op0=ALU.mult), [rden, nlam], [rden])
                    for m, od in ((0, o1), (1, o2)):
                        for jj in range(2):
                            po = ps_o[m][jj]
                            op("dve", lambda e, po=po, m=m, jj=jj, od=od: e.tensor_tensor(
                                out=od[:, 2 * jj:2 * jj + 2, :], in0=po[:, :, 0:128],
                                in1=rden[:, m, 2 * jj:2 * jj + 2].unsqueeze(2).to_broadcast([128, 2, 128]),
                                op=ALU.mult), [po, rden], [od])
                    op("dve", lambda e: e.tensor_tensor(out=o1[:], in0=o1[:], in1=o2[:], op=ALU.add), [o1, o2], [o1])
                    op("dve", lambda e: e.tensor_tensor(out=osq[:], in0=o1[:], in1=o1[:], op=ALU.mult), [o1], [osq])
                    op("dve", lambda e: e.reduce_sum(out=ssq[:], in_=osq[:], axis=AX.X), [osq], [ssq])
                    op("dve", lambda e: e.tensor_scalar(out=ssq[:], in0=ssq[:], scalar1=1.0 / 128, scalar2=1e-6,
                                                        op0=ALU.mult, op1=ALU.add), [ssq], [ssq])
                    op("act", lambda e: e.sqrt(out=ssq[:], in_=ssq[:]), [ssq], [ssq])
                    op("dve", lambda e: e.reciprocal(out=ssq[:], in_=ssq[:]), [ssq], [ssq])
                    op("dve", lambda e: e.tensor_tensor(out=osq[:], in0=o1[:],
                                                        in1=ssq[:].unsqueeze(2).to_broadcast([128, 4, 128]),
                                                        op=ALU.mult), [o1, ssq], [osq])
                    op("dve", lambda e, h=h, ocb=ocb: e.tensor_tensor(
                        out=ocb[:, :, h * 128:(h + 1) * 128], in0=osq[:],
                        in1=subw[:].unsqueeze(1).to_broadcast([128, 4, 128]), op=ALU.mult), [osq, subw], [ocb])
                for j in range(4):
                    t0 = (4 * Q + j) * 128
                    P.dma("sp", mixed2_d[t0:t0 + 128, 0:512], ocb[:, j, :], reads=[ocb], writes=[mixed2_d])
            P.barrier()
        P.emit()
        if MAXPH < 6:
            raise _Stop()

        dbg_idma("after1B")
        if SMP:
          t_es = contextlib.ExitStack()
          with t_es:
            def TT(name, shape, dt=F32, psum=False):
                return T(P, t_es, "t_" + name, shape, dt, psum)
            ident_f = TT("ident_f", [128, 128])
            P.dma("sp", ident_f[:], cst["ident_f"], writes=[ident_f])
            bias_c = TT("bias_c", [128, 33, 64])
            P.dma("sp", bias_c[:].rearrange("p a b -> p (a b)"), cst["bias_c"], writes=[bias_c])
            kidx = TT("kidx", [128, 1])
            P.dma("sp", kidx[:], cst["kidx"], writes=[kidx])
            pti = TT("pti", [128, 256], I32); ptf = TT("ptf", [128, 256]); idxi = TT("idxi", [128, 256], I32)
            P.dma("sp", pti[:], ptab[0:1, :].partition_broadcast(128), writes=[pti])
            op("dve", lambda e: e.tensor_copy(out=ptf[:], in_=pti[:]), [pti], [ptf])
            op("dve", lambda e: e.tensor_scalar(out=ptf[:], in0=ptf[:], scalar1=128.0, scalar2=kidx[:, 0:1],
                                                op0=ALU.mult, op1=ALU.add), [ptf, kidx], [ptf])
            op("dve", lambda e: e.tensor_copy(out=idxi[:], in_=ptf[:]), [ptf], [idxi])
            lqk = TT("lqk", [128, 4, 64])
            for i, src_ in enumerate((dlq1, dlk1, dlq2, dlk2)):
                P.dma("sp", lqk[:, i, :], src_[0:1, :].partition_broadcast(128), writes=[lqk])
            lam2 = TT("lam2", [128, 2]); lprod = TT("lprod", [128, 2, 64]); nlam = TT("nlam", [128, 1])
            op("dve", lambda e: e.tensor_tensor(out=lprod[:, 0, :], in0=lqk[:, 0, :], in1=lqk[:, 1, :], op=ALU.mult),
               [lqk], [lprod])
            op("dve", lambda e: e.tensor_tensor(out=lprod[:, 1, :], in0=lqk[:, 2, :], in1=lqk[:, 3, :], op=ALU.mult),
               [lqk, lprod], [lprod])
            op("dve", lambda e: e.reduce_sum(out=lam2[:], in_=lprod[:], axis=AX.X), [lprod], [lam2])
            op("act", lambda e: e.activation(out=lam2[:], in_=lam2[:], func=AF.Exp), [lam2], [lam2])
            op("dve", lambda e: e.tensor_tensor(out=nlam[:], in0=lam2[:, 1:2], in1=lam2[:, 0:1], op=ALU.subtract),
               [lam2], [nlam])
            op("dve", lambda e: e.tensor_scalar(out=nlam[:], in0=nlam[:], scalar1=-LAM_INIT, scalar2=None,
                                                op0=ALU.add), [nlam], [nlam])
            subw = TT("subw", [128, 128])
            P.dma("sp", subw[:], dsubln[0:1, :].partition_broadcast(128), writes=[subw])
            op("dve", lambda e: e.tensor_scalar(out=subw[:], in0=subw[:], scalar1=1.0 - LAM_INIT, scalar2=None,
                                                op0=ALU.mult), [subw], [subw])
            q2n = TT("q2n", [128, 4, 128], BF16); k2n = TT("k2n", [128, 4, 128], BF16)
            for pr in range(4):
                P.dma("sp", q2n[:, pr, :], q2T_d[pr, :, SEQ:SEQ + 128], reads=[q2T_d], writes=[q2n])
                P.dma("sp", k2n[:, pr, :], k2T_d[pr, :, SEQ:SEQ + 128], reads=[k2T_d], writes=[k2n])
            vC = [TT(f"vC{i}", [128, 17, 520], BF16) for i in range(2)]
            for i in range(2):
                op("dve", lambda e, i=i: e.memset(vC[i][:], 1.0), [], [vC[i]])
                P.dma("sp", vC[i][:, 16, :], Vc_d[NT], reads=[Vc_d], writes=[vC[i]])
            kpg = [TT(f"kpg{i}", [128, 512]) for i in range(2)]
            vpg = [TT(f"vpg{i}", [128, 512]) for i in range(2)]
            k2c = [TT(f"k2c{i}", [128, 4, 128], BF16) for i in range(2)]
            tmpc = [TT(f"tmpc{i}", [128, 64]) for i in range(2)]
            PTc = [TT(f"PTc{i}", [128, 17, 64], BF16) for i in range(2)]
            rdc = TT("rdc", [8, 4, 2]); oc1 = TT("oc1", [8, 4, 128]); oc2 = TT("oc2", [8, 4, 128])
            osq = TT("osq", [8, 4, 128]); ssq = TT("ssq", [8, 4])
            ocs = [TT(f"ocs{i}", [8, 4, 128], BF16) for i in range(2)]
            ps_kt = [TT(f"ps_kt{i}", [128, 512], F32, psum=True) for i in range(2)]
            ps_sc = [TT(f"ps_sc{i}", [128, 64], F32, psum=True) for i in range(2)]
            ps_po = [TT(f"ps_po{i}", [128, 2, 256], F32, psum=True) for i in range(4)]
            it = 0
            for sq_ in range(16):
                vb, ptb = vC[sq_ % 2], PTc[sq_ % 2]
                for j in range(17):
                    b = it % 2
                    it += 1
                    pss, tm = ps_sc[b], tmpc[b]
                    if j < 16:
                        col = sq_ * 16 + j
                        P.idma(kpg[b][:], pool_k[:, :], idxi[:, col:col + 1], 2560 * 128, reads=[idxi],
                               writes=[kpg[b]])
                        P.idma(vpg[b][:], pool_v[:, :], idxi[:, col:col + 1], 2560 * 128, reads=[idxi],
                               writes=[vpg[b]])
                        op("dve", lambda e, b=b, vb=vb, j=j: e.tensor_copy(
                            out=vb[:, j, :].rearrange("p (h d) -> p h d", d=130)[:, :, 0:128],
                            in_=vpg[b][:].rearrange("p (h d) -> p h d", d=128)), [vpg[b]], [vb])
                        pk = ps_kt[b]
                        for c4 in range(4):
                            op("pe", lambda e, c4=c4, pk=pk, b=b: e.transpose(
                                out=pk[:, c4 * 128:(c4 + 1) * 128], in_=kpg[b][:, c4 * 128:(c4 + 1) * 128],
                                identity=ident_f[:]), [kpg[b], ident_f], [pk])
                        kt_ = k2c[b]
                        op("act", lambda e, pk=pk, kt_=kt_: e.copy(out=kt_[:].rearrange("p a b -> p (a b)"),
                                                                   in_=pk[:]), [pk], [kt_])
                        bi = j
                    else:
                        kt_ = k2n
                        bi = 16 + sq_
                    for h in range(4):
                        for m in range(2):
                            g8 = (h * 2 + m) * 8
                            op("pe", lambda e, h=h, m=m, g8=g8, pss=pss, kt_=kt_, sq_=sq_: e.matmul(
                                pss[:, g8:g8 + 8], lhsT=kt_[m * 64:(m + 1) * 64, h, :],
                                rhs=q2n[m * 64:(m + 1) * 64, h, sq_ * 8:(sq_ + 1) * 8], start=True, stop=True),
                               [kt_, q2n], [pss])
                    op("dve", lambda e, pss=pss, tm=tm, bi=bi: e.tensor_tensor(
                        out=tm[:], in0=pss[:], in1=bias_c[:, bi, :], op=ALU.add), [pss, bias_c], [tm])
                    op("act", lambda e, tm=tm, ptb=ptb, j=j: e.activation(out=ptb[:, j, :], in_=tm[:], func=AF.Exp),
                       [tm], [ptb])
                for h in range(4):
                    po = ps_po[h]
                    for m in range(2):
                        g8 = (h * 2 + m) * 8
                        for j in range(17):
                            op("pe", lambda e, h=h, m=m, g8=g8, j=j, po=po, ptb=ptb, vb=vb: e.matmul(
                                po[0:8, m, 0:130], lhsT=ptb[:, j, g8:g8 + 8], rhs=vb[:, j, h * 130:(h + 1) * 130],
                                start=(j == 0), stop=(j == 16)), [ptb, vb], [po])
                for h in range(4):
                    po = ps_po[h]
                    op("dve", lambda e, po=po, h=h: e.reciprocal(out=rdc[:, h, :], in_=po[0:8, :, 128]), [po], [rdc])
                op("dve", lambda e: e.tensor_scalar(out=rdc[:, :, 1], in0=rdc[:, :, 1], scalar1=nlam[0:8, 0:1],
                                                    scalar2=None, op0=ALU.mult), [rdc, nlam], [rdc])
                for h in range(4):
                    po = ps_po[h]
                    op("dve", lambda e, po=po, h=h: e.tensor_scalar(out=oc1[:, h, :], in0=po[0:8, 0, 0:128],
                                                                    scalar1=rdc[:, h, 0:1], scalar2=None,
                                                                    op0=ALU.mult), [po, rdc], [oc1])
                    op("dve", lambda e, po=po, h=h: e.tensor_scalar(out=oc2[:, h, :], in0=po[0:8, 1, 0:128],
                                                                    scalar1=rdc[:, h, 1:2], scalar2=None,
                                                                    op0=ALU.mult), [po, rdc], [oc2])
                op("dve", lambda e: e.tensor_tensor(out=oc1[:], in0=oc1[:], in1=oc2[:], op=ALU.add), [oc1, oc2], [oc1])
                op("dve", lambda e: e.tensor_tensor(out=osq[:], in0=oc1[:], in1=oc1[:], op=ALU.mult), [oc1], [osq])
                op("dve", lambda e: e.reduce_sum(out=ssq[:], in_=osq[:], axis=AX.X), [osq], [ssq])
                op("dve", lambda e: e.tensor_scalar(out=ssq[:], in0=ssq[:], scalar1=1.0 / 128, scalar2=1e-6,
                                                    op0=ALU.mult, op1=ALU.add), [ssq], [ssq])
                op("act", lambda e: e.sqrt(out=ssq[:], in_=ssq[:]), [ssq], [ssq])
                op("dve", lambda e: e.reciprocal(out=ssq[:], in_=ssq[:]), [ssq], [ssq])
                op("dve", lambda e: e.tensor_tensor(out=osq[:], in0=oc1[:],
                                                    in1=ssq[:].unsqueeze(2).to_broadcast([8, 4, 128]), op=ALU.mult),
                   [oc1, ssq], [osq])
                ob_ = ocs[sq_ % 2]
                op("dve", lambda e, ob_=ob_: e.tensor_tensor(
                    out=ob_[:], in0=osq[:], in1=subw[0:8, :].unsqueeze(1).to_broadcast([8, 4, 128]), op=ALU.mult),
                   [osq, subw], [ob_])
                P.dma("sp", mixed2_d[SEQ + sq_ * 8:SEQ + sq_ * 8 + 8, 0:512], ob_[:].rearrange("p h d -> p (h d)"),
                      reads=[ob_], writes=[mixed2_d])
            P.barrier()
          P.emit()

        TWO_PI = 2.0 * np.pi
        g_es = contextlib.ExitStack()
        with g_es:
            def TG(name, shape, dt=F32, psum=False):
                return T(P, g_es, "g_" + name, shape, dt, psum)
            idx1 = TG("idx1", [128, 1]); nidx1 = TG("nidx1", [128, 1])
            P.dma("sp", idx1[:], cst["idx1"], writes=[idx1])
            P.dma("sp", nidx1[:], cst["nidx1"], writes=[nidx1])
            lt128 = TG("lt128", [128, 128]); sel127 = TG("sel127", [128, 128]); gmask = TG("gmask", [128, 8])
            ident_f = TG("ident_f", [128, 128])
            P.dma("sp", lt128[:], cst["lt128"], writes=[lt128])
            P.dma("sp", sel127[:], cst["sel127"], writes=[sel127])
            P.dma("sp", gmask[:], cst["gmask"], writes=[gmask])
            P.dma("sp", ident_f[:], cst["ident_f"], writes=[ident_f])
            are_b = TG("are_b", [128, 32, 64]); aim_b = TG("aim_b", [128, 32, 64]); dt_b = TG("dt_b", [128, 32])
            P.dma("sp", are_b[:].rearrange("p g q -> p (g q)"),
                  s5_a_re.rearrange("g q -> (g q)").partition_broadcast(128), writes=[are_b])
            P.dma("sp", aim_b[:].rearrange("p g q -> p (g q)"),
                  s5_a_im.rearrange("g q -> (g q)").partition_broadcast(128), writes=[aim_b])
            P.dma("sp", dt_b[:], s5_log_dt[0:1, :].partition_broadcast(128), writes=[dt_b])
            op("act", lambda e: e.activation(out=dt_b[:], in_=dt_b[:], func=AF.Exp), [dt_b], [dt_b])
            dtb3 = dt_b[:].unsqueeze(2).to_broadcast([128, 32, 64])
            op("dve", lambda e: e.tensor_tensor(out=are_b[:], in0=are_b[:], in1=dtb3, op=ALU.mult),
               [are_b, dt_b], [are_b])
            op("dve", lambda e: e.tensor_tensor(out=aim_b[:], in0=aim_b[:], in1=dtb3, op=ALU.mult),
               [aim_b, dt_b], [aim_b])
            PNr = TG("PNr", [128, 32, 64]); PNi = TG("PNi", [128, 32, 64])
            PPr = TG("PPr", [128, 32, 64]); PPi = TG("PPi", [128, 32, 64])
            magn = TG("magn", [128, 32, 64]); ang = TG("ang", [128, 32, 64])
            ki32 = TG("ki32", [128, 32, 64], I32)
            kf32 = TG("kf32", [128, 32, 64])
            idx8 = TG("idx8", [128, 1]); nidx8 = TG("nidx8", [128, 1])
            P.dma("sp", idx8[:], cst["idx8"], writes=[idx8])
            P.dma("sp", nidx8[:], cst["nidx8"], writes=[nidx8])

            def sin_of(dst, x_t, shape_tiles):
                ki, kf = shape_tiles
                op("dve", lambda e: e.tensor_scalar(out=ki[:], in0=x_t[:], scalar1=float(1.0 / TWO_PI), scalar2=None,
                                                    op0=ALU.mult), [x_t], [ki])
                op("dve", lambda e: e.tensor_copy(out=kf[:], in_=ki[:]), [ki], [kf])
                op("dve", lambda e: e.scalar_tensor_tensor(out=x_t[:], in0=kf[:], scalar=float(-TWO_PI), in1=x_t[:],
                                                           op0=ALU.mult, op1=ALU.add), [kf, x_t], [x_t])
                op("dve", lambda e: e.tensor_scalar(out=kf[:], in0=x_t[:], scalar1=float(np.pi), scalar2=None,
                                                    op0=ALU.is_gt), [x_t], [kf])
                op("dve", lambda e: e.scalar_tensor_tensor(out=x_t[:], in0=kf[:], scalar=float(-TWO_PI), in1=x_t[:],
                                                           op0=ALU.mult, op1=ALU.add), [kf, x_t], [x_t])
                op("dve", lambda e: e.tensor_scalar(out=kf[:], in0=x_t[:], scalar1=float(-np.pi), scalar2=None,
                                                    op0=ALU.is_lt), [x_t], [kf])
                op("dve", lambda e: e.scalar_tensor_tensor(out=x_t[:], in0=kf[:], scalar=float(TWO_PI), in1=x_t[:],
                                                           op0=ALU.mult, op1=ALU.add), [kf, x_t], [x_t])
                op("act", lambda e: e.activation(out=dst[:], in_=x_t[:], func=AF.Sin), [x_t], [dst])

            def build_tables(idx_t, nidx_t):
                op("act", lambda e: e.activation(out=magn[:], in_=are_b[:], func=AF.Exp, scale=nidx_t[:, 0:1]),
                   [are_b, nidx_t], [magn])
                op("act", lambda e: e.activation(out=PPr[:], in_=are_b[:], func=AF.Exp, scale=idx_t[:, 0:1]),
                   [are_b, idx_t], [PPr])
                for shift, dst in ((0.0, PNi), (0.5 * np.pi, PNr)):
                    op("dve", lambda e, shift=shift: e.tensor_scalar(out=ang[:], in0=aim_b[:], scalar1=idx_t[:, 0:1],
                                                                     scalar2=float(shift), op0=ALU.mult, op1=ALU.add),
                       [aim_b, idx_t], [ang])
                    sin_of(dst, ang, (ki32, kf32))
                op("dve", lambda e: e.tensor_tensor(out=PPi[:], in0=PPr[:], in1=PNi[:], op=ALU.mult), [PPr, PNi], [PPi])
                op("dve", lambda e: e.tensor_tensor(out=PPr[:], in0=PPr[:], in1=PNr[:], op=ALU.mult), [PPr, PNr], [PPr])
                op("dve", lambda e: e.tensor_tensor(out=PNr[:], in0=magn[:], in1=PNr[:], op=ALU.mult), [magn, PNr], [PNr])
                op("dve", lambda e: e.scalar_tensor_tensor(out=PNi[:], in0=magn[:], scalar=-1.0, in1=PNi[:],
                                                           op0=ALU.mult, op1=ALU.mult), [magn, PNi], [PNi])

            build_tables(idx1, nidx1)
            BB = TG("BB", [128, 8, 512], BF16)
            Cm = TG("Cm", [128, 32, 16], BF16)
            pa = contextlib.ExitStack()
            with pa:
                def TP(name, shape, dt=F32, psum=False):
                    return T(P, pa, "gp_" + name, shape, dt, psum)
                ar = TP("ar", [128, 32]); ai = TP("ai", [128, 32]); dtp = TP("dtp", [128, 32])
                bre = TP("bre", [128, 32, 16]); bim = TP("bim", [128, 32, 16])
                for half in range(2):
                    sl = slice(half * 64, (half + 1) * 64)
                    P.dma("sp", ar[sl, :], s5_a_re.rearrange("g q -> q g"), writes=[ar], slow=True)
                    P.dma("sp", ai[sl, :], s5_a_im.rearrange("g q -> q g"), writes=[ai], slow=True)
                    P.dma("sp", bre[sl, :, :], s5_b_re.rearrange("g q c -> q g c"), writes=[bre])
                    P.dma("sp", bim[sl, :, :], s5_b_im.rearrange("g q c -> q g c"), writes=[bim])
                P.dma("sp", dtp[:], s5_log_dt[0:1, :].partition_broadcast(128), writes=[dtp])
                op("act", lambda e: e.activation(out=dtp[:], in_=dtp[:], func=AF.Exp), [dtp], [dtp])
                mg = TP("mg", [128, 32]); th = TP("th", [128, 32]); sn = TP("sn", [128, 32]); cs = TP("cs", [128, 32])
                den = TP("den", [128, 32]); zr = TP("zr", [128, 32]); zi = TP("zi", [128, 32])
                t1 = TP("t1", [128, 32]); t2 = TP("t2", [128, 32])
                op("dve", lambda e: e.tensor_tensor(out=mg[:], in0=ar[:], in1=dtp[:], op=ALU.mult), [ar, dtp], [mg])
                op("act", lambda e: e.activation(out=mg[:], in_=mg[:], func=AF.Exp), [mg], [mg])
                op("dve", lambda e: e.tensor_tensor(out=th[:], in0=ai[:], in1=dtp[:], op=ALU.mult), [ai, dtp], [th])
                ki_s = TP("ki_s", [128, 32], I32)
                kf_s = TP("kf_s", [128, 32])
                for shift, dst in ((0.0, sn), (0.5 * np.pi, cs)):
                    op("dve", lambda e, shift=shift: e.tensor_scalar(out=t1[:], in0=th[:], scalar1=float(shift),
                                                                     scalar2=None, op0=ALU.add), [th], [t1])
                    sin_of(dst, t1, (ki_s, kf_s))
                op("dve", lambda e: e.tensor_tensor(out=cs[:], in0=cs[:], in1=mg[:], op=ALU.mult), [cs, mg], [cs])
                op("dve", lambda e: e.tensor_scalar(out=cs[:], in0=cs[:], scalar1=-1.0, scalar2=None, op0=ALU.add),
                   [cs], [cs])
                op("dve", lambda e: e.tensor_tensor(out=sn[:], in0=sn[:], in1=mg[:], op=ALU.mult), [sn, mg], [sn])
                op("dve", lambda e: e.tensor_tensor(out=den[:], in0=ar[:], in1=ar[:], op=ALU.mult), [ar], [den])
                op("dve", lambda e: e.tensor_tensor(out=t1[:], in0=ai[:], in1=ai[:], op=ALU.mult), [ai], [t1])
                op("dve", lambda e: e.tensor_tensor(out=den[:], in0=den[:], in1=t1[:], op=ALU.add), [den, t1], [den])
                op("dve", lambda e: e.reciprocal(out=den[:], in_=den[:]), [den], [den])
                op("dve", lambda e: e.tensor_tensor(out=t1[:], in0=cs[:], in1=ar[:], op=ALU.mult), [cs, ar], [t1])
                op("dve", lambda e: e.tensor_tensor(out=t2[:], in0=sn[:], in1=ai[:], op=ALU.mult), [sn, ai], [t2])
                op("dve", lambda e: e.tensor_tensor(out=zr[:], in0=t1[:], in1=t2[:], op=ALU.add), [t1, t2], [zr])
                op("dve", lambda e: e.tensor_tensor(out=zr[:], in0=zr[:], in1=den[:], op=ALU.mult), [zr, den], [zr])
                op("dve", lambda e: e.tensor_tensor(out=t1[:], in0=sn[:], in1=ar[:], op=ALU.mult), [sn, ar], [t1])
                op("dve", lambda e: e.tensor_tensor(out=t2[:], in0=cs[:], in1=ai[:], op=ALU.mult), [cs, ai], [t2])
                op("dve", lambda e: e.tensor_tensor(out=zi[:], in0=t1[:], in1=t2[:], op=ALU.subtract), [t1, t2], [zi])
                op("dve", lambda e: e.tensor_tensor(out=zi[:], in0=zi[:], in1=den[:], op=ALU.mult), [zi, den], [zi])
                Mb = TP("Mb", [128, 32, 16]); tb = TP("tb", [128, 32, 16])
                zr3 = zr[:].unsqueeze(2).to_broadcast([128, 32, 16])
                zi3 = zi[:].unsqueeze(2).to_broadcast([128, 32, 16])
                lo, hi = slice(0, 64), slice(64, 128)
                op("dve", lambda e: e.tensor_tensor(out=Mb[lo], in0=bre[lo], in1=zr3[lo], op=ALU.mult),
                   [bre, zr], [Mb])
                op("dve", lambda e: e.tensor_tensor(out=tb[lo], in0=bim[lo], in1=zi3[lo], op=ALU.mult),
                   [bim, zi], [tb])
                op("dve", lambda e: e.tensor_tensor(out=Mb[lo], in0=Mb[lo], in1=tb[lo], op=ALU.subtract),
                   [Mb, tb], [Mb])
                op("dve", lambda e: e.tensor_tensor(out=Mb[hi], in0=bim[hi], in1=zr3[hi], op=ALU.mult),
                   [bim, zr, Mb], [Mb])
                op("dve", lambda e: e.tensor_tensor(out=tb[hi], in0=bre[hi], in1=zi3[hi], op=ALU.mult),
                   [bre, zi, tb], [tb])
                op("dve", lambda e: e.tensor_tensor(out=Mb[hi], in0=Mb[hi], in1=tb[hi], op=ALU.add),
                   [Mb, tb], [Mb])
                MT = TP("MT", [128, 128])
                ps_t = TP("ps_t", [128, 128], F32, psum=True)
                Cst = TP("Cst", [128, 128])
                for ch in range(4):
                    op("pe", lambda e, ch=ch: e.transpose(
                        out=ps_t[:], in_=Mb[:, ch * 8:(ch + 1) * 8, :].rearrange("p g c -> p (g c)"),
                        identity=ident_f[:]), [Mb, ident_f], [ps_t])
                    op("act", lambda e: e.copy(out=MT[:], in_=ps_t[:]), [ps_t], [MT])
                    for gl in range(8):
                        n, j = ch * 2 + gl // 4, gl % 4
                        op("dve", lambda e, n=n, j=j, gl=gl: e.tensor_scalar(
                            out=BB[:, n, j * 128:(j + 1) * 128], in0=MT[:], scalar1=gmask[:, gl:gl + 1],
                            scalar2=None, op0=ALU.mult), [MT, gmask], [BB])
                    P.dma("sp", Cst[:, 0:64], s5_c_re[ch * 128:(ch + 1) * 128, :], writes=[Cst])
                    P.dma("sp", Cst[:, 64:128], s5_c_im[ch * 128:(ch + 1) * 128, :], writes=[Cst])
                    op("pe", lambda e: e.transpose(out=ps_t[:], in_=Cst[:], identity=ident_f[:]),
                       [Cst, ident_f], [ps_t])
                    op("act", lambda e, ch=ch: e.copy(
                        out=Cm[0:64, ch * 8:(ch + 1) * 8, :].rearrange("p g c -> p (g c)"), in_=ps_t[0:64, :]),
                       [ps_t], [Cm])
                    op("act", lambda e, ch=ch: e.mul(
                        out=Cm[64:128, ch * 8:(ch + 1) * 8, :].rearrange("p g c -> p (g c)"), in_=ps_t[64:128, :],
                        mul=-1.0), [ps_t, Cm], [Cm])
                P.barrier()
            wglu = TG("wglu", [128, 4, 512], BF16)
            load_w(wglu, s5_w_glu.rearrange("(kc kp) n -> kp kc n", kp=128), 4)
            bglu = TG("bglu", [128, 512]); dsk = TG("dsk", [128, 512])
            P.dma("sp", bglu[:], s5_b_glu[0:1, :].partition_broadcast(128), writes=[bglu])
            P.dma("sp", dsk[:], s5_d[0:1, :].partition_broadcast(128), writes=[dsk])
            uTt = [TG(f"uTt{i}", [128, 4, 128], BF16) for i in range(2)]
            utok = [TG(f"utok{i}", [128, 512]) for i in range(2)]
            hblk = [TG(f"hblk{n}", [128, 4, 2, 64]) for n in range(8)]
            Wb = [TG(f"Wb{i}", [128, 4, 2, 64]) for i in range(2)]
            tA = [TG(f"tA{i}", [128, 4, 2, 64]) for i in range(2)]
            tB = [TG(f"tB{i}", [128, 4, 2, 64]) for i in range(2)]
            hTb = [TG(f"hTb{i}", [128, 4, 128], BF16) for i in range(2)]
            ysb = TG("ysb", [128, 512]); zsb = TG("zsb", [128, 512]); z2 = TG("z2", [128, 512])
            zbf = TG("zbf", [128, 512], BF16); zT = TG("zT", [128, 4, 128], BF16)
            od_bf = [TG(f"od{i}", [128, 512], BF16) for i in range(2)]
            ps_bu = [TG(f"ps_bu{i}", [128, 4, 2, 64], F32, psum=True) for i in range(2)]
            ps_z = [TG(f"ps_z{i}", [128, 4, 2, 64], F32, psum=True) for i in range(2)]
            ps_hT = [TG(f"ps_hT{i}", [128, 512], F32, psum=True) for i in range(2)]
            ps_y = TG("ps_y", [128, 512], F32, psum=True)
            ps_zt = TG("ps_zt", [128, 1024], BF16, psum=True)

            def cmul(dst, tbl_r, tbl_i, src, n, tA_, tB_, src_res):
                tr = tbl_r[:, 4 * n:4 * n + 4, :].unsqueeze(2).to_broadcast([128, 4, 2, 64])
                ti = tbl_i[:, 4 * n:4 * n + 4, :]
                op("dve", lambda e: e.tensor_tensor(out=tA_[:], in0=src[:], in1=tr, op=ALU.mult),
                   [src_res, tbl_r], [tA_])
                op("dve", lambda e: e.tensor_tensor(out=tB_[:, :, 0, :], in0=src[:, :, 1, :], in1=ti, op=ALU.mult),
                   [src_res, tbl_i], [tB_])
                op("dve", lambda e: e.tensor_tensor(out=tB_[:, :, 1, :], in0=src[:, :, 0, :], in1=ti, op=ALU.mult),
                   [src_res, tbl_i, tB_], [tB_])
                op("dve", lambda e: e.tensor_tensor(out=dst[:, :, 0, :], in0=tA_[:, :, 0, :], in1=tB_[:, :, 0, :],
                                                    op=ALU.subtract), [tA_, tB_], [dst])
                op("dve", lambda e: e.tensor_tensor(out=dst[:, :, 1, :], in0=tA_[:, :, 1, :], in1=tB_[:, :, 1, :],
                                                    op=ALU.add), [tA_, tB_, dst], [dst])

            lt8s = TG("lt8s", [128, 128]); seqselT = TG("seqselT", [16, 128]); sel8 = TG("sel8", [128, 16])
            P.dma("sp", lt8s[:], cst["lt8"], writes=[lt8s])
            P.dma("sp", seqselT[:], cst["seqselT"], writes=[seqselT])
            P.dma("sp", sel8[:], cst["sel8"], writes=[sel8])
            h0 = TG("h0", [16, 32, 2, 64])
            P.dma("sp", h0[:, :, 0, :], st_s5r, writes=[h0])
            P.dma("sp", h0[:, :, 1, :], st_s5i, writes=[h0])
            hfin = TG("hfin", [16, 8, 512])
            for tt in list(range(NT)) + ([NT] if SMP else []):
                b = tt % 2
                smp = tt == NT
                if smp:
                    build_tables(idx8, nidx8)
                ltm = lt8s if smp else lt128
                for ch in range(4):
                    P.dma("sp", uTt[b][:, ch, :], uT_d[ch, :, tt * 128:(tt + 1) * 128], reads=[uT_d], writes=[uTt[b]])
                P.dma("sp", utok[b][:], ud_d[tt * 128:(tt + 1) * 128, :], reads=[ud_d], writes=[utok[b]])
                def s5_stageA(n):
                        pb, pz = ps_bu[n % 2], ps_z[n % 2]
                        wb, ta, tb_ = Wb[n % 2], tA[n % 2], tB[n % 2]
                        op("pe", lambda e, n=n, pb=pb, b=b: e.matmul(
                            pb[:].rearrange("p a r q -> p (a r q)"), lhsT=uTt[b][:, n // 2, :], rhs=BB[:, n, :],
                            start=True, stop=True), [uTt[b], BB], [pb])
                        cmul(wb, PNr, PNi, pb, n, ta, tb_, pb)

                def s5_stageB(n):
                        pb, pz = ps_bu[n % 2], ps_z[n % 2]
                        wb, ta, tb_ = Wb[n % 2], tA[n % 2], tB[n % 2]
                        op("pe", lambda e, pz=pz, wb=wb, tt=tt, ltm=ltm: e.matmul(
                            pz[:].rearrange("p a r q -> p (a r q)"), lhsT=ltm[:],
                            rhs=wb[:].rearrange("p a r q -> p (a r q)"), start=True, stop=(tt == 0)),
                           [ltm, wb], [pz])
                        if smp:
                            op("pe", lambda e, pz=pz, n=n: e.matmul(
                                pz[:].rearrange("p a r q -> p (a r q)"), lhsT=seqselT[:],
                                rhs=h0[:, 4 * n:4 * n + 4, :, :].rearrange("p a r q -> p (a r q)"),
                                start=False, stop=True), [seqselT, h0], [pz])
                        elif tt > 0:
                            op("pe", lambda e, pz=pz, n=n: e.matmul(
                                pz[:].rearrange("p a r q -> p (a r q)"), lhsT=sel127[:],
                                rhs=hblk[n][:].rearrange("p a r q -> p (a r q)"), start=False, stop=True),
                               [sel127, hblk[n]], [pz])
                        cmul(hblk[n], PPr, PPi, pz, n, ta, tb_, pz)
                        if smp:
                            pf = ps_bu[n % 2]
                            op("pe", lambda e, pf=pf, n=n: e.matmul(
                                pf[0:16].rearrange("p a r q -> p (a r q)"), lhsT=sel8[:],
                                rhs=hblk[n][:].rearrange("p a r q -> p (a r q)"), start=True, stop=True),
                               [sel8, hblk[n]], [pf])
                            op("act", lambda e, pf=pf, n=n: e.copy(out=hfin[:, n, :],
                                                                   in_=pf[0:16].rearrange("p a r q -> p (a r q)")),
                               [pf], [hfin])
                        ph = ps_hT[n % 2]
                        for j in range(4):
                            op("pe", lambda e, j=j, ph=ph, n=n: e.transpose(
                                out=ph[:, j * 128:(j + 1) * 128],
                                in_=hblk[n][:, j, :, :].rearrange("p r q -> p (r q)"), identity=ident_f[:]),
                               [hblk[n], ident_f], [ph])
                        hb = hTb[n % 2]
                        op("act", lambda e, ph=ph, hb=hb: e.copy(out=hb[:].rearrange("p a b -> p (a b)"), in_=ph[:]),
                           [ph], [hb])
                        for j in range(4):
                            g = 4 * n + j
                            op("pe", lambda e, j=j, g=g, hb=hb: e.matmul(
                                ps_y[:, g * 16:(g + 1) * 16], lhsT=hb[:, j, :], rhs=Cm[:, g, :], start=True, stop=True),
                               [hb, Cm], [ps_y])

                s5_stageA(0)
                for n in range(8):
                    if n + 1 < 8:
                        s5_stageA(n + 1)
                    s5_stageB(n)
                op("dve", lambda e, b=b: e.tensor_tensor(out=ysb[:], in0=utok[b][:], in1=dsk[:], op=ALU.mult),
                   [utok[b], dsk], [ysb])
                op("dve", lambda e: e.tensor_tensor(out=ysb[:], in0=ysb[:], in1=ps_y[:], op=ALU.add),
                   [ysb, ps_y], [ysb])
                op("dve", lambda e: e.tensor_tensor(out=z2[:], in0=ysb[:], in1=ysb[:], op=ALU.mult), [ysb], [z2])
                op("dve", lambda e: e.tensor_scalar(out=z2[:], in0=z2[:], scalar1=0.044715, scalar2=1.0,
                                                    op0=ALU.mult, op1=ALU.add), [z2], [z2])
                op("dve", lambda e: e.tensor_tensor(out=z2[:], in0=z2[:], in1=ysb[:], op=ALU.mult), [z2, ysb], [z2])
                op("act", lambda e: e.activation(out=z2[:], in_=z2[:], func=AF.Tanh, scale=0.7978845608028654),
                   [z2], [z2])
                op("dve", lambda e: e.scalar_tensor_tensor(out=zsb[:], in0=z2[:], scalar=1.0, in1=ysb[:],
                                                           op0=ALU.add, op1=ALU.mult), [z2, ysb], [zsb])
                op("dve", lambda e: e.tensor_scalar(out=zsb[:], in0=zsb[:], scalar1=0.5, scalar2=None,
                                                    op0=ALU.mult), [zsb], [zsb])
                op("act", lambda e: e.copy(out=zbf[:], in_=zsb[:]), [zsb], [zbf])
                for c in range(4):
                    op("pe", lambda e, c=c: e.transpose(out=ps_zt[:, c * 128:(c + 1) * 128],
                                                        in_=zbf[:, c * 128:(c + 1) * 128], identity=ident[:]),
                       [zbf, ident], [ps_zt])
                op("act", lambda e: e.copy(out=zT[:].rearrange("p a b -> p (a b)"), in_=ps_zt[:, 0:512]),
                   [ps_zt], [zT])
                pgl = ps_hT[0]
                for kc in range(4):
                    op("pe", lambda e, kc=kc, pgl=pgl: e.matmul(pgl[:], lhsT=zT[:, kc, :], rhs=wglu[:, kc, :],
                                                                start=(kc == 0), stop=(kc == 3)), [zT, wglu], [pgl])
                op("dve", lambda e, pgl=pgl: e.tensor_tensor(out=z2[:], in0=pgl[:], in1=bglu[:], op=ALU.add),
                   [pgl, bglu], [z2])
                op("act", lambda e: e.activation(out=z2[:], in_=z2[:], func=AF.Tanh, scale=0.5), [z2], [z2])
                op("dve", lambda e: e.scalar_tensor_tensor(out=z2[:], in0=z2[:], scalar=1.0, in1=zsb[:],
                                                           op0=ALU.add, op1=ALU.mult), [z2, zsb], [z2])
                ob = od_bf[b]
                op("dve", lambda e, ob=ob: e.tensor_scalar(out=ob[:], in0=z2[:], scalar1=0.5, scalar2=None,
                                                           op0=ALU.mult), [z2], [ob])
                P.dma("sp", mixed2_d[tt * 128:(tt + 1) * 128, 512:1024], ob[:], reads=[ob], writes=[mixed2_d])
                if tt == NT - 1:
                    for n in range(8):
                        P.dma("sp", o_s5r[4 * n:4 * n + 4, :], hblk[n][127:128, :, 0, :], reads=[hblk[n]],
                              writes=[o_s5r])
                        P.dma("sp", o_s5i[4 * n:4 * n + 4, :], hblk[n][127:128, :, 1, :], reads=[hblk[n]],
                              writes=[o_s5i])
            if SMP:
                hv = hfin[:].rearrange("p n (a r q) -> p n a r q", a=4, r=2)
                for n in range(8):
                    P.dma("sp", o_s5r_s[:, 4 * n:4 * n + 4, :], hv[:, n, :, 0, :], reads=[hfin], writes=[o_s5r_s])
                    P.dma("sp", o_s5i_s[:, 4 * n:4 * n + 4, :], hv[:, n, :, 1, :], reads=[hfin], writes=[o_s5i_s])
            for n in range(0):
                P.dma("sp", o_s5r[4 * n:4 * n + 4, :], hblk[n][127:128, :, 0, :], reads=[hblk[n]], writes=[o_s5r])
                P.dma("sp", o_s5i[4 * n:4 * n + 4, :], hblk[n][127:128, :, 1, :], reads=[hblk[n]], writes=[o_s5i])
            P.barrier()
        P.emit()
        if MAXPH < 7:
            raise _Stop()

        out_proj_phase("h_", w_out_odd, mixed2_d, x2_d.ap, x2_d, x3_d, NTS)
        if MAXPH < 8:
            raise _Stop()

        r_es = contextlib.ExitStack()
        with r_es:
            def TR(name, shape, dt=F32, psum=False):
                return T(P, r_es, "r_" + name, shape, dt, psum)
            wr = TR("wr", [128, 8, 8], BF16)
            load_w(wr, moe_rw.rearrange("(kc kp) n -> kp kc n", kp=128), 8)
            rb = TR("rb", [128, 8])
            P.dma("sp", rb[:], moe_rb[0:1, :].partition_broadcast(128), writes=[rb])
            gf1 = TR("gf1", [128, D])
            P.dma("sp", gf1[:], norm_ffn[1:2, :].partition_broadcast(128), writes=[gf1])
            xt = [TR(f"xt{i}", [128, D]) for i in range(2)]
            hn = [TR(f"hn{i}", [128, D], BF16) for i in range(2)]
            hnT = [TR(f"hnT{i}", [128, 8, 128], BF16) for i in range(2)]
            sq = TR("sq", [128, D], BF16)
            ss = [TR(f"ss{i}", [128, 1]) for i in range(2)]
            rstd = [TR(f"rstd{i}", [128, 1]) for i in range(2)]
            lg = TR("lg", [128, 8]); l2 = TR("l2", [128, 8]); m1 = TR("m1", [128, 1]); m2 = TR("m2", [128, 1])
            eq = TR("eq", [128, 8]); sm = TR("sm", [128, 1])
            cw = [TR(f"cw{i}", [128, 8]) for i in range(2)]
            ps_tr = [TR(f"ps_tr{i}", [128, 1024], BF16, psum=True) for i in range(2)]
            ps_l = [TR(f"ps_l{i}", [128, 8], F32, psum=True) for i in range(2)]
            for tt in range(NTS):
                b = tt % 2
                P.dma("sp", xt[b][:], x3_d[tt * 128:(tt + 1) * 128, :], reads=[x3_d], writes=[xt[b]])
                rmsnorm_T(xt[b][:], xt[b], gf1, hn[b], sq, ss[b], rstd[b], ps_tr[b], hnT[b][:], hnT[b])
                pl = ps_l[b]
                for kc in range(8):
                    op("pe", lambda e, kc=kc, pl=pl, b=b: e.matmul(pl[:], lhsT=hnT[b][:, kc, :], rhs=wr[:, kc, :],
                                                                   start=(kc == 0), stop=(kc == 7)),
                       [hnT[b], wr], [pl])
                op("dve", lambda e, pl=pl: e.tensor_tensor(out=lg[:], in0=pl[:], in1=rb[:], op=ALU.add),
                   [pl, rb], [lg])
                op("dve", lambda e: e.reduce_max(out=m1[:], in_=lg[:], axis=AX.X), [lg], [m1])
                op("dve", lambda e: e.tensor_scalar(out=eq[:], in0=lg[:], scalar1=m1[:, 0:1], scalar2=-1.0e30,
                                                    op0=ALU.is_equal, op1=ALU.mult), [lg, m1], [eq])
                op("dve", lambda e: e.tensor_tensor(out=l2[:], in0=lg[:], in1=eq[:], op=ALU.add), [lg, eq], [l2])
                op("dve", lambda e: e.reduce_max(out=m2[:], in_=l2[:], axis=AX.X), [l2], [m2])
                op("dve", lambda e: e.tensor_scalar(out=eq[:], in0=lg[:], scalar1=m2[:, 0:1], scalar2=None,
                                                    op0=ALU.is_ge), [lg, m2], [eq])
                op("dve", lambda e: e.tensor_scalar(out=l2[:], in0=lg[:], scalar1=m1[:, 0:1], scalar2=None,
                                                    op0=ALU.subtract), [lg, m1], [l2])
                op("act", lambda e: e.activation(out=l2[:], in_=l2[:], func=AF.Exp), [l2], [l2])
                op("dve", lambda e: e.tensor_tensor(out=l2[:], in0=l2[:], in1=eq[:], op=ALU.mult), [l2, eq], [l2])
                op("dve", lambda e: e.reduce_sum(out=sm[:], in_=l2[:], axis=AX.X), [l2], [sm])
                op("dve", lambda e: e.reciprocal(out=sm[:], in_=sm[:]), [sm], [sm])
                cb = cw[b]
                op("dve", lambda e, cb=cb: e.tensor_scalar(out=cb[:], in0=l2[:], scalar1=sm[:, 0:1], scalar2=None,
                                                           op0=ALU.mult), [l2, sm], [cb])
                P.dma("sp", cw_d[tt * 128:(tt + 1) * 128, :], cb[:], reads=[cb], writes=[cw_d])
            P.barrier()
        P.emit()
        if MAXPH < 9:
            raise _Stop()

        NOWN = NT // 2
        o_es = contextlib.ExitStack()
        with o_es:
            def TO(name, shape, dt=F32, psum=False):
                return T(P, o_es, "o_" + name, shape, dt, psum)
            oidx = TO("oidx", [128, NOWN], I32)
            P.dma("sp", oidx[:], own_rows, writes=[oidx])
            xo = [TO(f"xo{i}", [128, D]) for i in range(2)]
            co = [TO(f"co{i}", [128, 8]) for i in range(2)]
            for i in range(NOWN + (1 if SMP else 0)):
                b = i % 2
                if i < NOWN:
                    P.idma(xo[b][:], x3_d.ap[:, :], oidx[:, i:i + 1], SEQ + 128, reads=[oidx, x3_d], writes=[xo[b]])
                    P.idma(co[b][:], cw_d.ap[:, :], oidx[:, i:i + 1], SEQ + 128, reads=[oidx, cw_d], writes=[co[b]])
                else:
                    P.dma("sp", xo[b][:], x3_d[SEQ:SEQ + 128, :], reads=[x3_d], writes=[xo[b]])
                    P.dma("sp", co[b][:], cw_d[SEQ:SEQ + 128, :], reads=[cw_d], writes=[co[b]])
                P.dma("sp", x3o_d[i * 128:(i + 1) * 128, :], xo[b][:], reads=[xo[b]], writes=[x3o_d])
                P.dma("sp", cwo_d[i * 128:(i + 1) * 128, :], co[b][:], reads=[co[b]], writes=[cwo_d])
            P.barrier()
        P.emit()

        NEXP = int(os.environ.get("KNEXP", "8"))
        OGROUPS = [list(range(g, min(g + 4, NOWN))) for g in range(0, NOWN, 4)] + ([[NOWN]] if SMP else [])
        src = x3o_d
        for ex in range(NEXP):
            dst = ya_d if ex % 2 == 0 else yb_d
            ffn_phase(f"x{ex}_", moe_wg[ex], moe_wu[ex], moe_wd[ex], norm_ffn[1:2, :], x3o_d, src, dst, OGROUPS,
                      cw_src=cwo_d, ecol=ex)
            src = dst
        if MAXPH < 10:
            raise _Stop()

        n_es = contextlib.ExitStack()
        with n_es:
            def TN(name, shape, dt=F32, psum=False):
                return T(P, n_es, "n_" + name, shape, dt, psum)
            gfin = TN("gfin", [128, D])
            P.dma("sp", gfin[:], norm_final[0:1, :].partition_broadcast(128), writes=[gfin])
            xt = [TN(f"xt{i}", [128, D]) for i in range(2)]
            yt = [TN(f"yt{i}", [128, D]) for i in range(2)]
            sq = TN("sq", [128, D], BF16)
            ss = [TN(f"ss{i}", [128, 1]) for i in range(2)]
            for tt in range(NOWN + (1 if SMP else 0)):
                b = tt % 2
                P.dma("sp", xt[b][:], src[tt * 128:(tt + 1) * 128, :], reads=[src], writes=[xt[b]])
                op("act", lambda e, b=b: e.activation(out=sq[:], in_=xt[b][:], func=AF.Square, accum_out=ss[b][:]),
                   [xt[b]], [sq, ss[b]])
                op("dve", lambda e, b=b: e.tensor_scalar(out=ss[b][:], in0=ss[b][:], scalar1=1.0 / D, scalar2=1e-6,
                                                         op0=ALU.mult, op1=ALU.add), [ss[b]], [ss[b]])
                op("act", lambda e, b=b: e.sqrt(out=ss[b][:], in_=ss[b][:]), [ss[b]], [ss[b]])
                op("dve", lambda e, b=b: e.reciprocal(out=ss[b][:], in_=ss[b][:]), [ss[b]], [ss[b]])
                op("dve", lambda e, b=b: e.scalar_tensor_tensor(out=yt[b][:], in0=xt[b][:], scalar=ss[b][:, 0:1],
                                                                in1=gfin[:], op0=ALU.mult, op1=ALU.mult),
                   [xt[b], ss[b], gfin], [yt[b]])
                if tt < NOWN:
                    P.dma("sp", o_yo[tt * 128:(tt + 1) * 128, :], yt[b][:], reads=[yt[b]], writes=[o_yo])
                else:
                    P.dma("sp", o_y_s[:], yt[b][:], reads=[yt[b]], writes=[o_y_s])
            P.barrier()
        P.emit()
    return nc


_CACHE = {}


def kernel(**inputs):
    f32 = np.float32
    if "nc" not in _CACHE:
        _CACHE["nc"] = build_program()
    nc = _CACHE["nc"]
    xp = np.asarray(inputs["x_prompt"], f32)
    xs = np.asarray(inputs["x_sample"], f32)
    hc = host_consts()

    def A(name, idx=None):
        a = np.asarray(inputs[name], f32)
        if idx is not None:
            a = a[idx]
        return np.ascontiguousarray(a)

    shared = {
        "norm_mix": A("norm_mix"), "norm_ffn": A("norm_ffn"),
        "w_in_even": A("w_in_even", 0), "w_out_even": A("w_out_even", 0),
        "hgrn_lb": A("hgrn_lb"), "hgrn_gnorm": A("hgrn_gnorm"),
        "ffn_w_gate": A("ffn_w_gate", 0), "ffn_w_up": A("ffn_w_up", 0), "ffn_w_down": A("ffn_w_down", 0),
        "w_in_odd": A("w_in_odd", 0), "w_out_odd": A("w_out_odd", 0),
        "norm_final": A("norm_final").reshape(1, D),
        "diff_lq1": A("diff_lq1"), "diff_lk1": A("diff_lk1"), "diff_lq2": A("diff_lq2"), "diff_lk2": A("diff_lk2"),
        "diff_subln": A("diff_subln"),
        "s5_a_re": A("s5_a_re", 0), "s5_a_im": A("s5_a_im", 0), "s5_log_dt": A("s5_log_dt"),
        "s5_b_re": A("s5_b_re", 0), "s5_b_im": A("s5_b_im", 0),
        "s5_c_re": A("s5_c_re", 0).reshape(512, 64), "s5_c_im": A("s5_c_im", 0).reshape(512, 64),
        "s5_d": A("s5_d", 0).reshape(1, 512), "s5_w_glu": A("s5_w_glu", 0), "s5_b_glu": A("s5_b_glu"),
        "moe_router_w": A("moe_router_w", 0), "moe_router_b": A("moe_router_b"),
        "moe_w_gate": A("moe_w_gate", 0), "moe_w_up": A("moe_w_up", 0), "moe_w_down": A("moe_w_down", 0),
    }
    for k, v in hc.items():
        shared["c_" + k] = v
    shared["pool_k"] = np.ascontiguousarray(np.asarray(inputs["cache_c_k"], f32)[0]).reshape(2560 * 128, 512)
    shared["pool_v"] = np.ascontiguousarray(np.asarray(inputs["cache_c_v"], f32)[0]).reshape(2560 * 128, 512)
    in_maps = []
    for c in range(NCORES):
        m = dict(shared)
        m["own_rows"] = np.ascontiguousarray(
            ((c % 2) * (SEQ // 2) + np.arange(SEQ // 2, dtype=np.int32)).reshape(NT // 2, 128).T)
        m["page_table"] = np.ascontiguousarray(np.asarray(inputs["page_table"], np.int32)[16 * c:16 * c + 16]).reshape(1, 256)
        m["x_seq"] = np.ascontiguousarray(xp[c // 2][:SEQ])
        m["x_smp"] = np.ascontiguousarray(xs[16 * c:16 * c + 16].reshape(128, D))
        m["cache_a_k"] = np.ascontiguousarray(np.asarray(inputs["cache_a_k"], f32)[0, 16 * c:16 * c + 16]).reshape(16, 2048, 512)
        m["cache_a_v"] = np.ascontiguousarray(np.asarray(inputs["cache_a_v"], f32)[0, 16 * c:16 * c + 16]).reshape(16, 2048, 512)
        m["state_s5_re"] = np.ascontiguousarray(np.asarray(inputs["state_s5_re"], f32)[0, 16 * c:16 * c + 16])
        m["state_s5_im"] = np.ascontiguousarray(np.asarray(inputs["state_s5_im"], f32)[0, 16 * c:16 * c + 16])
        m["state_hgrn"] = np.ascontiguousarray(np.asarray(inputs["state_hgrn"], f32)[0, 16 * c:16 * c + 16])
        in_maps.append(m)
    res = run_bass_kernel_spmd(nc, in_maps, core_ids=list(range(NCORES))).results

    B, DB, DS = 4, 128, 8
    y_prompt = np.stack([np.concatenate([res[2 * b]["o_yo"], res[2 * b + 1]["o_yo"]]) for b in range(B)])
    y_sample = np.concatenate([res[c]["o_y_s"].reshape(16, 8, D) for c in range(NCORES)])
    a_k = np.stack([res[2 * b]["o_ak"].reshape(NKEEP * 128, 8, 64) for b in range(B)])[None]
    a_v = np.stack([res[2 * b]["o_av"].reshape(NKEEP * 128, 8, 64) for b in range(B)])[None]
    hgrn = np.stack([res[2 * b]["o_hg"] for b in range(B)])[None]
    c_k = np.stack([res[2 * b]["o_ck"].reshape(SEQ, 4, 128) for b in range(B)])[None]
    c_v = np.stack([res[2 * b]["o_cv"].reshape(SEQ, 4, 128) for b in range(B)])[None]
    s5r = np.stack([res[2 * b]["o_s5r"] for b in range(B)])[None]
    s5i = np.stack([res[2 * b]["o_s5i"] for b in range(B)])[None]
    a_k_s = np.concatenate([res[c]["o_ak_s"].reshape(16, 8, 8, 64) for c in range(NCORES)])[None]
    a_v_s = np.concatenate([res[c]["o_av_s"].reshape(16, 8, 8, 64) for c in range(NCORES)])[None]
    hgrn_s = np.concatenate([res[c]["o_hg_s"] for c in range(NCORES)])[None]
    c_k_s = np.concatenate([res[c]["o_ck_s"].reshape(16, 8, 4, 128) for c in range(NCORES)])[None]
    c_v_s = np.concatenate([res[c]["o_cv_s"].reshape(16, 8, 4, 128) for c in range(NCORES)])[None]
    s5r_s = np.concatenate([res[c]["o_s5r_s"] for c in range(NCORES)])[None]
    s5i_s = np.concatenate([res[c]["o_s5i_s"] for c in range(NCORES)])[None]
    return (y_prompt, y_sample, a_k, a_v, hgrn, c_k, c_v, s5r, s5i,
            a_k_s, a_v_s, hgrn_s, c_k_s, c_v_s, s5r_s, s5i_s)
```
